# Optimizing a Trainium2 kernel written in Bass

```python
import math
import jax, jax.numpy as jnp
from jax import lax
import numpy as np

D_MODEL = 4096
BATCH = 8
SEQ = 2048
DEPTH = 2

PLE_DIM = 256
HEAD_DIM = 128
MOBA_HEADS = 8
MOBA_BLOCK = 256
MOBA_TOPK = 3
MOBA_QCHUNK = 16
GDN_HEADS = 16
GDN_DK = 128
GDN_DV = 128
GDN_CONV = 4
GDN_CHUNK = 64
MLA_HEADS = 8
MLA_Q_RANK = 768
MLA_KV_RANK = 512
MLA_NOPE = 128
MLA_ROPE = 64
MLA_V = 128
MLA_QBLOCK = 128
ROPE_THETA = 10000.0
D_FF = -(-8 * D_MODEL // (3 * 256)) * 256

MOBA_W = MOBA_HEADS * HEAD_DIM
GDN_KW = GDN_HEADS * GDN_DK
GDN_VW = GDN_HEADS * GDN_DV
MLA_VW = MLA_HEADS * MLA_V
IN_SIZES = (MOBA_W, MOBA_W, MOBA_W, GDN_KW, GDN_KW, GDN_VW, GDN_HEADS, GDN_HEADS, GDN_VW,
            MLA_Q_RANK, MLA_KV_RANK, MLA_ROPE)
IN_WIDTH = sum(IN_SIZES)

kernel_name = 'hybrid_moba_gdn_mla_gated_block'


def _rmsnorm(x, gain, eps=1e-6):
    xf = x.astype(jnp.float32)
    xf = xf * lax.rsqrt(jnp.mean(xf * xf, axis=-1, keepdims=True) + eps)
    return (xf * gain.astype(jnp.float32)).astype(x.dtype)


def _l2norm(x, eps=1e-6):
    return x * lax.rsqrt(jnp.sum(x * x, axis=-1, keepdims=True) + eps)


def _alibi_slopes(n_heads):
    return 2.0 ** (-8.0 * jnp.arange(1, n_heads + 1, dtype=jnp.float32) / n_heads)


def _rope(x, pos):
    half = x.shape[-1] // 2
    inv_freq = 1.0 / (ROPE_THETA ** (jnp.arange(half, dtype=jnp.float32) / half))
    ang = pos.astype(jnp.float32)[..., None] * inv_freq
    ang = ang.reshape(ang.shape[:2] + (1,) * (x.ndim - 3) + (half,))
    cos, sin = jnp.cos(ang), jnp.sin(ang)
    xf = x.astype(jnp.float32)
    x1, x2 = xf[..., :half], xf[..., half:]
    return jnp.concatenate([x1 * cos - x2 * sin, x1 * sin + x2 * cos], axis=-1).astype(x.dtype)


def _moba_attention(q, k, v, pos):
    B, S, H, dh = q.shape
    blk, qcl = MOBA_BLOCK, MOBA_QCHUNK
    nb = -(-S // blk)
    sp = nb * blk
    pad = ((0, 0), (0, sp - S), (0, 0), (0, 0))
    q, k, v = (jnp.pad(t, pad).transpose(0, 2, 1, 3) for t in (q, k, v))
    posp = jnp.pad(pos, ((0, 0), (0, sp - S)), mode='edge')
    kb = k.reshape(B, H, nb, blk, dh)
    vb = v.reshape(B, H, nb, blk, dh)
    pb = posp.reshape(B, nb, blk)
    kmean = jnp.mean(kb.astype(jnp.float32), axis=3)
    gate = jnp.einsum('bhsd,bhnd->bhsn', q.astype(jnp.float32), kmean)
    q_block = jnp.arange(sp) // blk
    fully_past = jnp.arange(nb)[None, :] < q_block[:, None]
    gate = jnp.where(fully_past, gate, -jnp.inf)
    n_sel = min(MOBA_TOPK, nb)
    _, sel = lax.top_k(gate, n_sel)
    slopes = _alibi_slopes(H)
    scale = dh ** -0.5
    bi = jnp.arange(B)[:, None, None, None]
    hi = jnp.arange(H)[None, :, None, None]
    nc = sp // qcl
    q_chunks = q.reshape(B, H, nc, qcl, dh).transpose(2, 0, 1, 3, 4)
    sel_chunks = sel.reshape(B, H, nc, qcl, n_sel).transpose(2, 0, 1, 3, 4)

    def chunk(args):
        qc, ic, c = args
        start = c * qcl
        own = start // blk
        tq = start + jnp.arange(qcl)
        pq = lax.dynamic_slice_in_dim(posp, start, qcl, axis=1)
        ks = kb[bi, hi, ic]
        vs = vb[bi, hi, ic]
        ps = pb[bi, ic]
        s_sel = jnp.einsum('bhqd,bhqnkd->bhqnk', qc, ks).astype(jnp.float32) * scale
        s_sel = s_sel - slopes[:, None, None, None] * jnp.abs(pq[:, None, :, None, None] - ps).astype(jnp.float32)
        s_sel = jnp.where((ic < own)[..., None], s_sel, -jnp.inf).reshape(B, H, qcl, n_sel * blk)
        ko = lax.dynamic_slice_in_dim(k, own * blk, blk, axis=2)
        vo = lax.dynamic_slice_in_dim(v, own * blk, blk, axis=2)
        po = lax.dynamic_slice_in_dim(posp, own * blk, blk, axis=1)
        s_own = jnp.einsum('bhqd,bhkd->bhqk', qc, ko).astype(jnp.float32) * scale
        s_own = s_own - slopes[:, None, None] * jnp.abs(pq[:, None, :, None] - po[:, None, None, :]).astype(jnp.float32)
        tk = own * blk + jnp.arange(blk)
        s_own = jnp.where(tk[None, :] <= tq[:, None], s_own, -jnp.inf)
        probs = jax.nn.softmax(jnp.concatenate([s_sel, s_own], axis=-1), axis=-1).astype(v.dtype)
        o = jnp.einsum('bhqk,bhqkd->bhqd', probs[..., :n_sel * blk], vs.reshape(B, H, qcl, n_sel * blk, dh))
        return o + jnp.einsum('bhqk,bhkd->bhqd', probs[..., n_sel * blk:], vo)

    out = lax.map(chunk, (q_chunks, sel_chunks, jnp.arange(nc)))
    return out.transpose(1, 0, 3, 2, 4).reshape(B, sp, H * dh)[:, :S]


def _gated_delta_net(q, k, v, a, b, z, conv_w, a_log, dt_bias, norm_w):
    B, S, _ = q.shape
    H, dk, dv, L = GDN_HEADS, GDN_DK, GDN_DV, GDN_CHUNK
    qkv = jnp.concatenate([q, k, v], axis=-1)
    ch = qkv.shape[-1]
    qkv = lax.conv_general_dilated(qkv, conv_w[:, None, :].astype(qkv.dtype), window_strides=(1,),
                                   padding=[(GDN_CONV - 1, 0)], dimension_numbers=('NWC', 'WIO', 'NWC'),
                                   feature_group_count=ch)
    qkv = jax.nn.silu(qkv.astype(jnp.float32))
    q, k, v = jnp.split(qkv, [H * dk, 2 * H * dk], axis=-1)
    q = _l2norm(q.reshape(B, S, H, dk)) * dk ** -0.5
    k = _l2norm(k.reshape(B, S, H, dk))
    v = v.reshape(B, S, H, dv)
    beta = jax.nn.sigmoid(b.astype(jnp.float32))
    g = -jnp.exp(a_log.astype(jnp.float32)) * jax.nn.softplus(a.astype(jnp.float32) + dt_bias.astype(jnp.float32))
    n = S // L

    def chunks(t):
        return t.reshape(B, n, L, H, -1).transpose(0, 3, 1, 2, 4)

    qc, kc, vc = chunks(q), chunks(k), chunks(v)
    gc = jnp.cumsum(chunks(g[..., None])[..., 0], axis=-1)
    bc = chunks(beta[..., None])
    incl = jnp.tril(jnp.ones((L, L), dtype=bool))
    strict = jnp.tril(jnp.ones((L, L), dtype=bool), -1)
    decay = jnp.exp(jnp.where(incl, gc[..., :, None] - gc[..., None, :], -jnp.inf))
    kbeta = kc * bc
    a_kk = jnp.where(strict, jnp.einsum('bhnid,bhnjd->bhnij', kbeta, kc) * decay, 0.0)
    eye = jnp.eye(L, dtype=jnp.float32)
    rhs = jnp.concatenate([vc * bc, kbeta * jnp.exp(gc)[..., None]], axis=-1)
    uw = lax.linalg.triangular_solve(a_kk + eye, rhs, left_side=True, lower=True, unit_diagonal=True)
    u, w = uw[..., :dv], uw[..., dv:]
    a_qk = jnp.where(incl, jnp.einsum('bhnid,bhnjd->bhnij', qc, kc) * decay, 0.0)
    q_dec = qc * jnp.exp(gc)[..., None]
    k_tail = kc * jnp.exp(gc[..., -1:] - gc)[..., None]
    g_last = jnp.exp(gc[..., -1])

    def step(state, xs):
        qd, kt, uu, ww, aqk, gl = xs
        v_new = uu - jnp.einsum('bhlk,bhkv->bhlv', ww, state)
        o = jnp.einsum('bhlk,bhkv->bhlv', qd, state) + jnp.einsum('bhij,bhjv->bhiv', aqk, v_new)
        state = state * gl[..., None, None] + jnp.einsum('bhlk,bhlv->bhkv', kt, v_new)
        return state, o

    def mv(t):
        return jnp.moveaxis(t, 2, 0)

    s0 = jnp.zeros((B, H, dk, dv), jnp.float32)
    _, o = lax.scan(step, s0, (mv(q_dec), mv(k_tail), mv(u), mv(w), mv(a_qk), mv(g_last)))
    o = o.transpose(1, 0, 3, 2, 4).reshape(B, S, H, dv)
    o = _rmsnorm(o, norm_w) * jax.nn.silu(z.reshape(B, S, H, dv).astype(jnp.float32))
    return o.reshape(B, S, H * dv).astype(z.dtype)


def _causal_attention(q, k, v, scale):
    B, S, H, dq = q.shape
    dv = v.shape[-1]
    qb = MLA_QBLOCK
    nq = S // qb
    q_blocks = q.reshape(B, nq, qb, H, dq).transpose(1, 0, 2, 3, 4)
    tk = jnp.arange(S)

    def block(args):
        qi, i = args
        s = jnp.einsum('bqhd,bkhd->bhqk', qi, k).astype(jnp.float32) * scale
        tq = i * qb + jnp.arange(qb)
        s = jnp.where(tk[None, :] <= tq[:, None], s, -jnp.inf)
        probs = jax.nn.softmax(s, axis=-1).astype(v.dtype)
        return jnp.einsum('bhqk,bkhd->bqhd', probs, v)

    o = lax.map(block, (q_blocks, jnp.arange(nq)))
    return o.transpose(1, 0, 2, 3, 4).reshape(B, S, H * dv)


def _mla_attention(c_q, c_kv, k_rope, pos, q_norm_w, w_uq, kv_norm_w, w_ukv):
    B, S, _ = c_q.shape
    H = MLA_HEADS
    q = (_rmsnorm(c_q, q_norm_w) @ w_uq).reshape(B, S, H, MLA_NOPE + MLA_ROPE)
    kv = (_rmsnorm(c_kv, kv_norm_w) @ w_ukv).reshape(B, S, H, MLA_NOPE + MLA_V)
    q_nope, q_pe = q[..., :MLA_NOPE], q[..., MLA_NOPE:]
    k_nope, v = kv[..., :MLA_NOPE], kv[..., MLA_NOPE:]
    q_pe = _rope(q_pe, pos)
    k_pe = _rope(k_rope, pos)
    qf = jnp.concatenate([q_nope, q_pe], axis=-1)
    kf = jnp.concatenate([k_nope, jnp.broadcast_to(k_pe[:, :, None, :], (B, S, H, MLA_ROPE))], axis=-1)
    return _causal_attention(qf, kf, v, (MLA_NOPE + MLA_ROPE) ** -0.5)


def setup_inputs(seed: int = 0) -> dict:
    key = jax.random.key(seed)
    ks = jax.random.split(key, 32)
    Lr, D = DEPTH, D_MODEL

    def dense(k, shape, fan_in):
        return jax.random.normal(k, shape, jnp.float32) * fan_in ** -0.5

    def gain(k, shape):
        return 1.0 + 0.05 * jax.random.normal(k, shape, jnp.float32)

    x = jax.random.normal(ks[0], (BATCH, SEQ, D), jnp.float32)
    p = jax.random.normal(ks[1], (Lr, BATCH, SEQ, PLE_DIM), jnp.float32)
    start = jax.random.randint(ks[2], (BATCH, 1), 0, 1024, dtype=jnp.int32)
    steps = jax.random.randint(ks[3], (BATCH, SEQ), 1, 3, dtype=jnp.int32)
    positions = start + jnp.cumsum(steps, axis=1) - 1
    dt = jnp.exp(jax.random.uniform(ks[4], (Lr, GDN_HEADS), jnp.float32, math.log(1e-3), math.log(0.1)))
    gdn_dt_bias = dt + jnp.log(-jnp.expm1(-dt))
    gdn_a_log = jnp.log(jax.random.uniform(ks[5], (Lr, GDN_HEADS), jnp.float32, 1.0, 16.0))
    return {
        'x': x,
        'p': p,
        'positions': positions,
        'norm_mix_in': gain(ks[6], (Lr, D)),
        'w_in': dense(ks[7], (Lr, D, IN_WIDTH), D),
        'gdn_conv_w': dense(ks[8], (Lr, GDN_CONV, 2 * GDN_KW + GDN_VW), GDN_CONV),
        'gdn_a_log': gdn_a_log,
        'gdn_dt_bias': gdn_dt_bias,
        'gdn_norm_w': gain(ks[9], (Lr, GDN_DV)),
        'mla_q_norm_w': gain(ks[10], (Lr, MLA_Q_RANK)),
        'mla_w_uq': dense(ks[11], (Lr, MLA_Q_RANK, MLA_HEADS * (MLA_NOPE + MLA_ROPE)), MLA_Q_RANK),
        'mla_kv_norm_w': gain(ks[12], (Lr, MLA_KV_RANK)),
        'mla_w_ukv': dense(ks[13], (Lr, MLA_KV_RANK, MLA_HEADS * (MLA_NOPE + MLA_V)), MLA_KV_RANK),
        'w_branch_gate': dense(ks[14], (Lr, D, 3 * D), D),
        'w_branch_a': dense(ks[15], (Lr, MOBA_W, D), MOBA_W),
        'w_branch_b': dense(ks[16], (Lr, GDN_VW, D), GDN_VW),
        'w_branch_c': dense(ks[17], (Lr, MLA_VW, D), MLA_VW),
        'w_out': dense(ks[18], (Lr, D, D), D),
        'norm_mix_out': gain(ks[19], (Lr, D)),
        'norm_ffn_in': gain(ks[20], (Lr, D)),
        'w_ffn_gate': dense(ks[21], (Lr, D, D_FF), D),
        'w_ffn_up': dense(ks[22], (Lr, D, D_FF), D),
        'w_ffn_down': dense(ks[23], (Lr, D_FF, D), D_FF),
        'norm_ffn_out': gain(ks[24], (Lr, D)),
        'w_ple_gate': dense(ks[25], (Lr, D, D), D),
        'w_ple_proj': dense(ks[26], (Lr, PLE_DIM, D), PLE_DIM),
    }


def reference(x, p, positions, norm_mix_in, w_in, gdn_conv_w, gdn_a_log, gdn_dt_bias, gdn_norm_w,
              mla_q_norm_w, mla_w_uq, mla_kv_norm_w, mla_w_ukv, w_branch_gate, w_branch_a, w_branch_b,
              w_branch_c, w_out, norm_mix_out, norm_ffn_in, w_ffn_gate, w_ffn_up, w_ffn_down,
              norm_ffn_out, w_ple_gate, w_ple_proj):
    B, S, D = x.shape
    split_at = np.cumsum(IN_SIZES)[:-1].tolist()
    moba_shape = (B, S, MOBA_HEADS, HEAD_DIM)
    for i in range(DEPTH):
        h = _rmsnorm(x, norm_mix_in[i])
        (a_q, a_k, a_v, b_q, b_k, b_v, b_a, b_b, b_z, c_q, c_kv, c_kr) = jnp.split(h @ w_in[i], split_at, axis=-1)
        y_a = _moba_attention(a_q.reshape(moba_shape), a_k.reshape(moba_shape), a_v.reshape(moba_shape), positions)
        y_b = _gated_delta_net(b_q, b_k, b_v, b_a, b_b, b_z, gdn_conv_w[i], gdn_a_log[i], gdn_dt_bias[i], gdn_norm_w[i])
        y_c = _mla_attention(c_q, c_kv, c_kr, positions, mla_q_norm_w[i], mla_w_uq[i], mla_kv_norm_w[i], mla_w_ukv[i])
        g_a, g_b, g_c = jnp.split(jax.nn.sigmoid(h @ w_branch_gate[i]), 3, axis=-1)
        merged = g_a * (y_a @ w_branch_a[i]) + g_b * (y_b @ w_branch_b[i]) + g_c * (y_c @ w_branch_c[i])
        x = x + _rmsnorm(merged @ w_out[i], norm_mix_out[i])
        h = _rmsnorm(x, norm_ffn_in[i])
        f = (jax.nn.silu(h @ w_ffn_gate[i]) * (h @ w_ffn_up[i])) @ w_ffn_down[i]
        x = x + _rmsnorm(f, norm_ffn_out[i])
        x = x + jax.nn.sigmoid(x @ w_ple_gate[i]) * (p[i] @ w_ple_proj[i])
    return x
```

```python
import math
import numpy as np
import concourse.bass as bass
import concourse.mybir as mybir
from concourse.bass_utils import run_bass_kernel_spmd

F32 = mybir.dt.float32
BF16 = mybir.dt.bfloat16
I32 = mybir.dt.int32
AF = mybir.ActivationFunctionType
ALU = mybir.AluOpType
AX = mybir.AxisListType

ENGS = ("pe", "act", "dve", "pool", "sp")
S_TOK = 2048
NEG = -30000.0


class Res:
    __slots__ = ("w", "r")

    def __init__(self):
        self.w = None
        self.r = []


class Op:
    __slots__ = ("eng", "fn", "deps", "dma", "needed", "cnt", "dsem", "dval", "done", "key")

    def __init__(self, eng, fn, dma):
        self.eng = eng
        self.fn = fn
        self.deps = []
        self.dma = dma
        self.needed = False
        self.cnt = 0
        self.dsem = None
        self.dval = 0
        self.done = False


class Sched:
    NDMA = 12

    def __init__(self, nc):
        self.nc = nc
        self.ops = {e: [] for e in ENGS}
        self.esem = {e: nc.alloc_semaphore(name=f"es_{e}") for e in ENGS}
        self.ecnt = {e: 0 for e in ENGS}
        qs = ("sp", "act", "pool")
        self.dsems = {e: [nc.alloc_semaphore(name=f"ds_{e}{i}") for i in range(self.NDMA)] for e in qs}
        self.dcnt = {e: [0] * self.NDMA for e in qs}
        self.dnext = {e: 0 for e in qs}
        self.dlast = {e: [None] * self.NDMA for e in qs}
        self.waited = {e: {} for e in ENGS}
        self.pending_dma = []
        self.nops = 0
        self.cur = None
        self.nstreams = 0
        self.sops = []
        self.sidx = []
        self.sdn = []

    def begin_streams(self, n):
        self.nstreams = n
        self.sops = [{e: [] for e in ENGS} for _ in range(n)]
        self.sidx = [0] * n
        per = self.NDMA // n
        self.sslots = [list(range(i * per, (i + 1) * per)) for i in range(n)]
        self.sdn = [{q: 0 for q in ("sp", "act", "pool")} for _ in range(n)]

    def merge_streams(self):
        n = self.nstreams
        lens = [max(1, self.sidx[i]) for i in range(n)]
        for e in ENGS:
            allops = []
            for i in range(n):
                for o in self.sops[i][e]:
                    o.key = (o.key[0] / lens[i], i)
                    allops.append(o)
            allops.sort(key=lambda o: o.key)
            self.ops[e].extend(allops)
        self.cur = None
        self.nstreams = 0
        self.sops = []

    def op(self, eng, fn, reads=(), writes=(), dma=False):
        o = Op(eng, fn, dma)
        deps = o.deps
        for r in reads:
            if r.w is not None and not r.w.done:
                deps.append(r.w)
        for w in writes:
            if w.w is not None and not w.w.done:
                deps.append(w.w)
            for x in w.r:
                if not x.done:
                    deps.append(x)
        for r in reads:
            r.r.append(o)
        for w in writes:
            w.w = o
            w.r = []
        if dma:
            q = eng
            if self.cur is None:
                i = self.dnext[q]
                self.dnext[q] = (i + 1) % self.NDMA
            else:
                sl_ = self.sslots[self.cur]
                i = sl_[self.sdn[self.cur][q] % len(sl_)]
                self.sdn[self.cur][q] += 1
            prev = self.dlast[q][i]
            if prev is not None and not prev.done:
                deps.append(prev)
            self.dlast[q][i] = o
            self.dcnt[q][i] += 16
            o.dsem = self.dsems[q][i]
            o.dval = self.dcnt[q][i]
            self.pending_dma.append(o)
        if self.cur is None:
            self.ops[eng].append(o)
        else:
            c = self.cur
            o.key = (self.sidx[c], c)
            self.sidx[c] += 1
            self.sops[c][eng].append(o)
        self.nops += 1
        return o

    def flush(self):
        nc = self.nc
        for e in ENGS:
            for o in self.ops[e]:
                for d in o.deps:
                    if d.dma:
                        continue
                    if d.eng == "pe" and o.eng == "pe" and not o.dma:
                        continue
                    d.needed = True
        for e in ENGS:
            c = self.ecnt[e]
            for o in self.ops[e]:
                if o.dma:
                    continue
                if o.needed:
                    c += 1
                    o.cnt = c
            self.ecnt[e] = c
        pend = self.pending_dma
        esem = self.esem
        with nc.Block() as block:
            for e in ENGS:
                ops = self.ops[e]
                is_last = (e == "sp")
                if not ops and not (is_last and pend):
                    continue
                waited = self.waited[e]

                def body(eng, ops=ops, e=e, waited=waited, is_last=is_last):
                    for o in ops:
                        for d in o.deps:
                            if d.dma:
                                s, v = d.dsem, d.dval
                            else:
                                if d.eng == "pe" and e == "pe" and not o.dma:
                                    continue
                                s, v = esem[d.eng], d.cnt
                            k = id(s)
                            if waited.get(k, 0) >= v:
                                continue
                            waited[k] = v
                            eng.wait_ge(s, v)
                        inst = o.fn(eng)
                        if o.dma:
                            inst.then_inc(o.dsem, 16)
                        elif o.needed:
                            inst.then_inc(esem[e], 1)
                    if is_last:
                        for o in pend:
                            k = id(o.dsem)
                            if waited.get(k, 0) >= o.dval:
                                continue
                            waited[k] = o.dval
                            eng.wait_ge(o.dsem, o.dval)

                {"pe": block.tensor, "act": block.scalar, "dve": block.vector,
                 "pool": block.gpsimd, "sp": block.sync}[e](body)
        for e in ENGS:
            for o in self.ops[e]:
                o.fn = None
                o.done = True
        self.ops = {e: [] for e in ENGS}
        self.pending_dma = []


MOBA_W = 1024
GDN_KW = 2048
GDN_VW = 2048
IN_SIZES = (1024, 1024, 1024, 2048, 2048, 2048, 16, 16, 2048, 768, 512, 64)
IN_OFF = np.concatenate([[0], np.cumsum(IN_SIZES)]).astype(int).tolist()
IN_WIDTH = IN_OFF[-1]


class KB:
    def __init__(self, D, DFF, DEPTH, debug=()):
        self.D, self.DFF, self.DEPTH = D, DFF, DEPTH
        self.C = D // 128
        self.CF = DFF // 128
        self.debug = set(debug)
        self.nc = nc = bass.Bass("TRN2", target_bir_lowering=False)
        self.S = Sched(nc)
        self.BASE = 16512
        self.TOP = 229344
        self.off = self.BASE
        self.uid = 0
        self.banks = [(nc.alloc_psum_tensor(f"psb{i}", [128, 512], F32), Res()) for i in range(8)]
        self.rot = [0, 1, 2, 3]
        self.ri = 0
        self.fxmap = {}
        self.dr = {}

    def sb(self, shape, dt):
        esz = 2 if dt == BF16 else 4
        nb = int(np.prod(shape[1:])) * esz
        nb = (nb + 63) // 64 * 64
        assert self.off + nb <= self.TOP, f"SBUF overflow {self.off + nb}"
        self.uid += 1
        t = self.nc.alloc_sbuf_tensor_at(f"sb{self.uid}", list(shape), dt, offset=self.off)
        self.off += nb
        return t, Res()

    def mark(self):
        return self.off

    def release(self, m):
        self.off = m

    def bank(self):
        i = self.rot[self.ri % len(self.rot)]
        self.ri += 1
        return self.banks[i]

    def fixed(self, i):
        return self.banks[self.fxmap.get(i, i)]

    def use_stream(self, sid, rot, fxmap=None):
        self.S.cur = sid
        self.rot = list(rot)
        self.ri = 0
        self.fxmap = dict(fxmap or {})

    def end_streams(self):
        self.S.merge_streams()
        self.rot = [0, 1, 2, 3]
        self.ri = 0
        self.fxmap = {}

    def dram(self, name, shape, dt, kind=None):
        if kind is None:
            kind = "ExternalOutput" if name in self.debug else "Internal"
        t = self.nc.dram_tensor(name, list(shape), dt, kind=kind).ap()
        self.dr[name] = t
        return t

    def mm(self, out, lhsT, rhs, start, stop, R, W):
        self.S.op("pe", lambda e: e.matmul(out, lhsT, rhs, start=start, stop=stop), R, W)

    def tr(self, out, in_, ident, R, W):
        self.S.op("pe", lambda e: e.transpose(out, in_, ident), R, W)

    def act(self, out, in_, func, R, W, scale=None, bias=None):
        kw = {}
        if scale is not None:
            kw["scale"] = scale
        if bias is not None:
            kw["bias"] = bias
        self.S.op("act", lambda e: e.activation(out=out, in_=in_, func=func, **kw), R, W)

    def tt(self, out, in0, in1, op, R, W, eng="dve"):
        self.S.op(eng, lambda e: e.tensor_tensor(out=out, in0=in0, in1=in1, op=op), R, W)

    def ts(self, out, in0, s1, op0, R, W, s2=None, op1=None, eng="dve"):
        if op1 is None:
            self.S.op(eng, lambda e: e.tensor_scalar(out=out, in0=in0, scalar1=s1, scalar2=None, op0=op0), R, W)
        else:
            self.S.op(eng, lambda e: e.tensor_scalar(out=out, in0=in0, scalar1=s1, scalar2=s2, op0=op0, op1=op1), R, W)

    def stt(self, out, in0, scalar, in1, op0, op1, R, W):
        self.S.op("dve", lambda e: e.scalar_tensor_tensor(out=out, in0=in0, scalar=scalar, in1=in1, op0=op0, op1=op1), R, W)

    def cp(self, eng, out, in_, R, W):
        if eng == "act":
            self.S.op("act", lambda e: e.activation(out=out, in_=in_, func=AF.Copy), R, W)
        else:
            self.S.op(eng, lambda e: e.tensor_copy(out=out, in_=in_), R, W)

    def recip(self, out, in_, R, W):
        self.S.op("dve", lambda e: e.reciprocal(out=out, in_=in_), R, W)

    def memset(self, eng, ap, val, W):
        self.S.op(eng, lambda e: e.memset(ap, val), (), W)

    def dma(self, q, out, in_, R, W):
        self.S.op(q, lambda e: e.dma_start(out=out, in_=in_), R, W, dma=True)

    def flush(self):
        self.S.flush()

    def setup_consts(self, c_ident):
        self.ident_f, self.r_ident_f = self.sb([128, 128], F32)
        self.ident_b, self.r_ident_b = self.sb([128, 128], BF16)
        self.ones_b, self.r_ones_b = self.sb([128, 128], BF16)
        self.cb, self.r_cb = self.sb([128, 4], F32)
        self.dma("sp", self.ident_f[:], c_ident, [], [self.r_ident_f])
        self.dma("pool", self.ident_b[:], c_ident, [], [self.r_ident_b])
        self.memset("dve", self.ones_b[:], 1.0, [self.r_ones_b])
        self.memset("dve", self.cb[:, 0:1], 1e-6, [self.r_cb])
        self.memset("dve", self.cb[:, 1:2], 1.0, [self.r_cb])
        self.memset("dve", self.cb[:, 2:3], math.pi / 2, [self.r_cb])
        self.memset("dve", self.cb[:, 3:4], 0.0, [self.r_cb])
        self.eps = self.cb[:, 0:1]
        self.persist = self.off
        self.flush()

    def rstd(self, out, ores, ps, pres, n, scale, tmp, tres):
        self.act(tmp[:n, :], ps[:n, :], AF.Sqrt, [pres, self.r_cb], [tres], scale=scale, bias=self.cb[:n, 0:1])
        self.recip(out[:n, :], tmp[:n, :], [tres], [ores])

    def ph_norm(self, src, C, gain_dram, dst, dst_dt=BF16):
        m = self.mark()
        g, gr = self.sb([128, C], F32)
        self.dma("sp", g[:], gain_dram, [], [gr])
        xts = [self.sb([128, C, 512], F32) for _ in range(2)]
        hts = [self.sb([128, C, 512], dst_dt) for _ in range(2)]
        sqs = [self.sb([128, 512], BF16) for _ in range(4)]
        tmp, tmr = self.sb([128, 512], F32)
        rs, rsr = self.sb([128, 512], F32)
        sv = src.rearrange("(c p) s -> p c s", p=128)
        dv = dst.rearrange("(c p) s -> p c s", p=128)
        for tb in range(4):
            xt, xr = xts[tb % 2]
            ht, hr = hts[tb % 2]
            sl = slice(tb * 512, (tb + 1) * 512)
            self.dma("sp", xt[:], sv[:, :, sl], [], [xr])
            ps, pr = self.bank()
            for c in range(C):
                sq, sr = sqs[c % 4]
                self.act(sq[:], xt[:, c, :], AF.Square, [xr], [sr])
                self.mm(ps[:], self.ones_b[:], sq[:], c == 0, c == C - 1, [sr, self.r_ones_b], [pr])
            self.rstd(rs, rsr, ps, pr, 128, 1.0 / (C * 128), tmp, tmr)
            for c in range(C):
                self.stt(ht[:, c, :], xt[:, c, :], g[:, c:c + 1], rs[:], ALU.mult, ALU.mult, [xr, gr, rsr], [hr])
            self.dma("sp", dv[:, :, sl], ht[:], [hr], [])
        self.flush()
        self.release(m)

    def ph_resnorm(self, xsrc, usrc, gain_u, xdst, gain_n=None, ndst=None, bdst=None):
        C = self.C
        m = self.mark()
        gu, gur = self.sb([128, C], F32)
        self.dma("sp", gu[:], gain_u, [], [gur])
        if gain_n is not None:
            gn, gnr = self.sb([128, C], F32)
            self.dma("sp", gn[:], gain_n, [], [gnr])
        xt, xr = self.sb([128, C, 512], F32)
        ut, ur = self.sb([128, C, 512], F32)
        ht, hr = self.sb([128, C, 512], BF16)
        sqs = [self.sb([128, 512], BF16) for _ in range(4)]
        tmp, tmr = self.sb([128, 512], F32)
        rs, rsr = self.sb([128, 512], F32)
        xv = xsrc.rearrange("(c p) s -> p c s", p=128)
        uv = usrc.rearrange("(c p) s -> p c s", p=128)
        xdv = xdst.rearrange("(c p) s -> p c s", p=128)
        for tb in range(4):
            sl = slice(tb * 512, (tb + 1) * 512)
            self.dma("sp", xt[:], xv[:, :, sl], [], [xr])
            self.dma("sp", ut[:], uv[:, :, sl], [], [ur])
            ps, pr = self.bank()
            for c in range(C):
                sq, sr = sqs[c % 4]
                self.act(sq[:], ut[:, c, :], AF.Square, [ur], [sr])
                self.mm(ps[:], self.ones_b[:], sq[:], c == 0, c == C - 1, [sr, self.r_ones_b], [pr])
            self.rstd(rs, rsr, ps, pr, 128, 1.0 / (C * 128), tmp, tmr)
            if gain_n is not None:
                ps2, pr2 = self.bank()
            for c in range(C):
                self.stt(ut[:, c, :], ut[:, c, :], gu[:, c:c + 1], rs[:], ALU.mult, ALU.mult, [ur, gur, rsr], [ur])
                self.tt(xt[:, c, :], xt[:, c, :], ut[:, c, :], ALU.add, [xr, ur], [xr], eng="pool")
                if gain_n is not None:
                    sq, sr = sqs[c % 4]
                    self.act(sq[:], xt[:, c, :], AF.Square, [xr], [sr])
                    self.mm(ps2[:], self.ones_b[:], sq[:], c == 0, c == C - 1, [sr, self.r_ones_b], [pr2])
                if bdst is not None:
                    self.cp("act", ht[:, c, :], xt[:, c, :], [xr], [hr])
            self.dma("sp", xdv[:, :, sl], xt[:], [xr], [])
            if gain_n is not None:
                self.rstd(rs, rsr, ps2, pr2, 128, 1.0 / (C * 128), tmp, tmr)
                for c in range(C):
                    self.stt(ht[:, c, :], xt[:, c, :], gn[:, c:c + 1], rs[:], ALU.mult, ALU.mult, [xr, gnr, rsr], [hr])
                self.dma("sp", ndst.rearrange("(c p) s -> p c s", p=128)[:, :, sl], ht[:], [hr], [])
            if bdst is not None:
                self.dma("sp", bdst.rearrange("(c p) s -> p c s", p=128)[:, :, sl], ht[:], [hr], [])
        self.flush()
        self.release(m)

    def load_x(self, src, KC, dt=BF16):
        xt, xr = self.sb([128, KC, S_TOK], dt)
        sv = src.rearrange("(c p) s -> p c s", p=128)
        step = max(1, KC // 4)
        for c0 in range(0, KC, step):
            c1 = min(KC, c0 + step)
            self.dma("sp", xt[:, c0:c1, :], sv[:, c0:c1, :], [], [xr])
        return xt, xr

    def make_wbufs(self, KCs, n=3):
        return [[self.sb([128, kc, 128], BF16) for _ in range(n)] for kc in KCs]

    def multilinear(self, srcs, ncols_total, epi, wbufs, tbs=(0, 1, 2, 3), tw=512):
        wi = getattr(self, "_wi", 0)
        chunks = [(n0, min(128, ncols_total - n0)) for n0 in range(0, ncols_total, 128)]

        def issue(ci, wi_):
            n0, n_ = chunks[ci]
            wts = []
            for si, (xt, xres, KC, w) in enumerate(srcs):
                wt, wres = wbufs[si][wi_ % len(wbufs[si])]
                wv = w.rearrange("(c p) n -> p c n", p=128)
                self.dma("pool", wt[:, :KC, :n_], wv[:, :, n0:n0 + n_], [], [wres])
                wts.append((wt, wres))
            return wts
        nxt = issue(0, wi)
        for ci, (n0, n_) in enumerate(chunks):
            wts = nxt
            wi += 1
            if ci + 1 < len(chunks):
                nxt = issue(ci + 1, wi)
            for tb in tbs:
                pss = []
                for si, (xt, xres, KC, w) in enumerate(srcs):
                    ps, pres = self.bank()
                    wt, wres = wts[si]
                    for kc in range(KC):
                        self.mm(ps[:n_, :tw], wt[:, kc, :n_], xt[:, kc, tb * tw:(tb + 1) * tw],
                                kc == 0, kc == KC - 1, [wres, xres], [pres])
                    pss.append((ps, pres))
                epi(pss, n0, n_, tb)
        self._wi = wi

    def epi_store(self, dst, dt, func=None, scale=None, stg=None, row0=0):
        k = [0]

        def epi(pss, n0, n_, tb):
            ps, pres = pss[0]
            st, sr = stg[k[0] % len(stg)]
            k[0] += 1
            sl = slice(tb * 512, (tb + 1) * 512)
            if func is None and (k[0] % 2 == 0):
                if scale is None:
                    self.cp("dve", st[:n_, :], ps[:n_, :], [pres], [sr])
                else:
                    self.ts(st[:n_, :], ps[:n_, :], scale, ALU.mult, [pres], [sr])
            else:
                self.act(st[:n_, :], ps[:n_, :], func or AF.Copy, [pres], [sr], scale=scale)
            self.dma("sp", dst[row0 + n0:row0 + n0 + n_, sl], st[:n_, :], [sr], [])
        return epi

    def ph_inproj(self, L, hT, W):
        D, C = self.D, self.C
        m = self.mark()
        self.rot = [0, 1, 2, 3, 4, 5, 6, 7]
        xt, xr = self.load_x(hT, C)
        wb = self.make_wbufs([C], 3)
        stf = [self.sb([128, 512], F32) for _ in range(3)]
        stb = [self.sb([128, 512], BF16) for _ in range(3)]
        w_in = W["w_in"][L]
        d = self.dr

        def grp(gi):
            return w_in[:, IN_OFF[gi]:IN_OFF[gi + 1]]
        srcs = lambda gi: [(xt, xr, C, grp(gi))]
        self.multilinear(srcs(0), 1024, self.epi_store(d["mqT"], BF16, scale=128 ** -0.5, stg=stb), wb)
        self.multilinear(srcs(1), 1024, self.epi_store(d["mkT"], BF16, stg=stb), wb)
        self.multilinear([(xt, xr, C, w_in[:, IN_OFF[3]:IN_OFF[6]])], 6144, self.epi_store(d["gqkvT"], F32, stg=stf), wb)
        self.multilinear([(xt, xr, C, w_in[:, IN_OFF[6]:IN_OFF[8]])], 32, self.epi_store(d["gabT"], F32, stg=stf), wb)
        self.multilinear(srcs(8), 2048, self.epi_store(d["gzT"], BF16, func=AF.Silu, stg=stb), wb)
        self.multilinear(srcs(9), 768, self.epi_store(d["cqT"], F32, stg=stf), wb)
        self.multilinear(srcs(10), 512, self.epi_store(d["ckvT"], F32, stg=stf), wb)
        self.multilinear(srcs(11), 64, self.epi_store(d["krT"], F32, stg=stf), wb)
        o = IN_OFF[11]
        self.multilinear([(xt, xr, C, w_in[:, o + 32:o + 64])], 32, self.epi_store(d["krsT"], F32, stg=stf, row0=0), wb)
        self.multilinear([(xt, xr, C, w_in[:, o:o + 32])], 32, self.epi_store(d["krsT"], F32, stg=stf, row0=32), wb)
        self.multilinear([(xt, xr, C, W["w_branch_gate"][L])], 3 * D, self.epi_store(d["gateT"], BF16, func=AF.Sigmoid, stg=stb), wb)
        wt, wtr = self.sb([128, C, 512], BF16)
        wv = w_in[:, IN_OFF[2]:IN_OFF[3]].rearrange("(c p) n -> p c n", p=128)
        k = 0
        for n0 in range(0, 1024, 512):
            self.dma("pool", wt[:], wv[:, :, n0:n0 + 512], [], [wtr])
            for t in range(16):
                ps, pres = self.bank()
                for kc in range(C):
                    self.mm(ps[:], xt[:, kc, t * 128:(t + 1) * 128], wt[:, kc, :], kc == 0, kc == C - 1, [xr, wtr], [pres])
                st, sr = stb[k % 3]
                k += 1
                self.cp("act" if k % 2 else "dve", st[:], ps[:], [pres], [sr])
                self.dma("sp", d["mv"][t * 128:(t + 1) * 128, n0:n0 + 512], st[:], [sr], [])
        self.flush()
        self.rot = [0, 1, 2, 3]
        self.release(m)

    def attn_core(self, qk_parts, extras, V, Vr, masks, mr, dst_rows, pts, ystg):
        pk = 0
        for j in range(4):
            num, numr = self.fixed(4 + 2 * (j % 2))
            den, denr = self.fixed(5 + 2 * (j % 2))
            nk = 4 * j + 4
            qs = slice(j * 512, (j + 1) * 512)
            for i in range(nk):
                ks = slice(i * 128, (i + 1) * 128)
                ps, pres = self.bank()
                lst = [(kT[:, ks], qT[:, qs], [kr, qr]) for (kT, kr, qT, qr) in qk_parts]
                lst += extras(i, j)
                if i >= 4 * j:
                    lst.append((self.ident_b[:], masks[:, i - 4 * j, :], [self.r_ident_b, mr]))
                for idx, (l, r, R) in enumerate(lst):
                    self.mm(ps[:], l, r, idx == 0, idx == len(lst) - 1, R, [pres])
                pt, ptr = pts[pk % len(pts)]
                pk += 1
                self.act(pt[:], ps[:], AF.Exp, [pres], [ptr])
                self.mm(num[:], V[:, i, :], pt[:], i == 0, i == nk - 1, [Vr, ptr], [numr])
                self.mm(den[:], self.ones_b[:], pt[:], i == 0, i == nk - 1, [self.r_ones_b, ptr], [denr])
            rd, rdr = ystg[0]
            yb, ybr = ystg[1 + (j % 2)]
            self.recip(rd[:], den[:], [denr], [rdr])
            self.tt(yb[:], num[:], rd[:], ALU.mult, [numr, rdr], [ybr])
            self.dma("sp", dst_rows[:, qs], yb[:], [ybr], [])

    def ph_moba(self, CN, pre=None):
        d = self.dr
        m = self.mark()
        masks, mr = self.sb([128, 4, 512], BF16)
        self.dma("pool", masks[:], CN["causal"].rearrange("m p q -> p m q"), [], [mr])
        pastneg, pnr = self.sb([128, 16, 8], F32)
        notown, nor = self.sb([128, 16, 8], F32)
        self.dma("sp", pastneg[:], CN["pastneg"], [], [pnr])
        self.dma("sp", notown[:], CN["notown"], [], [nor])
        E, Er = self.sb([8, 8, 128], BF16)
        self.dma("pool", E[:], CN["E"], [], [Er])
        QB, QBr = self.sb([4, S_TOK], BF16)
        self.dma("sp", QB[:], d["QBd"], [], [QBr])
        def alloc_stream():
            pts = [self.sb([128, 512], BF16) for _ in range(2)]
            ystg = [self.sb([128, 512], F32)] + [self.sb([128, 512], BF16) for _ in range(2)]
            hb = []
            for _ in range(1 if pre is not None else 2):
                hb.append(dict(q=self.sb([128, S_TOK], BF16), k=self.sb([128, S_TOK], BF16),
                               v=self.sb([128, 16, 128], BF16), kb=self.sb([4, S_TOK], BF16),
                               km=self.sb([128, 8], F32), kmb=self.sb([128, 8], BF16),
                               gm=self.sb([128, 16, 8], F32), m8=self.sb([128, 16, 8], F32),
                               thr=self.sb([128, 16], F32), ns=self.sb([128, 16, 8], F32),
                               nsT=self.sb([8, S_TOK], BF16)))
            return pts, ystg, hb
        SB_ = [alloc_stream() for _ in range(2)]
        if pre is not None:
            pre_alloc = pre[0]()
        self.S.begin_streams(3 if pre is not None else 2)
        for si in range(2):
          pts, ystg, hb = SB_[si]
          b0 = 3 * si
          self.use_stream(si, [b0], {4: b0 + 1, 5: b0 + 2, 6: b0 + 1, 7: b0 + 2})
          for h in range(4 * si, 4 * si + 4):
            B = hb[h % len(hb)]
            (q, qr), (k, kr), (v, vr), (kb, kbr) = B["q"], B["k"], B["v"], B["kb"]
            rows = slice(h * 128, (h + 1) * 128)
            self.dma("sp", q[:], d["mqT"][rows, :], [], [qr])
            self.dma("sp", k[:], d["mkT"][rows, :], [], [kr])
            self.dma("sp", v[:], d["mv"].rearrange("(t p) c -> p t c", p=128)[:, :, rows], [], [vr])
            self.dma("sp", kb[:], d["KBd"][h], [], [kbr])
            km, kmr = B["km"]
            kmb, kmbr = B["kmb"]
            self.S.op("dve", lambda e, km=km, k=k: e.tensor_reduce(out=km[:], in_=k[:].rearrange("p (n b) -> p n b", b=256), axis=AX.X, op=ALU.add), [kr], [kmr])
            self.ts(kmb[:], km[:], 1.0 / 256, ALU.mult, [kmr], [kmbr])
            gps, gpr = self.bank()
            for t in range(16):
                self.mm(gps[:, t * 8:(t + 1) * 8], q[:, t * 128:(t + 1) * 128], kmb[:], True, True, [qr, kmbr], [gpr])
            gm, gmr = B["gm"]
            m8, m8r = B["m8"]
            thr, thrr = B["thr"]
            ns, nsr = B["ns"]
            self.tt(gm[:], gps[:, 0:128].rearrange("p (t n) -> p t n", n=8), pastneg[:], ALU.add, [gpr, pnr], [gmr])
            for t in range(16):
                self.S.op("dve", lambda e, m8=m8, gm=gm, t=t: e.max(m8[:, t, :], gm[:, t, :]), [gmr], [m8r])
            self.ts(thr[:], m8[:, :, 2], -1e29, ALU.max, [m8r], [thrr])
            self.tt(ns[:], gm[:], thr[:].unsqueeze(2).broadcast_to([128, 16, 8]), ALU.is_lt, [gmr, thrr], [nsr])
            self.stt(ns[:], ns[:], NEG, notown[:], ALU.mult, ALU.mult, [nsr, nor], [nsr])
            nsT, nsTr = B["nsT"]
            for g4 in range(4):
                tp, tpr = self.bank()
                for tt_ in range(4):
                    t = g4 * 4 + tt_
                    self.tr(tp[0:8, tt_ * 128:(tt_ + 1) * 128], ns[:, t, :], self.ident_f[:], [nsr, self.r_ident_f], [tpr])
                self.cp("act", nsT[:, g4 * 512:(g4 + 1) * 512], tp[0:8, :], [tpr], [nsTr])

            def extras(i, j, kb=kb, kbr=kbr, nsT=nsT, nsTr=nsTr):
                ks = slice(i * 128, (i + 1) * 128)
                qs = slice(j * 512, (j + 1) * 512)
                return [(kb[:, ks], QB[:, qs], [kbr, QBr]),
                        (E[:, i // 2, :], nsT[:, qs], [Er, nsTr])]
            self.attn_core([(k, kr, q, qr)], extras, v, vr, masks, mr, d["yT"][rows, :], pts, ystg)
        if pre is not None:
            self.use_stream(2, [6, 7], {})
            pre[1](pre_alloc)
        self.end_streams()
        self.flush()
        self.release(m)

    def ph_mla(self, L, W, CN):
        d = self.dr
        m = self.mark()
        sc = 192 ** -0.5
        masks, mr = self.sb([128, 4, 512], BF16)
        self.dma("pool", masks[:], CN["causal"].rearrange("m p q -> p m q"), [], [mr])
        cqn, cqnr = self.sb([128, 6, S_TOK], BF16)
        ckn, cknr = self.sb([128, 4, S_TOK], BF16)
        wuq, wuqr = self.sb([128, 6, 1536], BF16)
        wukv, wukvr = self.sb([128, 4, 2048], BF16)
        wsw, wswr = self.sb([128, 6, 512], BF16)
        self.dma("pool", wuq[:], W["mla_w_uq"][L].rearrange("(c p) n -> p c n", p=128), [], [wuqr])
        self.dma("pool", wukv[:], W["mla_w_ukv"][L].rearrange("(c p) n -> p c n", p=128), [], [wukvr])
        wq4 = W["mla_w_uq"][L].rearrange("(c p) (h e) -> p c h e", p=128, e=192)
        wsw4 = wsw[:].rearrange("p c (h e) -> p c h e", e=64)
        for c in range(6):
            self.dma("pool", wsw4[:, c, :, 0:32], wq4[:, c, :, 160:192], [], [wswr])
            self.dma("pool", wsw4[:, c, :, 32:64], wq4[:, c, :, 128:160], [], [wswr])
        rope, roper = self.sb([64, 4, S_TOK], F32)
        self.dma("sp", rope[:], d["ropeT"].rearrange("f p s -> p f s"), [], [roper])
        kpe, kper = self.sb([64, S_TOK], BF16)
        tmp, tmr = self.sb([128, 512], F32)
        rs, rsr = self.sb([128, 512], F32)
        m2 = self.mark()
        sqs = [self.sb([128, 512], BF16) for _ in range(3)]
        for (src, Cn, gname, dstt, dr_) in ((d["cqT"], 6, "mla_q_norm_w", cqn, cqnr), (d["ckvT"], 4, "mla_kv_norm_w", ckn, cknr)):
            g, gr = self.sb([128, Cn], F32)
            self.dma("sp", g[:], W[gname][L], [], [gr])
            xt, xr = self.sb([128, Cn, 512], F32)
            sv = src.rearrange("(c p) s -> p c s", p=128)
            for tb in range(4):
                sl = slice(tb * 512, (tb + 1) * 512)
                self.dma("sp", xt[:], sv[:, :, sl], [], [xr])
                ps, pr = self.bank()
                for c in range(Cn):
                    sq, sr = sqs[c % 3]
                    self.act(sq[:], xt[:, c, :], AF.Square, [xr], [sr])
                    self.mm(ps[:], self.ones_b[:], sq[:], c == 0, c == Cn - 1, [sr, self.r_ones_b], [pr])
                self.rstd(rs, rsr, ps, pr, 128, 1.0 / (Cn * 128), tmp, tmr)
                for c in range(Cn):
                    self.stt(dstt[:, c, sl], xt[:, c, :], g[:, c:c + 1], rs[:], ALU.mult, ALU.mult, [xr, gr, rsr], [dr_])
        kr_, krr = self.sb([64, S_TOK], F32)
        krs, krsr = self.sb([64, S_TOK], F32)
        self.dma("sp", kr_[:], d["krT"], [], [krr])
        self.dma("sp", krs[:], d["krsT"], [], [krsr])
        self.tt(kr_[:], kr_[:], rope[:, 0, :], ALU.mult, [krr, roper], [krr])
        self.tt(krs[:], krs[:], rope[:, 1, :], ALU.mult, [krsr, roper], [krsr])
        self.tt(kpe[:], kr_[:], krs[:], ALU.add, [krr, krsr], [kper])
        self.flush()
        self.release(m2)
        def alloc_stream():
            pts = [self.sb([128, 512], BF16) for _ in range(2)]
            ystg = [self.sb([128, 512], F32)] + [self.sb([128, 512], BF16) for _ in range(2)]
            t1 = self.sb([64, 512], F32)
            t2 = self.sb([64, 512], F32)
            hb = [dict(qn=self.sb([128, S_TOK], BF16), qpe=self.sb([64, S_TOK], BF16),
                       kn=self.sb([128, S_TOK], BF16), v=self.sb([128, 16, 128], BF16)) for _ in range(1)]
            return pts, ystg, t1, t2, hb
        SB_ = [alloc_stream() for _ in range(2)]
        self.S.begin_streams(2)
        for si in range(2):
          pts, ystg, (t1, t1r), (t2, t2r), hb = SB_[si]
          b0 = 4 * si
          self.use_stream(si, [b0, b0 + 1], {4: b0 + 2, 5: b0 + 3, 6: b0 + 2, 7: b0 + 3})
          for h in range(4 * si, 4 * si + 4):
            B = hb[0]
            (qn, qnr), (qpe, qper), (kn, knr), (v, vr) = B["qn"], B["qpe"], B["kn"], B["v"]
            for tb in range(4):
                sl = slice(tb * 512, (tb + 1) * 512)
                ps, pr = self.bank()
                for c in range(6):
                    self.mm(ps[:], wuq[:, c, h * 192:h * 192 + 128], cqn[:, c, sl], c == 0, c == 5, [wuqr, cqnr], [pr])
                self.act(qn[:, sl], ps[:], AF.Copy, [pr], [qnr], scale=sc)
                ps, pr = self.bank()
                for c in range(4):
                    self.mm(ps[:], wukv[:, c, h * 256:h * 256 + 128], ckn[:, c, sl], c == 0, c == 3, [wukvr, cknr], [pr])
                self.cp("dve", kn[:, sl], ps[:], [pr], [knr])
                ps, pr = self.bank()
                for c in range(6):
                    self.mm(ps[0:64, :], wuq[:, c, h * 192 + 128:h * 192 + 192], cqn[:, c, sl], c == 0, c == 5, [wuqr, cqnr], [pr])
                ps2, pr2 = self.bank()
                for c in range(6):
                    self.mm(ps2[0:64, :], wsw[:, c, h * 64:(h + 1) * 64], cqn[:, c, sl], c == 0, c == 5, [wswr, cqnr], [pr2])
                self.tt(t1[:], ps[0:64, :], rope[:, 2, sl], ALU.mult, [pr, roper], [t1r])
                self.tt(t2[:], ps2[0:64, :], rope[:, 3, sl], ALU.mult, [pr2, roper], [t2r])
                self.tt(qpe[:, sl], t1[:], t2[:], ALU.add, [t1r, t2r], [qper])
            for t in range(16):
                ps, pr = self.bank()
                for c in range(4):
                    self.mm(ps[:, 0:128], ckn[:, c, t * 128:(t + 1) * 128], wukv[:, c, h * 256 + 128:h * 256 + 256], c == 0, c == 3, [cknr, wukvr], [pr])
                self.cp("act" if t % 2 else "dve", v[:, t, :], ps[:, 0:128], [pr], [vr])
            self.attn_core([(kn, knr, qn, qnr), (kpe, kper, qpe, qper)], lambda i, j: [], v, vr, masks, mr,
                           d["yT"][3072 + h * 128:3072 + (h + 1) * 128, :], pts, ystg)
        self.end_streams()
        self.flush()
        self.release(m)

    def ph_posconst(self, pos, CN):
        d = self.dr
        m = self.mark()
        pi_, pir = self.sb([64, S_TOK], I32)
        self.dma("sp", pi_[:], pos.broadcast_to([64, S_TOK]), [], [pir])
        pf, pfr = self.sb([64, S_TOK], F32)
        self.cp("dve", pf[:], pi_[:], [pir], [pfr])
        cols, colr = self.sb([64, 2], F32)
        self.dma("sp", cols[:], CN["ropecols"], [], [colr])
        a, ar = self.sb([64, S_TOK], F32)
        k_, kr = self.sb([64, S_TOK], F32)
        r_, rr = self.sb([64, S_TOK], F32)
        o, orr = self.sb([64, 4, S_TOK], F32)
        MAG = 12582912.0
        self.ts(a[:], pf[:], cols[:, 0:1], ALU.mult, [pfr, colr], [ar])
        self.ts(k_[:], a[:], 1.0 / (2 * math.pi), ALU.mult, [ar], [kr], s2=MAG, op1=ALU.add)
        self.ts(k_[:], k_[:], -MAG, ALU.add, [kr], [kr])
        C1 = 6.28125
        C2 = 2 * math.pi - C1
        self.stt(r_[:], k_[:], -C1, a[:], ALU.mult, ALU.add, [kr, ar], [rr])
        self.stt(r_[:], k_[:], -C2, r_[:], ALU.mult, ALU.add, [kr, rr], [rr])
        PI_ = 3.1415925
        self.ts(r_[:], r_[:], PI_, ALU.min, [rr], [rr], s2=-PI_, op1=ALU.max)
        self.act(o[:, 1, :], r_[:], AF.Sin, [rr], [orr])
        self.ts(a[:], r_[:], -1.0, ALU.mult, [rr], [ar])
        self.tt(a[:], a[:], r_[:], ALU.min, [ar, rr], [ar])
        self.ts(a[:], a[:], math.pi / 2, ALU.add, [ar], [ar], s2=1.5707963, op1=ALU.min)
        self.act(o[:, 0, :], a[:], AF.Sin, [ar], [orr])
        self.ts(o[:, 1, :], o[:, 1, :], cols[:, 1:2], ALU.mult, [orr, colr], [orr])
        sc = 192 ** -0.5
        self.ts(o[:, 2, :], o[:, 0, :], sc, ALU.mult, [orr], [orr])
        self.ts(o[:, 3, :], o[:, 1, :], sc, ALU.mult, [orr], [orr])
        self.dma("sp", d["ropeT"].rearrange("f p s -> p f s"), o[:], [orr], [])
        hf, hfr = self.sb([1, S_TOK], F32)
        lf, lfr = self.sb([1, S_TOK], F32)
        self.ts(hf[:], pf[0:1, :], 1.0 / 128, ALU.mult, [pfr], [hfr], s2=-127.0 / 256, op1=ALU.add)
        self.ts(hf[:], hf[:], MAG, ALU.add, [hfr], [hfr])
        self.ts(hf[:], hf[:], -MAG, ALU.add, [hfr], [hfr])
        self.stt(lf[:], hf[:], -128.0, pf[0:1, :], ALU.mult, ALU.add, [hfr, pfr], [lfr])
        rows = [self.sb([1, S_TOK], BF16) for _ in range(4)]
        rb, rbr = rows[0]
        self.cp("dve", rb[:], hf[:], [hfr], [rbr])
        self.dma("sp", d["QBd"][0:1, :], rb[:], [rbr], [])
        rb, rbr = rows[1]
        self.cp("dve", rb[:], lf[:], [lfr], [rbr])
        self.dma("sp", d["QBd"][1:2, :], rb[:], [rbr], [])
        rb, rbr = rows[2]
        self.memset("dve", rb[:], 1.0, [rbr])
        self.dma("sp", d["QBd"][2:3, :], rb[:], [rbr], [])
        self.dma("sp", d["QBd"][3:4, :], rb[:], [rbr], [])
        k = 0
        for h in range(8):
            s = 2.0 ** -(h + 1)
            for ri, (src, sr_, val) in enumerate(((None, None, -128 * s), (None, None, -s), (hf, hfr, 128 * s), (lf, lfr, s))):
                rb, rbr = rows[k % 4]
                k += 1
                if src is None:
                    self.memset("dve", rb[:], val, [rbr])
                else:
                    self.ts(rb[:], src[:], val, ALU.mult, [sr_], [rbr])
                self.dma("sp", d["KBd"][h, ri:ri + 1, :], rb[:], [rbr], [])
        self.flush()
        self.release(m)

    def gdn_pre_alloc(self):
        A = {}
        A["cw"] = self.sb([128, 48, 4], F32)
        A["xps"] = [self.sb([128, S_TOK + 3], F32) for _ in range(2)]
        A["accs"] = [self.sb([128, S_TOK], F32) for _ in range(2)]
        A["sqs"] = [self.sb([128, 512], BF16) for _ in range(2)]
        A["lns"] = [self.sb([128, 512], F32) for _ in range(2)]
        A["rss"] = [self.sb([128, 512], F32) for _ in range(2)]
        A["a"] = self.sb([16, S_TOK], F32)
        A["b"] = self.sb([16, S_TOK], F32)
        A["hc"] = self.sb([16, 2], F32)
        A["cm"] = self.sb([16, S_TOK], F32)
        A["G"] = self.sb([16, 6, S_TOK], F32)
        A["x"] = self.sb([16, S_TOK], F32)
        A["y"] = self.sb([16, S_TOK], F32)
        A["nA"] = self.sb([16, 1], F32)
        return A

    def gdn_pre_emit(self, L, W, CN, A):
        d = self.dr
        cw, cwr = A["cw"]
        self.dma("sp", cw[:], W["gdn_conv_w"][L], [], [cwr])
        xps, accs, sqs, lns, rss = A["xps"], A["accs"], A["sqs"], A["lns"], A["rss"]
        for (xp, xpr) in xps:
            self.memset("dve", xp[:, 0:3], 0.0, [xpr])
        kk = 0
        for c in range(48):
            xp, xpr = xps[c % 2]
            acc, accr = accs[c % 2]
            self.dma("sp", xp[:, 3:], d["gqkvT"][c * 128:(c + 1) * 128, :], [], [xpr])
            self.ts(acc[:], xp[:, 0:S_TOK], cw[:, c, 0:1], ALU.mult, [xpr, cwr], [accr])
            for j in range(1, 4):
                self.stt(acc[:], xp[:, j:j + S_TOK], cw[:, c, j:j + 1], acc[:], ALU.mult, ALU.add, [xpr, cwr, accr], [accr])
            self.act(acc[:], acc[:], AF.Silu, [accr], [accr])
            if c < 32:
                for tb in range(4):
                    sl = slice(tb * 512, (tb + 1) * 512)
                    sq, sr = sqs[kk % 2]
                    ln, lnr = lns[kk % 2]
                    rs, rsr = rss[kk % 2]
                    kk += 1
                    ps, pr = self.bank()
                    self.act(sq[:], acc[:, sl], AF.Square, [accr], [sr])
                    self.mm(ps[:], self.ones_b[:], sq[:], True, True, [sr, self.r_ones_b], [pr])
                    self.act(ln[:], ps[:], AF.Ln, [pr, self.r_cb], [lnr], bias=self.cb[:, 0:1])
                    self.act(rs[:], ln[:], AF.Exp, [lnr], [rsr], scale=-0.5)
                    if c < 16:
                        self.stt(acc[:, sl], acc[:, sl], 128 ** -0.5, rs[:], ALU.mult, ALU.mult, [accr, rsr], [accr])
                    else:
                        self.tt(acc[:, sl], acc[:, sl], rs[:], ALU.mult, [accr, rsr], [accr])
            self.dma("act", d["gcT"][c * 128:(c + 1) * 128, :], acc[:], [accr], [])
        (a_, ar), (b_, br), (hc, hcr), (cm, cmr), (G, Gr), (x_, xr), (y_, yr), (nA, nAr) = \
            A["a"], A["b"], A["hc"], A["cm"], A["G"], A["x"], A["y"], A["nA"]
        self.dma("sp", a_[:], d["gabT"][0:16, :], [], [ar])
        self.dma("sp", b_[:], d["gabT"][16:32, :], [], [br])
        self.dma("sp", hc[:], W["gdn_hcols"][L], [], [hcr])
        self.dma("sp", cm[:], CN["cmask"], [], [cmr])
        self.act(nA[:], hc[:, 0:1], AF.Exp, [hcr], [nAr])
        self.ts(nA[:], nA[:], -1.0, ALU.mult, [nAr], [nAr])
        self.ts(x_[:], a_[:], hc[:, 1:2], ALU.add, [ar, hcr], [xr])
        self.ts(y_[:], x_[:], -1.0, ALU.mult, [xr], [yr])
        self.tt(y_[:], y_[:], x_[:], ALU.max, [yr, xr], [yr])
        self.act(y_[:], y_[:], AF.Exp, [yr], [yr], scale=-1.0)
        self.act(y_[:], y_[:], AF.Ln, [yr, self.r_cb], [yr], bias=self.cb[0:16, 1:2])
        self.ts(x_[:], x_[:], 0.0, ALU.max, [xr], [xr])
        self.tt(x_[:], x_[:], y_[:], ALU.add, [xr, yr], [xr])
        self.ts(x_[:], x_[:], nA[:, 0:1], ALU.mult, [xr, nAr], [xr])
        self.S.op("dve", lambda e: e.tensor_tensor_scan(out=G[:, 0, :], data0=cm[:], data1=x_[:], initial=0.0, op0=ALU.mult, op1=ALU.add), [cmr, xr], [Gr])
        self.act(G[:, 1, :], b_[:], AF.Sigmoid, [br], [Gr])
        self.act(G[:, 2, :], G[:, 0, :], AF.Exp, [Gr], [Gr])
        self.tt(G[:, 3, :], G[:, 1, :], G[:, 2, :], ALU.mult, [Gr], [Gr])
        gc3 = G[:, 0, :].rearrange("p (n l) -> p n l", l=64)
        self.tt(y_[:].rearrange("p (n l) -> p n l", l=64), gc3[:, :, 63:64].broadcast_to([16, 32, 64]), gc3, ALU.subtract, [Gr], [yr])
        self.act(G[:, 4, :], y_[:], AF.Exp, [yr], [Gr])
        self.ts(G[:, 5, :], G[:, 0, :], -1.0, ALU.mult, [Gr], [Gr])
        self.dma("sp", d["gG"].rearrange("f h s -> h f s"), G[:], [Gr], [])

    def ph_gdn_pre(self, L, W, CN):
        m = self.mark()
        A = self.gdn_pre_alloc()
        self.gdn_pre_emit(L, W, CN, A)
        self.flush()
        self.release(m)

    def ph_gdn(self, L, W, CN):
        d = self.dr
        m = self.mark()
        G, Gr = self.sb([16, 6, S_TOK], F32)
        self.dma("sp", G[:], d["gG"].rearrange("f h s -> h f s"), [], [Gr])
        oh, ohr = self.sb([16, 16, 128], F32)
        self.dma("sp", oh[:], CN["onehot16"], [], [ohr])
        negU, negUr = self.sb([128, 4, 128], F32)
        strU, strUr = self.sb([128, 4, 128], F32)
        id8, id8r = self.sb([128, 4, 128], F32)
        self.dma("sp", negU[:], CN["negU"], [], [negUr])
        self.dma("sp", strU[:], CN["strU"], [], [strUr])
        self.dma("sp", id8[:], CN["id8"], [], [id8r])
        nw, nwr = self.sb([128, 1], F32)
        self.dma("sp", nw[:], W["gdn_norm_w"][L], [], [nwr])
        ngT, ngTr = self.sb([128, 16, 16], F32)
        for g8 in range(2):
            tp, tpr = self.bank()
            for cc in range(8):
                c = g8 * 8 + cc
                self.tr(tp[:, cc * 16:(cc + 1) * 16], G[:, 5, c * 128:(c + 1) * 128], self.ident_f[0:16, 0:16], [Gr, self.r_ident_f], [tpr])
            self.cp("dve", ngT[:, g8 * 8:(g8 + 1) * 8, :].rearrange("p c h -> p (c h)"), tp[:, 0:128], [tpr], [ngTr])

        def alloc_head():
            def T3():
                return self.sb([128, 4, 128], F32)
            ins = [dict(q=self.sb([128, 512], F32), k=self.sb([128, 512], F32), v=self.sb([128, 512], F32),
                        z=self.sb([128, 512], BF16)) for _ in range(2)]
            kbgT, kbgTr = self.sb([128, 512], F32)
            ktlT, ktlTr = self.sb([128, 512], F32)
            vbT, vbTr = self.sb([128, 512], F32)
            qd, qdr = self.sb([128, 512], F32)
            glc, glcr = self.sb([128, 8], F32)
            tdt, tdtr = T3()
            DT, DTr = T3()
            DTs, DTsr = T3()
            Bm, Bmr = T3()
            Am, Amr = T3()
            B2, B2r = T3()
            A2, A2r = T3()
            P, Pr = T3()
            Aqk, Aqkr = T3()
            kbg, kbgr = T3()
            ktl, ktlr = T3()
            vb, vbr = T3()
            u, ur = T3()
            wT, wTr = self.sb([128, 512], F32)
            vn, vnr = self.sb([128, 128], F32)
            St, Str = self.sb([128, 128], F32)
            oT, oTr = self.sb([128, 512], F32)
            sq, sqr = self.sb([128, 512], BF16)
            tmp, tmr = self.sb([128, 512], F32)
            rs, rsr = self.sb([128, 512], F32)
            ybs = [self.sb([128, 512], BF16) for _ in range(2)]
            return dict(locals())
        HBs = [alloc_head() for _ in range(2)]
        FX = self.fixed
        V4 = lambda ap: ap.rearrange("p (c l) -> p c l", l=128)

        def bcast(fi, h, sl, bank):
            ps, pr = bank
            self.mm(ps[:, :], oh[:, h, :], G[:, fi, sl], True, True, [ohr, Gr], [pr])
            return ps, pr

        def head(h, HB):
            ins = HB["ins"]
            ybs = HB["ybs"]
            kbgT = HB["kbgT"]
            kbgTr = HB["kbgTr"]
            ktlT = HB["ktlT"]
            ktlTr = HB["ktlTr"]
            vbT = HB["vbT"]
            vbTr = HB["vbTr"]
            qd = HB["qd"]
            qdr = HB["qdr"]
            glc = HB["glc"]
            glcr = HB["glcr"]
            tdt = HB["tdt"]
            tdtr = HB["tdtr"]
            DT = HB["DT"]
            DTr = HB["DTr"]
            DTs = HB["DTs"]
            DTsr = HB["DTsr"]
            Bm = HB["Bm"]
            Bmr = HB["Bmr"]
            Am = HB["Am"]
            Amr = HB["Amr"]
            B2 = HB["B2"]
            B2r = HB["B2r"]
            A2 = HB["A2"]
            A2r = HB["A2r"]
            P = HB["P"]
            Pr = HB["Pr"]
            Aqk = HB["Aqk"]
            Aqkr = HB["Aqkr"]
            kbg = HB["kbg"]
            kbgr = HB["kbgr"]
            ktl = HB["ktl"]
            ktlr = HB["ktlr"]
            vb = HB["vb"]
            vbr = HB["vbr"]
            u = HB["u"]
            ur = HB["ur"]
            wT = HB["wT"]
            wTr = HB["wTr"]
            vn = HB["vn"]
            vnr = HB["vnr"]
            St = HB["St"]
            Str = HB["Str"]
            oT = HB["oT"]
            oTr = HB["oTr"]
            sq = HB["sq"]
            sqr = HB["sqr"]
            tmp = HB["tmp"]
            tmr = HB["tmr"]
            rs = HB["rs"]
            rsr = HB["rsr"]
            self.memset("dve", St[:], 0.0, [Str])
            rows = slice(h * 128, (h + 1) * 128)
            for g in range(4):
                sl = slice(g * 512, (g + 1) * 512)
                I = ins[g % 2]
                yb, ybr = ybs[g % 2]
                (qT, qTr), (kT, kTr), (vT, vTr), (zT, zTr) = I["q"], I["k"], I["v"], I["z"]
                self.dma("sp", qT[:], d["gcT"][h * 128:(h + 1) * 128, sl], [], [qTr])
                self.dma("sp", kT[:], d["gcT"][2048 + h * 128:2048 + (h + 1) * 128, sl], [], [kTr])
                self.dma("sp", vT[:], d["gcT"][4096 + h * 128:4096 + (h + 1) * 128, sl], [], [vTr])
                self.dma("sp", zT[:], d["gzT"][rows, sl], [], [zTr])
                ps, pr = bcast(0, h, sl, FX(4))
                self.tt(tdt[:], V4(ps[:, :]), negU[:], ALU.add, [pr, negUr], [tdtr])
                for pp in range(4):
                    t_ = g * 4 + pp
                    self.act(DT[:, pp, :], tdt[:, pp, :], AF.Exp, [tdtr, ngTr], [DTr], bias=ngT[:, t_, h:h + 1])
                self.tt(DTs[:], DT[:], strU[:], ALU.mult, [DTr, strUr], [DTsr], eng="pool")
                ps, pr = bcast(3, h, sl, FX(5))
                self.tt(kbgT[:], kT[:], ps[:], ALU.mult, [kTr, pr], [kbgTr])
                ps, pr = bcast(4, h, sl, FX(6))
                self.tt(ktlT[:], kT[:], ps[:], ALU.mult, [kTr, pr], [ktlTr])
                psb, prb = bcast(1, h, sl, FX(7))
                self.tt(vbT[:], vT[:], psb[:], ALU.mult, [vTr, prb], [vbTr])
                pk, pkr = FX(5)
                pq, pqr = FX(6)
                for pp in range(4):
                    cs = slice(pp * 128, (pp + 1) * 128)
                    self.mm(pk[:, cs], kT[:, cs], kT[:, cs], True, True, [kTr], [pkr])
                    self.mm(pq[:, cs], kT[:, cs], qT[:, cs], True, True, [kTr, qTr], [pqr])
                self.tt(Bm[:], V4(pk[:, :]), DTs[:], ALU.mult, [pkr, DTsr], [Bmr])
                self.tt(Bm[:], Bm[:], V4(psb[:, :]), ALU.mult, [Bmr, prb], [Bmr])
                self.tt(Aqk[:], V4(pq[:, :]), DT[:], ALU.mult, [pqr, DTr], [Aqkr])
                pse, pre = bcast(2, h, sl, FX(4))
                self.tt(qd[:], qT[:], pse[:], ALU.mult, [qTr, pre], [qdr])
                self.cp("dve", glc[:], pse[:].rearrange("p (c l) -> p c l", l=64)[:, :, 63], [pre], [glcr])
                pa, par = FX(7)
                for pp in range(4):
                    self.tr(pa[:, pp * 128:(pp + 1) * 128], Bm[:, pp, :], self.ident_f[:], [Bmr, self.r_ident_f], [par])
                self.cp("act", Am[:], V4(pa[:, :]), [par], [Amr])
                self.tt(P[:], id8[:], Bm[:], ALU.subtract, [id8r, Bmr], [Pr], eng="pool")
                Ac, Acr, Bc, Bcr = Am, Amr, Bm, Bmr
                An, Anr, Bn, Bnr = A2, A2r, B2, B2r
                for lvl in range(5):
                    p1, p1r = FX(5)
                    p2, p2r = FX(6)
                    for pp in range(4):
                        cs = slice(pp * 128, (pp + 1) * 128)
                        self.mm(p1[:, cs], Bc[:, pp, :], Ac[:, pp, :], True, True, [Bcr, Acr], [p1r])
                    if lvl < 4:
                        for pp in range(4):
                            cs = slice(pp * 128, (pp + 1) * 128)
                            self.mm(p2[:, cs], Ac[:, pp, :], Bc[:, pp, :], True, True, [Bcr, Acr], [p2r])
                    self.cp("act", An[:], V4(p1[:, :]), [p1r], [Anr])
                    p3, p3r = FX(4 if lvl % 2 == 0 else 7)
                    for pp in range(4):
                        cs = slice(pp * 128, (pp + 1) * 128)
                        self.mm(p3[:, cs], An[:, pp, :], P[:, pp, :], True, True, [Anr, Pr], [p3r])
                    if lvl < 4:
                        self.cp("act", Bn[:], V4(p2[:, :]), [p2r], [Bnr])
                    self.tt(P[:], P[:], V4(p3[:, :]), ALU.add, [Pr, p3r], [Pr])
                    Ac, Acr, Bc, Bcr, An, Anr, Bn, Bnr = An, Anr, Bn, Bnr, Ac, Acr, Bc, Bcr
                for qi, (src, sr_, dst, dr_) in enumerate(((kbgT, kbgTr, kbg, kbgr), (ktlT, ktlTr, ktl, ktlr), (vbT, vbTr, vb, vbr))):
                    tp, tpr = FX(4 + qi)
                    for pp in range(4):
                        cs = slice(pp * 128, (pp + 1) * 128)
                        self.tr(tp[:, cs], src[:, cs], self.ident_f[:], [sr_, self.r_ident_f], [tpr])
                    self.cp("act" if qi == 1 else "dve", dst[:], V4(tp[:, :]), [tpr], [dr_])
                pu, pur = FX(7)
                for pp in range(4):
                    self.mm(pu[:, pp * 128:(pp + 1) * 128], P[:, pp, :], vb[:, pp, :], True, True, [Pr, vbr], [pur])
                self.cp("dve", u[:], V4(pu[:, :]), [pur], [ur])
                pw, pwr = FX(4)
                for pp in range(4):
                    self.mm(pw[:, pp * 128:(pp + 1) * 128], kbg[:, pp, :], P[:, pp, :], True, True, [kbgr, Pr], [pwr])
                self.cp("act", wT[:], pw[:, :], [pwr], [wTr])
                po, por = FX(5)
                for cc in range(8):
                    pp, hf = cc // 2, cc % 2
                    prt = slice(hf * 64, hf * 64 + 64)
                    cs = slice(cc * 64, (cc + 1) * 64)
                    p1, p1r = self.bank()
                    self.mm(p1[prt, 0:128], wT[:, cs], St[:], True, True, [wTr, Str], [p1r])
                    self.tt(vn[prt, :], u[prt, pp, :], p1[prt, 0:128], ALU.subtract, [ur, p1r], [vnr])
                    self.mm(po[:, cs], St[:], qd[:, cs], True, False, [Str, qdr], [por])
                    self.mm(po[:, cs], vn[prt, :], Aqk[prt, pp, hf * 64:hf * 64 + 64], False, True, [vnr, Aqkr], [por])
                    p2, p2r = self.bank()
                    self.mm(p2[:, 0:128], ktl[prt, pp, :], vn[prt, :], True, True, [ktlr, vnr], [p2r])
                    self.stt(St[:], St[:], glc[:, cc:cc + 1], p2[:, 0:128], ALU.mult, ALU.add, [Str, glcr, p2r], [Str])
                self.cp("dve", oT[:], po[:], [por], [oTr])
                self.act(sq[:], po[:], AF.Square, [por], [sqr])
                pn, pnr = FX(6)
                self.mm(pn[:], self.ones_b[:], sq[:], True, True, [sqr, self.r_ones_b], [pnr])
                self.rstd(rs, rsr, pn, pnr, 128, 1.0 / 128, tmp, tmr)
                self.stt(oT[:], oT[:], nw[:, 0:1], rs[:], ALU.mult, ALU.mult, [oTr, nwr, rsr], [oTr])
                self.tt(yb[:], oT[:], zT[:], ALU.mult, [oTr, zTr], [ybr], eng="pool")
                self.dma("act", d["yT"][1024 + h * 128:1024 + (h + 1) * 128, sl], yb[:], [ybr], [])

        for hp in range(8):
            self.S.begin_streams(2)
            for si in range(2):
                b0 = 4 * si
                self.use_stream(si, [b0 + 3, b0 + 2], {4: b0, 5: b0 + 1, 6: b0 + 2, 7: b0 + 3})
                head(hp + 8 * si, HBs[si])
            self.end_streams()
        self.flush()
        self.release(m)

    def ph_merge(self, L, W):
        d = self.dr
        D = self.D
        m = self.mark()
        self.rot = [0, 1, 2, 3, 4, 5]
        yt, yr = self.load_x(d["yT"], 32)
        wb = self.make_wbufs([8, 16, 8], 2)
        gts = [[self.sb([128, 512], BF16) for _ in range(3)] for _ in range(2)]
        t1s = [self.sb([128, 512], F32) for _ in range(2)]
        t2s = [self.sb([128, 512], F32) for _ in range(2)]
        mbs = [self.sb([128, 512], BF16) for _ in range(2)]
        k = [0]
        srcs = [(yt[:, 0:8, :], yr, 8, W["w_branch_a"][L]), (yt[:, 8:24, :], yr, 16, W["w_branch_b"][L]),
                (yt[:, 24:32, :], yr, 8, W["w_branch_c"][L])]

        def epi(pss, n0, n_, tb):
            i = k[0] % 2
            k[0] += 1
            sl = slice(tb * 512, (tb + 1) * 512)
            gs = gts[i]
            for b in range(3):
                self.dma("sp", gs[b][0][:n_, :], d["gateT"][b * D + n0:b * D + n0 + n_, sl], [], [gs[b][1]])
            t1, t1r = t1s[i]
            t2, t2r = t2s[i]
            mb, mbr = mbs[i]
            self.tt(t1[:n_, :], pss[0][0][:n_, :], gs[0][0][:n_, :], ALU.mult, [pss[0][1], gs[0][1]], [t1r])
            self.tt(t2[:n_, :], pss[1][0][:n_, :], gs[1][0][:n_, :], ALU.mult, [pss[1][1], gs[1][1]], [t2r])
            self.tt(t1[:n_, :], t1[:n_, :], t2[:n_, :], ALU.add, [t1r, t2r], [t1r])
            self.tt(t2[:n_, :], pss[2][0][:n_, :], gs[2][0][:n_, :], ALU.mult, [pss[2][1], gs[2][1]], [t2r])
            self.tt(mb[:n_, :], t1[:n_, :], t2[:n_, :], ALU.add, [t1r, t2r], [mbr])
            self.dma("act", d["mT"][n0:n0 + n_, sl], mb[:n_, :], [mbr], [])
        self.multilinear(srcs, D, epi, wb)
        self.flush()
        self.rot = [0, 1, 2, 3]
        self.release(m)

    def ph_linear_simple(self, src, KC, w, ncols, dst, dt):
        m = self.mark()
        self.rot = [0, 1, 2, 3, 4, 5, 6, 7]
        xt, xr = self.load_x(src, KC)
        wb = self.make_wbufs([KC], 3)
        stg = [self.sb([128, 512], dt) for _ in range(3)]
        self.multilinear([(xt, xr, KC, w)], ncols, self.epi_store(dst, dt, stg=stg), wb)
        self.flush()
        self.rot = [0, 1, 2, 3]
        self.release(m)

    def ph_ffn(self, L, W):
        d = self.dr
        D, DFF, C, CF = self.D, self.DFF, self.C, self.CF
        m = self.mark()
        self.rot = [0, 1, 2, 3, 4, 5, 6, 7]
        xt, xr = self.load_x(d["h2T"], C)
        wb = self.make_wbufs([C, C], 3)
        sgs = [self.sb([128, 512], F32) for _ in range(2)]
        hbs = [self.sb([128, 512], BF16) for _ in range(3)]
        k = [0]

        def epi(pss, n0, n_, tb):
            sl = slice(tb * 512, (tb + 1) * 512)
            sg, sgr = sgs[k[0] % 2]
            hb, hbr = hbs[k[0] % 3]
            k[0] += 1
            self.act(sg[:n_, :], pss[0][0][:n_, :], AF.Silu, [pss[0][1]], [sgr])
            self.tt(hb[:n_, :], pss[1][0][:n_, :], sg[:n_, :], ALU.mult, [pss[1][1], sgr], [hbr])
            self.dma("sp", d["hidT"][n0:n0 + n_, sl], hb[:n_, :], [hbr], [])
        self.multilinear([(xt, xr, C, W["w_ffn_gate"][L]), (xt, xr, C, W["w_ffn_up"][L])], DFF, epi, wb)
        self.flush()
        self.release(m)
        m = self.mark()
        KP = 16
        npiece = (CF + KP - 1) // KP
        wps = [self.sb([128, KP, 512], BF16) for _ in range(4)]
        stg = [self.sb([128, 512], F32) for _ in range(3)]
        ht, hr = self.sb([128, CF, 512], BF16)
        hv = d["hidT"].rearrange("(c p) s -> p c s", p=128)
        wv = W["w_ffn_down"][L].rearrange("(c p) n -> p c n", p=128)
        wi = 0
        kk = 0
        for tb in range(4):
            sl = slice(tb * 512, (tb + 1) * 512)
            step = max(1, CF // 4)
            for c0 in range(0, CF, step):
                c1 = min(CF, c0 + step)
                self.dma("sp", ht[:, c0:c1, :], hv[:, c0:c1, sl], [], [hr])
            for n0 in range(0, D, 512):
                ncol = min(512, D - n0)
                nn_ = ncol // 128
                pss = [self.bank() for _ in range(nn_)]
                for pi in range(npiece):
                    k0 = pi * KP
                    k1 = min(CF, k0 + KP)
                    wt, wr = wps[wi % 4]
                    wi += 1
                    self.dma("pool", wt[:, 0:k1 - k0, 0:ncol], wv[:, k0:k1, n0:n0 + ncol], [], [wr])
                    for kc in range(k0, k1):
                        for nn in range(nn_):
                            self.mm(pss[nn][0][:, :], wt[:, kc - k0, nn * 128:(nn + 1) * 128], ht[:, kc, :],
                                    kc == 0, kc == CF - 1, [wr, hr], [pss[nn][1]])
                for nn in range(nn_):
                    st, sr = stg[kk % 3]
                    kk += 1
                    self.cp("act" if kk % 2 else "dve", st[:, :], pss[nn][0][:, :], [pss[nn][1]], [sr])
                    self.dma("act", d["fT"][n0 + nn * 128:n0 + (nn + 1) * 128, sl], st[:, :], [sr], [])
        self.flush()
        self.rot = [0, 1, 2, 3]
        self.release(m)

    def ph_ple(self, L, W, pT, xsrc, xdst):
        d = self.dr
        D, C = self.D, self.C
        m = self.mark()
        self.rot = [0, 1, 2, 3, 4, 5, 6, 7]
        xt, xr = self.load_x(d["xbT"], C)
        pt, pr = self.sb([128, 2, S_TOK], BF16)
        self.dma("pool", pt[:], pT[L].rearrange("(c p) s -> p c s", p=128), [], [pr])
        wb = self.make_wbufs([C, 2], 3)
        sgs = [self.sb([128, 512], F32) for _ in range(2)]
        xs = [self.sb([128, 512], F32) for _ in range(3)]
        k = [0]

        def epi(pss, n0, n_, tb):
            sl = slice(tb * 512, (tb + 1) * 512)
            sg, sgr = sgs[k[0] % 2]
            xx, xxr = xs[k[0] % 3]
            k[0] += 1
            self.dma("sp", xx[:n_, :], xsrc[n0:n0 + n_, sl], [], [xxr])
            self.act(sg[:n_, :], pss[0][0][:n_, :], AF.Sigmoid, [pss[0][1]], [sgr])
            self.tt(sg[:n_, :], pss[1][0][:n_, :], sg[:n_, :], ALU.mult, [pss[1][1], sgr], [sgr])
            self.tt(xx[:n_, :], xx[:n_, :], sg[:n_, :], ALU.add, [xxr, sgr], [xxr])
            self.dma("act", xdst[n0:n0 + n_, sl], xx[:n_, :], [xxr], [])
        self.multilinear([(xt, xr, C, W["w_ple_gate"][L]), (pt, pr, 2, W["w_ple_proj"][L])], D, epi, wb)
        self.flush()
        self.rot = [0, 1, 2, 3]
        self.release(m)


WNAMES = ["w_in", "mla_w_uq", "mla_w_ukv", "w_branch_gate", "w_branch_a", "w_branch_b", "w_branch_c",
          "w_out", "w_ffn_gate", "w_ffn_up", "w_ffn_down", "w_ple_gate", "w_ple_proj"]


def host_consts():
    S = S_TOK
    c = {}
    c["ident"] = np.eye(128, dtype=np.float32)
    causal = np.zeros((4, 128, 512), np.float32)
    for m_ in range(4):
        kk = m_ * 128 + np.arange(128)[:, None]
        qq = np.arange(512)[None, :]
        causal[m_] = np.where(kk <= qq, 0.0, NEG)
    c["causal"] = causal
    pastneg = np.zeros((128, 16, 8), np.float32)
    notown = np.ones((128, 16, 8), np.float32)
    for t in range(16):
        for n in range(8):
            if n >= t // 2:
                pastneg[:, t, n] = -1e30
            if n == t // 2:
                notown[:, t, n] = 0.0
    c["pastneg"] = pastneg
    c["notown"] = notown
    E = np.zeros((8, 8, 128), np.float32)
    for n in range(8):
        E[n, n, :] = 1.0
    c["E"] = E
    half = 32
    inv = (1.0 / (10000.0 ** (np.arange(half, dtype=np.float32) / half))).astype(np.float32)
    rc = np.zeros((64, 2), np.float32)
    rc[:, 0] = np.concatenate([inv, inv])
    rc[:, 1] = np.concatenate([-np.ones(32), np.ones(32)])
    c["ropecols"] = rc
    cm = np.ones((16, S), np.float32)
    cm[:, ::64] = 0.0
    c["cmask"] = cm
    oh = np.zeros((16, 16, 128), np.float32)
    for h in range(16):
        oh[h, h, :] = 1.0
    c["onehot16"] = oh
    a = np.arange(128)[:, None]
    b = np.arange(128)[None, :]
    same = (a // 64) == (b // 64)
    c["negU"] = np.ascontiguousarray(np.broadcast_to(np.where(same & (b >= a), 0.0, NEG)[:, None, :], (128, 4, 128))).astype(np.float32)
    c["strU"] = np.ascontiguousarray(np.broadcast_to((same & (b > a)).astype(np.float32)[:, None, :], (128, 4, 128)))
    c["id8"] = np.ascontiguousarray(np.broadcast_to((b == a).astype(np.float32)[:, None, :], (128, 4, 128)))
    return c


def build(D, DFF, DEPTH, debug=(), phases=None):
    kb = KB(D, DFF, DEPTH, debug)
    nc = kb.nc
    S = S_TOK
    C = D // 128

    def inp(name, shape, dt=F32):
        return nc.dram_tensor(name, list(shape), dt, kind="ExternalInput").ap()
    xT = inp("xT", [D, S])
    pT = inp("pT", [DEPTH, 256, S])
    pos = inp("pos", [1, S], I32)
    W = {}
    shapes = {"w_in": [D, IN_WIDTH], "mla_w_uq": [768, 1536], "mla_w_ukv": [512, 2048], "w_branch_gate": [D, 3 * D],
              "w_branch_a": [1024, D], "w_branch_b": [2048, D], "w_branch_c": [1024, D], "w_out": [D, D],
              "w_ffn_gate": [D, DFF], "w_ffn_up": [D, DFF], "w_ffn_down": [DFF, D], "w_ple_gate": [D, D],
              "w_ple_proj": [256, D]}
    for n in WNAMES:
        W[n] = inp(n, [DEPTH] + shapes[n])
    for n in ("norm_mix_in", "norm_mix_out", "norm_ffn_in", "norm_ffn_out"):
        W[n] = inp(n, [DEPTH, 128, C])
    W["mla_q_norm_w"] = inp("mla_q_norm_w", [DEPTH, 128, 6])
    W["mla_kv_norm_w"] = inp("mla_kv_norm_w", [DEPTH, 128, 4])
    W["gdn_norm_w"] = inp("gdn_norm_w", [DEPTH, 128, 1])
    W["gdn_conv_w"] = inp("gdn_conv_w", [DEPTH, 128, 48, 4])
    W["gdn_hcols"] = inp("gdn_hcols", [DEPTH, 16, 2])
    hc = host_consts()
    CN = {k: inp("c_" + k, v.shape) for k, v in hc.items()}
    outT = nc.dram_tensor("outT", [D, S], F32, kind="ExternalOutput").ap()
    dr = kb.dram
    dr("xA", [D, S], F32)
    dr("xB", [D, S], F32)
    dr("hT", [D, S], BF16)
    dr("mqT", [1024, S], BF16)
    dr("mkT", [1024, S], BF16)
    dr("mv", [S, 1024], BF16)
    dr("gqkvT", [6144, S], F32)
    dr("gcT", [6144, S], F32)
    dr("gabT", [32, S], F32)
    dr("gzT", [2048, S], BF16)
    dr("gG", [6, 16, S], F32)
    dr("cqT", [768, S], F32)
    dr("ckvT", [512, S], F32)
    dr("krT", [64, S], F32)
    dr("krsT", [64, S], F32)
    dr("gateT", [3 * D, S], BF16)
    dr("yT", [4096, S], BF16)
    dr("mT", [D, S], BF16)
    dr("oT", [D, S], F32)
    dr("h2T", [D, S], BF16)
    dr("hidT", [DFF, S], BF16)
    dr("fT", [D, S], F32)
    dr("xbT", [D, S], BF16)
    dr("ropeT", [4, 64, S], F32)
    dr("QBd", [4, S], BF16)
    dr("KBd", [8, 4, S], BF16)
    d = kb.dr
    kb.setup_consts(CN["ident"])
    kb.ph_posconst(pos, CN)
    xcur = xT
    cnt = [0]

    def go(fn, *a, **k):
        cnt[0] += 1
        if phases is None or cnt[0] <= phases:
            fn(*a, **k)
    for L in range(DEPTH):
        go(kb.ph_norm, xcur, C, W["norm_mix_in"][L], d["hT"])
        go(kb.ph_inproj, L, d["hT"], W)
        go(kb.ph_moba, CN, pre=(kb.gdn_pre_alloc, lambda A, L=L: kb.gdn_pre_emit(L, W, CN, A)))
        go(lambda: None)
        go(kb.ph_gdn, L, W, CN)
        go(kb.ph_mla, L, W, CN)
        go(kb.ph_merge, L, W)
        go(kb.ph_linear_simple, d["mT"], C, W["w_out"][L], D, d["oT"], F32)
        go(kb.ph_resnorm, xcur, d["oT"], W["norm_mix_out"][L], d["xA"], gain_n=W["norm_ffn_in"][L], ndst=d["h2T"])
        go(kb.ph_ffn, L, W)
        go(kb.ph_resnorm, d["xA"], d["fT"], W["norm_ffn_out"][L], d["xB"], bdst=d["xbT"])
        last = (L == DEPTH - 1)
        xnext = outT if last else d["xA"]
        go(kb.ph_ple, L, W, pT, d["xB"], xnext)
        xcur = xnext
    return kb


def make_inputs_for_core(b, inputs, DEPTH, consts):
    f = np.float32
    m = {}
    m["xT"] = np.ascontiguousarray(inputs["x"][b].T)
    m["pT"] = np.ascontiguousarray(np.transpose(inputs["p"][:, b], (0, 2, 1)))
    m["pos"] = np.ascontiguousarray(inputs["positions"][b][None, :]).astype(np.int32)
    for k, v in consts.items():
        m["c_" + k] = v
    return m


def shared_inputs(inputs, D):
    C = D // 128
    m = {}
    for n in WNAMES:
        m[n] = np.ascontiguousarray(inputs[n], dtype=np.float32)
    for n in ("norm_mix_in", "norm_mix_out", "norm_ffn_in", "norm_ffn_out"):
        v = np.asarray(inputs[n], np.float32)
        m[n] = np.ascontiguousarray(v.reshape(v.shape[0], C, 128).transpose(0, 2, 1))
    v = np.asarray(inputs["mla_q_norm_w"], np.float32)
    m["mla_q_norm_w"] = np.ascontiguousarray(v.reshape(-1, 6, 128).transpose(0, 2, 1))
    v = np.asarray(inputs["mla_kv_norm_w"], np.float32)
    m["mla_kv_norm_w"] = np.ascontiguousarray(v.reshape(-1, 4, 128).transpose(0, 2, 1))
    v = np.asarray(inputs["gdn_norm_w"], np.float32)
    m["gdn_norm_w"] = np.ascontiguousarray(v.reshape(-1, 128, 1))
    v = np.asarray(inputs["gdn_conv_w"], np.float32)
    m["gdn_conv_w"] = np.ascontiguousarray(v.reshape(v.shape[0], 4, 48, 128).transpose(0, 3, 2, 1))
    m["gdn_hcols"] = np.ascontiguousarray(np.stack([np.asarray(inputs["gdn_a_log"], np.float32),
                                                   np.asarray(inputs["gdn_dt_bias"], np.float32)], axis=-1))
    return m


_CACHE = {}


def run(inputs, D, DFF, DEPTH, debug=(), trace=False, phases=None):
    B = inputs["x"].shape[0]
    key = (D, DFF, DEPTH, tuple(debug))
    kb = build(D, DFF, DEPTH, debug, phases)
    consts = host_consts()
    sh = shared_inputs(inputs, D)
    in_maps = []
    for b in range(B):
        m = make_inputs_for_core(b, inputs, DEPTH, consts)
        m.update(sh)
        in_maps.append(m)
    res = run_bass_kernel_spmd(kb.nc, in_maps, core_ids=list(range(B)), trace=trace)
    out = np.stack([np.ascontiguousarray(r["outT"].T) for r in res.results], axis=0)
    return out, res


def kernel(**inputs):
    inputs = {k: np.asarray(v) for k, v in inputs.items()}
    out, _ = run(inputs, 4096, 11008, 2)
    return out.astype(np.float32)
```

```python
import math
import numpy as np
import concourse.bass as bass
import concourse.mybir as mybir
from concourse.bass_utils import run_bass_kernel_spmd

F32 = mybir.dt.float32
BF16 = mybir.dt.bfloat16
I32 = mybir.dt.int32
AF = mybir.ActivationFunctionType
ALU = mybir.AluOpType
AX = mybir.AxisListType

ENGS = ("pe", "act", "dve", "pool", "sp")
S_TOK = 2048
NEG = -30000.0


class Res:
    __slots__ = ("w", "r")

    def __init__(self):
        self.w = None
        self.r = []


class Op:
    __slots__ = ("eng", "fn", "deps", "dma", "needed", "cnt", "dsem", "dval", "done", "key")

    def __init__(self, eng, fn, dma):
        self.eng = eng
        self.fn = fn
        self.deps = []
        self.dma = dma
        self.needed = False
        self.cnt = 0
        self.dsem = None
        self.dval = 0
        self.done = False


class Sched:
    NDMA = 12

    def __init__(self, nc):
        self.nc = nc
        self.ops = {e: [] for e in ENGS}
        self.esem = {e: nc.alloc_semaphore(name=f"es_{e}") for e in ENGS}
        self.ecnt = {e: 0 for e in ENGS}
        qs = ("sp", "act", "pool")
        self.dsems = {e: [nc.alloc_semaphore(name=f"ds_{e}{i}") for i in range(self.NDMA)] for e in qs}
        self.dcnt = {e: [0] * self.NDMA for e in qs}
        self.dnext = {e: 0 for e in qs}
        self.dlast = {e: [None] * self.NDMA for e in qs}
        self.waited = {e: {} for e in ENGS}
        self.pending_dma = []
        self.nops = 0
        self.cur = None
        self.nstreams = 0
        self.sops = []
        self.sidx = []
        self.sdn = []

    def begin_streams(self, n, stagger=0.0):
        self.nstreams = n
        self.stagger = stagger
        self.sops = [{e: [] for e in ENGS} for _ in range(n)]
        self.sidx = [0] * n
        per = self.NDMA // n
        self.sslots = [list(range(i * per, (i + 1) * per)) for i in range(n)]
        self.sdn = [{q: 0 for q in ("sp", "act", "pool")} for _ in range(n)]

    def merge_streams(self):
        n = self.nstreams
        lens = [max(1, self.sidx[i]) for i in range(n)]
        for e in ENGS:
            allops = []
            for i in range(n):
                for o in self.sops[i][e]:
                    o.key = (o.key[0] / lens[i] + i * self.stagger, i)
                    allops.append(o)
            allops.sort(key=lambda o: o.key)
            self.ops[e].extend(allops)
        self.cur = None
        self.nstreams = 0
        self.sops = []

    def op(self, eng, fn, reads=(), writes=(), dma=False):
        o = Op(eng, fn, dma)
        deps = o.deps
        for r in reads:
            if r.w is not None and not r.w.done:
                deps.append(r.w)
        for w in writes:
            if w.w is not None and not w.w.done:
                deps.append(w.w)
            for x in w.r:
                if not x.done:
                    deps.append(x)
        for r in reads:
            r.r.append(o)
        for w in writes:
            w.w = o
            w.r = []
        if dma:
            q = eng
            if self.cur is None:
                i = self.dnext[q]
                self.dnext[q] = (i + 1) % self.NDMA
            else:
                sl_ = self.sslots[self.cur]
                i = sl_[self.sdn[self.cur][q] % len(sl_)]
                self.sdn[self.cur][q] += 1
            prev = self.dlast[q][i]
            if prev is not None and not prev.done:
                deps.append(prev)
            self.dlast[q][i] = o
            self.dcnt[q][i] += 16
            o.dsem = self.dsems[q][i]
            o.dval = self.dcnt[q][i]
            self.pending_dma.append(o)
        if self.cur is None:
            self.ops[eng].append(o)
        else:
            c = self.cur
            o.key = (self.sidx[c], c)
            self.sidx[c] += 1
            self.sops[c][eng].append(o)
        self.nops += 1
        return o

    def flush(self):
        nc = self.nc
        for e in ENGS:
            for o in self.ops[e]:
                for d in o.deps:
                    if d.dma:
                        continue
                    if d.eng == "pe" and o.eng == "pe" and not o.dma:
                        continue
                    d.needed = True
        for e in ENGS:
            c = self.ecnt[e]
            for o in self.ops[e]:
                if o.dma:
                    continue
                if o.needed:
                    c += 1
                    o.cnt = c
            self.ecnt[e] = c
        pend = self.pending_dma
        esem = self.esem
        with nc.Block() as block:
            for e in ENGS:
                ops = self.ops[e]
                is_last = (e == "sp")
                if not ops and not (is_last and pend):
                    continue
                waited = self.waited[e]

                def body(eng, ops=ops, e=e, waited=waited, is_last=is_last):
                    for o in ops:
                        for d in o.deps:
                            if d.dma:
                                s, v = d.dsem, d.dval
                            else:
                                if d.eng == "pe" and e == "pe" and not o.dma:
                                    continue
                                s, v = esem[d.eng], d.cnt
                            k = id(s)
                            if waited.get(k, 0) >= v:
                                continue
                            waited[k] = v
                            eng.wait_ge(s, v)
                        inst = o.fn(eng)
                        if o.dma:
                            inst.then_inc(o.dsem, 16)
                        elif o.needed:
                            inst.then_inc(esem[e], 1)
                    if is_last:
                        for o in pend:
                            k = id(o.dsem)
                            if waited.get(k, 0) >= o.dval:
                                continue
                            waited[k] = o.dval
                            eng.wait_ge(o.dsem, o.dval)

                {"pe": block.tensor, "act": block.scalar, "dve": block.vector,
                 "pool": block.gpsimd, "sp": block.sync}[e](body)
        for e in ENGS:
            for o in self.ops[e]:
                o.fn = None
                o.done = True
        self.ops = {e: [] for e in ENGS}
        self.pending_dma = []


MOBA_W = 1024
GDN_KW = 2048
GDN_VW = 2048
IN_SIZES = (1024, 1024, 1024, 2048, 2048, 2048, 16, 16, 2048, 768, 512, 64)
IN_OFF = np.concatenate([[0], np.cumsum(IN_SIZES)]).astype(int).tolist()
IN_WIDTH = IN_OFF[-1]


class KB:
    def __init__(self, D, DFF, DEPTH, debug=()):
        self.D, self.DFF, self.DEPTH = D, DFF, DEPTH
        self.C = D // 128
        self.CF = DFF // 128
        self.debug = set(debug)
        self.nc = nc = bass.Bass("TRN2", target_bir_lowering=False)
        self.S = Sched(nc)
        self.BASE = 16512
        self.TOP = 229344
        self.off = self.BASE
        self.uid = 0
        self.banks = [(nc.alloc_psum_tensor(f"psb{i}", [128, 512], F32), Res()) for i in range(8)]
        self.rot = [0, 1, 2, 3]
        self.ri = 0
        self.fxmap = {}
        self.dr = {}

    def sb(self, shape, dt):
        esz = 2 if dt == BF16 else 4
        nb = int(np.prod(shape[1:])) * esz
        nb = (nb + 63) // 64 * 64
        assert self.off + nb <= self.TOP, f"SBUF overflow {self.off + nb}"
        self.uid += 1
        t = self.nc.alloc_sbuf_tensor_at(f"sb{self.uid}", list(shape), dt, offset=self.off)
        self.off += nb
        return t, Res()

    def mark(self):
        return self.off

    def release(self, m):
        self.off = m

    def bank(self):
        i = self.rot[self.ri % len(self.rot)]
        self.ri += 1
        return self.banks[i]

    def fixed(self, i):
        return self.banks[self.fxmap.get(i, i)]

    def use_stream(self, sid, rot, fxmap=None):
        self.S.cur = sid
        self.rot = list(rot)
        self.ri = 0
        self.fxmap = dict(fxmap or {})

    def end_streams(self):
        self.S.merge_streams()
        self.rot = [0, 1, 2, 3]
        self.ri = 0
        self.fxmap = {}

    def dram(self, name, shape, dt, kind=None):
        if kind is None:
            kind = "ExternalOutput" if name in self.debug else "Internal"
        t = self.nc.dram_tensor(name, list(shape), dt, kind=kind).ap()
        self.dr[name] = t
        return t

    def mm(self, out, lhsT, rhs, start, stop, R, W):
        self.S.op("pe", lambda e: e.matmul(out, lhsT, rhs, start=start, stop=stop), R, W)

    def tr(self, out, in_, ident, R, W):
        self.S.op("pe", lambda e: e.transpose(out, in_, ident), R, W)

    def act(self, out, in_, func, R, W, scale=None, bias=None):
        kw = {}
        if scale is not None:
            kw["scale"] = scale
        if bias is not None:
            kw["bias"] = bias
        self.S.op("act", lambda e: e.activation(out=out, in_=in_, func=func, **kw), R, W)

    def tt(self, out, in0, in1, op, R, W, eng="dve"):
        self.S.op(eng, lambda e: e.tensor_tensor(out=out, in0=in0, in1=in1, op=op), R, W)

    def ts(self, out, in0, s1, op0, R, W, s2=None, op1=None, eng="dve"):
        if op1 is None:
            self.S.op(eng, lambda e: e.tensor_scalar(out=out, in0=in0, scalar1=s1, scalar2=None, op0=op0), R, W)
        else:
            self.S.op(eng, lambda e: e.tensor_scalar(out=out, in0=in0, scalar1=s1, scalar2=s2, op0=op0, op1=op1), R, W)

    def stt(self, out, in0, scalar, in1, op0, op1, R, W):
        self.S.op("dve", lambda e: e.scalar_tensor_tensor(out=out, in0=in0, scalar=scalar, in1=in1, op0=op0, op1=op1), R, W)

    def cp(self, eng, out, in_, R, W):
        if eng == "act":
            self.S.op("act", lambda e: e.activation(out=out, in_=in_, func=AF.Copy), R, W)
        else:
            self.S.op(eng, lambda e: e.tensor_copy(out=out, in_=in_), R, W)

    def recip(self, out, in_, R, W):
        self.S.op("dve", lambda e: e.reciprocal(out=out, in_=in_), R, W)

    def memset(self, eng, ap, val, W):
        self.S.op(eng, lambda e: e.memset(ap, val), (), W)

    def dma(self, q, out, in_, R, W):
        self.S.op(q, lambda e: e.dma_start(out=out, in_=in_), R, W, dma=True)

    def flush(self):
        self.S.flush()

    def setup_consts(self, c_ident):
        self.ident_f, self.r_ident_f = self.sb([128, 128], F32)
        self.ident_b, self.r_ident_b = self.sb([128, 128], BF16)
        self.ones_b, self.r_ones_b = self.sb([128, 128], BF16)
        self.cb, self.r_cb = self.sb([128, 4], F32)
        self.dma("sp", self.ident_f[:], c_ident, [], [self.r_ident_f])
        self.dma("pool", self.ident_b[:], c_ident, [], [self.r_ident_b])
        self.memset("dve", self.ones_b[:], 1.0, [self.r_ones_b])
        self.memset("dve", self.cb[:, 0:1], 1e-6, [self.r_cb])
        self.memset("dve", self.cb[:, 1:2], 1.0, [self.r_cb])
        self.memset("dve", self.cb[:, 2:3], math.pi / 2, [self.r_cb])
        self.memset("dve", self.cb[:, 3:4], 0.0, [self.r_cb])
        self.eps = self.cb[:, 0:1]
        self.persist = self.off
        self.flush()

    def rstd(self, out, ores, ps, pres, n, scale, tmp, tres):
        self.act(tmp[:n, :], ps[:n, :], AF.Sqrt, [pres, self.r_cb], [tres], scale=scale, bias=self.cb[:n, 0:1])
        self.recip(out[:n, :], tmp[:n, :], [tres], [ores])

    def ph_norm(self, src, C, gain_dram, dst, dst_dt=BF16):
        m = self.mark()
        g, gr = self.sb([128, C], F32)
        self.dma("sp", g[:], gain_dram, [], [gr])
        xts = [self.sb([128, C, 512], F32) for _ in range(2)]
        hts = [(self.sb([128, C, 512], dst_dt)[0], [Res() for _ in range(C)]) for _ in range(2)]
        sqs = [self.sb([128, 512], BF16) for _ in range(4)]
        tmp, tmr = self.sb([128, 512], F32)
        rs, rsr = self.sb([128, 512], F32)
        sv = src.rearrange("(c p) s -> p c s", p=128)
        dv = dst.rearrange("(c p) s -> p c s", p=128)
        for tb in range(4):
            xt, xr = xts[tb % 2]
            ht, hr = hts[tb % 2]
            sl = slice(tb * 512, (tb + 1) * 512)
            self.dma("sp", xt[:], sv[:, :, sl], [], [xr])
            ps, pr = self.bank()
            for c in range(C):
                sq, sr = sqs[c % 4]
                self.act(sq[:], xt[:, c, :], AF.Square, [xr], [sr])
                self.mm(ps[:], self.ones_b[:], sq[:], c == 0, c == C - 1, [sr, self.r_ones_b], [pr])
            self.rstd(rs, rsr, ps, pr, 128, 1.0 / (C * 128), tmp, tmr)
            for c in range(C):
                self.stt(ht[:, c, :], xt[:, c, :], g[:, c:c + 1], rs[:], ALU.mult, ALU.mult, [xr, gr, rsr], [hr[c]])
            self.dma("sp", dv[:, :, sl], ht[:], hr, [])
        self.flush()
        self.release(m)

    def ph_resnorm(self, xsrc, usrc, gain_u, xdst, gain_n=None, ndst=None, bdst=None):
        C = self.C
        TW = 256
        m = self.mark()
        gu, gur = self.sb([128, C], F32)
        self.dma("sp", gu[:], gain_u, [], [gur])
        if gain_n is not None:
            gn, gnr = self.sb([128, C], F32)
            self.dma("sp", gn[:], gain_n, [], [gnr])
        xts = [(self.sb([128, C, TW], F32)[0], [Res() for _ in range(C)]) for _ in range(2)]
        uts = [(self.sb([128, C, TW], F32)[0], [Res() for _ in range(C)]) for _ in range(2)]
        hts = [(self.sb([128, C, TW], BF16)[0], [Res() for _ in range(C)]) for _ in range(2)]
        sqs = [self.sb([128, TW], BF16) for _ in range(4)]
        tmps = [self.sb([128, 512], F32) for _ in range(2)]
        rss = [self.sb([128, 512], F32) for _ in range(2)]
        xv = xsrc.rearrange("(c p) s -> p c s", p=128)
        uv = usrc.rearrange("(c p) s -> p c s", p=128)
        xdv = xdst.rearrange("(c p) s -> p c s", p=128)
        for tb in range(S_TOK // TW):
            sl = slice(tb * TW, (tb + 1) * TW)
            xt, xr = xts[tb % 2]
            ut, ur = uts[tb % 2]
            ht, hr = hts[tb % 2]
            tmp, tmr = tmps[tb % 2]
            rs, rsr = rss[tb % 2]
            self.dma("sp", ut[:], uv[:, :, sl], [], ur)
            self.dma("sp", xt[:], xv[:, :, sl], [], xr)
            ps, pr = self.bank()
            for c in range(C):
                sq, sr = sqs[c % 4]
                self.act(sq[:], ut[:, c, :], AF.Square, [ur[c]], [sr])
                self.mm(ps[:, :TW], self.ones_b[:], sq[:], c == 0, c == C - 1, [sr, self.r_ones_b], [pr])
            self.act(tmp[:, :TW], ps[:, :TW], AF.Sqrt, [pr, self.r_cb], [tmr], scale=1.0 / (C * 128), bias=self.cb[:, 0:1])
            self.recip(rs[:, :TW], tmp[:, :TW], [tmr], [rsr])
            if gain_n is not None:
                ps2, pr2 = self.bank()
            for c in range(C):
                self.stt(ut[:, c, :], ut[:, c, :], gu[:, c:c + 1], rs[:, :TW], ALU.mult, ALU.mult, [ur[c], gur, rsr], [ur[c]])
                self.tt(xt[:, c, :], xt[:, c, :], ut[:, c, :], ALU.add, [xr[c], ur[c]], [xr[c]], eng="pool")
                if gain_n is not None:
                    sq, sr = sqs[c % 4]
                    self.act(sq[:], xt[:, c, :], AF.Square, [xr[c]], [sr])
                    self.mm(ps2[:, :TW], self.ones_b[:], sq[:], c == 0, c == C - 1, [sr, self.r_ones_b], [pr2])
                if bdst is not None:
                    self.cp("act", ht[:, c, :], xt[:, c, :], [xr[c]], [hr[c]])
            self.dma("sp", xdv[:, :, sl], xt[:], xr, [])
            if gain_n is not None:
                self.act(tmp[:, :TW], ps2[:, :TW], AF.Sqrt, [pr2, self.r_cb], [tmr], scale=1.0 / (C * 128), bias=self.cb[:, 0:1])
                self.recip(rs[:, :TW], tmp[:, :TW], [tmr], [rsr])
                for c in range(C):
                    self.stt(ht[:, c, :], xt[:, c, :], gn[:, c:c + 1], rs[:, :TW], ALU.mult, ALU.mult, [xr[c], gnr, rsr], [hr[c]])
                self.dma("sp", ndst.rearrange("(c p) s -> p c s", p=128)[:, :, sl], ht[:], hr, [])
            if bdst is not None:
                self.dma("sp", bdst.rearrange("(c p) s -> p c s", p=128)[:, :, sl], ht[:], hr, [])
        self.flush()
        self.release(m)

    def load_x(self, src, KC, dt=BF16):
        xt, _ = self.sb([128, KC, S_TOK], dt)
        sv = src.rearrange("(c p) s -> p c s", p=128)
        rs = [Res() for _ in range(4)]
        for q in range(4):
            sl = slice(q * 512, (q + 1) * 512)
            self.dma("sp", xt[:, :, sl], sv[:, :, sl], [], [rs[q]])
        return xt, rs

    def make_wbufs(self, KCs, n=3):
        return [[self.sb([128, kc, 128], BF16) for _ in range(n)] for kc in KCs]

    def multilinear(self, srcs, ncols_total, epi, wbufs, tbs=(0, 1, 2, 3), tw=512):
        wi = getattr(self, "_wi", 0)
        chunks = [(n0, min(128, ncols_total - n0)) for n0 in range(0, ncols_total, 128)]

        def issue(ci, wi_):
            n0, n_ = chunks[ci]
            wts = []
            for si, (xt, xres, KC, w) in enumerate(srcs):
                wt, wres = wbufs[si][wi_ % len(wbufs[si])]
                wv = w.rearrange("(c p) n -> p c n", p=128)
                self.dma("pool", wt[:, :KC, :n_], wv[:, :, n0:n0 + n_], [], [wres])
                wts.append((wt, wres))
            return wts
        nxt = issue(0, wi)
        for ci, (n0, n_) in enumerate(chunks):
            wts = nxt
            wi += 1
            if ci + 1 < len(chunks):
                nxt = issue(ci + 1, wi)
            for tb in tbs:
                pss = []
                for si, (xt, xres, KC, w) in enumerate(srcs):
                    ps, pres = self.bank()
                    wt, wres = wts[si]
                    for kc in range(KC):
                        self.mm(ps[:n_, :tw], wt[:, kc, :n_], xt[:, kc, tb * tw:(tb + 1) * tw],
                                kc == 0, kc == KC - 1, [wres, xres[tb] if isinstance(xres, list) else xres], [pres])
                    pss.append((ps, pres))
                epi(pss, n0, n_, tb)
        self._wi = wi

    def epi_store(self, dst, dt, func=None, scale=None, stg=None, row0=0):
        k = [0]

        def epi(pss, n0, n_, tb):
            ps, pres = pss[0]
            st, sr = stg[k[0] % len(stg)]
            k[0] += 1
            sl = slice(tb * 512, (tb + 1) * 512)
            if func is None and (k[0] % 2 == 0):
                if scale is None:
                    self.cp("dve", st[:n_, :], ps[:n_, :], [pres], [sr])
                else:
                    self.ts(st[:n_, :], ps[:n_, :], scale, ALU.mult, [pres], [sr])
            else:
                self.act(st[:n_, :], ps[:n_, :], func or AF.Copy, [pres], [sr], scale=scale)
            self.dma("sp", dst[row0 + n0:row0 + n0 + n_, sl], st[:n_, :], [sr], [])
        return epi

    def ph_inproj(self, L, hT, W):
        D, C = self.D, self.C
        m = self.mark()
        self.rot = [0, 1, 2, 3, 4, 5, 6, 7]
        xt, xr = self.load_x(hT, C)
        wb = self.make_wbufs([C], 3)
        stf = [self.sb([128, 512], F32) for _ in range(3)]
        stb = [self.sb([128, 512], BF16) for _ in range(3)]
        w_in = W["w_in"][L]
        d = self.dr

        def grp(gi):
            return w_in[:, IN_OFF[gi]:IN_OFF[gi + 1]]
        srcs = lambda gi: [(xt, xr, C, grp(gi))]
        self.multilinear(srcs(0), 1024, self.epi_store(d["mqT"], BF16, scale=128 ** -0.5, stg=stb), wb)
        self.multilinear(srcs(1), 1024, self.epi_store(d["mkT"], BF16, stg=stb), wb)
        self.multilinear([(xt, xr, C, w_in[:, IN_OFF[3]:IN_OFF[6]])], 6144, self.epi_store(d["gqkvT"], F32, stg=stf), wb)
        self.multilinear([(xt, xr, C, w_in[:, IN_OFF[6]:IN_OFF[8]])], 32, self.epi_store(d["gabT"], F32, stg=stf), wb)
        self.multilinear(srcs(8), 2048, self.epi_store(d["gzT"], BF16, func=AF.Silu, stg=stb), wb)
        self.multilinear(srcs(9), 768, self.epi_store(d["cqT"], F32, stg=stf), wb)
        self.multilinear(srcs(10), 512, self.epi_store(d["ckvT"], F32, stg=stf), wb)
        self.multilinear(srcs(11), 64, self.epi_store(d["krT"], F32, stg=stf), wb)
        o = IN_OFF[11]
        self.multilinear([(xt, xr, C, w_in[:, o + 32:o + 64])], 32, self.epi_store(d["krsT"], F32, stg=stf, row0=0), wb)
        self.multilinear([(xt, xr, C, w_in[:, o:o + 32])], 32, self.epi_store(d["krsT"], F32, stg=stf, row0=32), wb)
        self.multilinear([(xt, xr, C, W["w_branch_gate"][L])], 3 * D, self.epi_store(d["gateT"], BF16, func=AF.Sigmoid, stg=stb), wb)
        wt, wtr = self.sb([128, C, 512], BF16)
        wv = w_in[:, IN_OFF[2]:IN_OFF[3]].rearrange("(c p) n -> p c n", p=128)
        k = 0
        for n0 in range(0, 1024, 512):
            self.dma("pool", wt[:], wv[:, :, n0:n0 + 512], [], [wtr])
            for t in range(16):
                ps, pres = self.bank()
                for kc in range(C):
                    self.mm(ps[:], xt[:, kc, t * 128:(t + 1) * 128], wt[:, kc, :], kc == 0, kc == C - 1, [xr[t // 4], wtr], [pres])
                st, sr = stb[k % 3]
                k += 1
                self.cp("act" if k % 2 else "dve", st[:], ps[:], [pres], [sr])
                self.dma("sp", d["mv"][t * 128:(t + 1) * 128, n0:n0 + 512], st[:], [sr], [])
        self.flush()
        self.rot = [0, 1, 2, 3]
        self.release(m)

    def attn_core(self, qk_parts, extras, V, Vr, masks, mr, dst_rows, pts, ystg):
        pk = 0
        for j in range(4):
            num, numr = self.fixed(4 + 2 * (j % 2))
            den, denr = self.fixed(5 + 2 * (j % 2))
            nk = 4 * j + 4
            qs = slice(j * 512, (j + 1) * 512)
            for i in range(nk):
                ks = slice(i * 128, (i + 1) * 128)
                ps, pres = self.bank()
                lst = [(kT[:, ks], qT[:, qs], [kr, qr]) for (kT, kr, qT, qr) in qk_parts]
                lst += extras(i, j)
                if i >= 4 * j:
                    lst.append((self.ident_b[:], masks[:, i - 4 * j, :], [self.r_ident_b, mr]))
                for idx, (l, r, R) in enumerate(lst):
                    self.mm(ps[:], l, r, idx == 0, idx == len(lst) - 1, R, [pres])
                pt, ptr = pts[pk % len(pts)]
                pk += 1
                self.act(pt[:], ps[:], AF.Exp, [pres], [ptr])
                self.mm(num[:], V[:, i, :], pt[:], i == 0, i == nk - 1, [Vr, ptr], [numr])
                self.mm(den[:], self.ones_b[:], pt[:], i == 0, i == nk - 1, [self.r_ones_b, ptr], [denr])
            rd, rdr = ystg[0]
            yb, ybr = ystg[1 + (j % 2)]
            self.recip(rd[:], den[:], [denr], [rdr])
            self.tt(yb[:], num[:], rd[:], ALU.mult, [numr, rdr], [ybr])
            self.dma("sp", dst_rows[:, qs], yb[:], [ybr], [])

    def ph_moba(self, CN, pre=None):
        d = self.dr
        m = self.mark()
        masks, mr = self.sb([128, 4, 512], BF16)
        self.dma("pool", masks[:], CN["causal"].rearrange("m p q -> p m q"), [], [mr])
        pastneg, pnr = self.sb([128, 16, 8], F32)
        notown, nor = self.sb([128, 16, 8], F32)
        self.dma("sp", pastneg[:], CN["pastneg"], [], [pnr])
        self.dma("sp", notown[:], CN["notown"], [], [nor])
        E, Er = self.sb([8, 8, 128], BF16)
        self.dma("pool", E[:], CN["E"], [], [Er])
        QB, QBr = self.sb([4, S_TOK], BF16)
        self.dma("sp", QB[:], d["QBd"], [], [QBr])
        def alloc_stream():
            pts = [self.sb([128, 512], BF16) for _ in range(2)]
            ystg = [self.sb([128, 512], F32)] + [self.sb([128, 512], BF16) for _ in range(2)]
            hb = []
            for _ in range(1 if pre is not None else 2):
                hb.append(dict(q=self.sb([128, S_TOK], BF16), k=self.sb([128, S_TOK], BF16),
                               v=self.sb([128, 16, 128], BF16), kb=self.sb([4, S_TOK], BF16),
                               km=self.sb([128, 8], F32), kmb=self.sb([128, 8], BF16),
                               gm=self.sb([128, 16, 8], F32), m8=self.sb([128, 16, 8], F32),
                               thr=self.sb([128, 16], F32), ns=self.sb([128, 16, 8], F32),
                               nsT=self.sb([8, S_TOK], BF16)))
            return pts, ystg, hb
        SB_ = [alloc_stream() for _ in range(2)]
        if pre is not None:
            pre_alloc = pre[0]()
        self.S.begin_streams(3 if pre is not None else 2)
        for si in range(2):
          pts, ystg, hb = SB_[si]
          b0 = 3 * si
          self.use_stream(si, [b0], {4: b0 + 1, 5: b0 + 2, 6: b0 + 1, 7: b0 + 2})
          for h in range(4 * si, 4 * si + 4):
            B = hb[h % len(hb)]
            (q, qr), (k, kr), (v, vr), (kb, kbr) = B["q"], B["k"], B["v"], B["kb"]
            rows = slice(h * 128, (h + 1) * 128)
            self.dma("sp", q[:], d["mqT"][rows, :], [], [qr])
            self.dma("sp", k[:], d["mkT"][rows, :], [], [kr])
            self.dma("sp", v[:], d["mv"].rearrange("(t p) c -> p t c", p=128)[:, :, rows], [], [vr])
            self.dma("sp", kb[:], d["KBd"][h], [], [kbr])
            km, kmr = B["km"]
            kmb, kmbr = B["kmb"]
            self.S.op("dve", lambda e, km=km, k=k: e.tensor_reduce(out=km[:], in_=k[:].rearrange("p (n b) -> p n b", b=256), axis=AX.X, op=ALU.add), [kr], [kmr])
            self.ts(kmb[:], km[:], 1.0 / 256, ALU.mult, [kmr], [kmbr])
            gps, gpr = self.bank()
            for t in range(16):
                self.mm(gps[:, t * 8:(t + 1) * 8], q[:, t * 128:(t + 1) * 128], kmb[:], True, True, [qr, kmbr], [gpr])
            gm, gmr = B["gm"]
            m8, m8r = B["m8"]
            thr, thrr = B["thr"]
            ns, nsr = B["ns"]
            self.tt(gm[:], gps[:, 0:128].rearrange("p (t n) -> p t n", n=8), pastneg[:], ALU.add, [gpr, pnr], [gmr])
            for t in range(16):
                self.S.op("dve", lambda e, m8=m8, gm=gm, t=t: e.max(m8[:, t, :], gm[:, t, :]), [gmr], [m8r])
            self.ts(thr[:], m8[:, :, 2], -1e29, ALU.max, [m8r], [thrr])
            self.tt(ns[:], gm[:], thr[:].unsqueeze(2).broadcast_to([128, 16, 8]), ALU.is_lt, [gmr, thrr], [nsr])
            self.stt(ns[:], ns[:], NEG, notown[:], ALU.mult, ALU.mult, [nsr, nor], [nsr])
            nsT, nsTr = B["nsT"]
            for g4 in range(4):
                tp, tpr = self.bank()
                for tt_ in range(4):
                    t = g4 * 4 + tt_
                    self.tr(tp[0:8, tt_ * 128:(tt_ + 1) * 128], ns[:, t, :], self.ident_f[:], [nsr, self.r_ident_f], [tpr])
                self.cp("act", nsT[:, g4 * 512:(g4 + 1) * 512], tp[0:8, :], [tpr], [nsTr])

            def extras(i, j, kb=kb, kbr=kbr, nsT=nsT, nsTr=nsTr):
                ks = slice(i * 128, (i + 1) * 128)
                qs = slice(j * 512, (j + 1) * 512)
                return [(kb[:, ks], QB[:, qs], [kbr, QBr]),
                        (E[:, i // 2, :], nsT[:, qs], [Er, nsTr])]
            self.attn_core([(k, kr, q, qr)], extras, v, vr, masks, mr, d["yT"][rows, :], pts, ystg)
        if pre is not None:
            self.use_stream(2, [6, 7], {})
            pre[1](pre_alloc)
        self.end_streams()
        self.flush()
        self.release(m)

    def ph_mla(self, L, W, CN):
        d = self.dr
        m = self.mark()
        sc = 192 ** -0.5
        masks, mr = self.sb([128, 4, 512], BF16)
        self.dma("pool", masks[:], CN["causal"].rearrange("m p q -> p m q"), [], [mr])
        cqn, cqnr = self.sb([128, 6, S_TOK], BF16)
        ckn, cknr = self.sb([128, 4, S_TOK], BF16)
        wuq, wuqr = self.sb([128, 6, 1536], BF16)
        wukv, wukvr = self.sb([128, 4, 2048], BF16)
        wsw, wswr = self.sb([128, 6, 512], BF16)
        self.dma("pool", wuq[:], W["mla_w_uq"][L].rearrange("(c p) n -> p c n", p=128), [], [wuqr])
        self.dma("pool", wukv[:], W["mla_w_ukv"][L].rearrange("(c p) n -> p c n", p=128), [], [wukvr])
        wq4 = W["mla_w_uq"][L].rearrange("(c p) (h e) -> p c h e", p=128, e=192)
        wsw4 = wsw[:].rearrange("p c (h e) -> p c h e", e=64)
        for c in range(6):
            self.dma("pool", wsw4[:, c, :, 0:32], wq4[:, c, :, 160:192], [], [wswr])
            self.dma("pool", wsw4[:, c, :, 32:64], wq4[:, c, :, 128:160], [], [wswr])
        rope, roper = self.sb([64, 4, S_TOK], F32)
        self.dma("sp", rope[:], d["ropeT"].rearrange("f p s -> p f s"), [], [roper])
        kpe, kper = self.sb([64, S_TOK], BF16)
        tmp, tmr = self.sb([128, 512], F32)
        rs, rsr = self.sb([128, 512], F32)
        m2 = self.mark()
        sqs = [self.sb([128, 512], BF16) for _ in range(3)]
        for (src, Cn, gname, dstt, dr_) in ((d["cqT"], 6, "mla_q_norm_w", cqn, cqnr), (d["ckvT"], 4, "mla_kv_norm_w", ckn, cknr)):
            g, gr = self.sb([128, Cn], F32)
            self.dma("sp", g[:], W[gname][L], [], [gr])
            xt, xr = self.sb([128, Cn, 512], F32)
            sv = src.rearrange("(c p) s -> p c s", p=128)
            for tb in range(4):
                sl = slice(tb * 512, (tb + 1) * 512)
                self.dma("sp", xt[:], sv[:, :, sl], [], [xr])
                ps, pr = self.bank()
                for c in range(Cn):
                    sq, sr = sqs[c % 3]
                    self.act(sq[:], xt[:, c, :], AF.Square, [xr], [sr])
                    self.mm(ps[:], self.ones_b[:], sq[:], c == 0, c == Cn - 1, [sr, self.r_ones_b], [pr])
                self.rstd(rs, rsr, ps, pr, 128, 1.0 / (Cn * 128), tmp, tmr)
                for c in range(Cn):
                    self.stt(dstt[:, c, sl], xt[:, c, :], g[:, c:c + 1], rs[:], ALU.mult, ALU.mult, [xr, gr, rsr], [dr_])
        kr_, krr = self.sb([64, S_TOK], F32)
        krs, krsr = self.sb([64, S_TOK], F32)
        self.dma("sp", kr_[:], d["krT"], [], [krr])
        self.dma("sp", krs[:], d["krsT"], [], [krsr])
        self.tt(kr_[:], kr_[:], rope[:, 0, :], ALU.mult, [krr, roper], [krr])
        self.tt(krs[:], krs[:], rope[:, 1, :], ALU.mult, [krsr, roper], [krsr])
        self.tt(kpe[:], kr_[:], krs[:], ALU.add, [krr, krsr], [kper])
        self.flush()
        self.release(m2)
        def alloc_stream():
            pts = [self.sb([128, 512], BF16) for _ in range(2)]
            ystg = [self.sb([128, 512], F32)] + [self.sb([128, 512], BF16) for _ in range(2)]
            t1 = self.sb([64, 512], F32)
            t2 = self.sb([64, 512], F32)
            hb = [dict(qn=self.sb([128, S_TOK], BF16), qpe=self.sb([64, S_TOK], BF16),
                       kn=self.sb([128, S_TOK], BF16), v=self.sb([128, 16, 128], BF16)) for _ in range(1)]
            return pts, ystg, t1, t2, hb
        SB_ = [alloc_stream() for _ in range(2)]
        self.S.begin_streams(2)
        for si in range(2):
          pts, ystg, (t1, t1r), (t2, t2r), hb = SB_[si]
          b0 = 4 * si
          self.use_stream(si, [b0, b0 + 1], {4: b0 + 2, 5: b0 + 3, 6: b0 + 2, 7: b0 + 3})
          for h in range(4 * si, 4 * si + 4):
            B = hb[0]
            (qn, qnr), (qpe, qper), (kn, knr), (v, vr) = B["qn"], B["qpe"], B["kn"], B["v"]
            for tb in range(4):
                sl = slice(tb * 512, (tb + 1) * 512)
                ps, pr = self.bank()
                for c in range(6):
                    self.mm(ps[:], wuq[:, c, h * 192:h * 192 + 128], cqn[:, c, sl], c == 0, c == 5, [wuqr, cqnr], [pr])
                self.act(qn[:, sl], ps[:], AF.Copy, [pr], [qnr], scale=sc)
                ps, pr = self.bank()
                for c in range(4):
                    self.mm(ps[:], wukv[:, c, h * 256:h * 256 + 128], ckn[:, c, sl], c == 0, c == 3, [wukvr, cknr], [pr])
                self.cp("dve", kn[:, sl], ps[:], [pr], [knr])
                ps, pr = self.bank()
                for c in range(6):
                    self.mm(ps[0:64, :], wuq[:, c, h * 192 + 128:h * 192 + 192], cqn[:, c, sl], c == 0, c == 5, [wuqr, cqnr], [pr])
                ps2, pr2 = self.bank()
                for c in range(6):
                    self.mm(ps2[0:64, :], wsw[:, c, h * 64:(h + 1) * 64], cqn[:, c, sl], c == 0, c == 5, [wswr, cqnr], [pr2])
                self.tt(t1[:], ps[0:64, :], rope[:, 2, sl], ALU.mult, [pr, roper], [t1r])
                self.tt(t2[:], ps2[0:64, :], rope[:, 3, sl], ALU.mult, [pr2, roper], [t2r])
                self.tt(qpe[:, sl], t1[:], t2[:], ALU.add, [t1r, t2r], [qper])
            for t in range(16):
                ps, pr = self.bank()
                for c in range(4):
                    self.mm(ps[:, 0:128], ckn[:, c, t * 128:(t + 1) * 128], wukv[:, c, h * 256 + 128:h * 256 + 256], c == 0, c == 3, [cknr, wukvr], [pr])
                self.cp("act" if t % 2 else "dve", v[:, t, :], ps[:, 0:128], [pr], [vr])
            self.attn_core([(kn, knr, qn, qnr), (kpe, kper, qpe, qper)], lambda i, j: [], v, vr, masks, mr,
                           d["yT"][3072 + h * 128:3072 + (h + 1) * 128, :], pts, ystg)
        self.end_streams()
        self.flush()
        self.release(m)

    def ph_posconst(self, pos, CN):
        d = self.dr
        m = self.mark()
        pi_, pir = self.sb([64, S_TOK], I32)
        self.dma("sp", pi_[:], pos.broadcast_to([64, S_TOK]), [], [pir])
        pf, pfr = self.sb([64, S_TOK], F32)
        self.cp("dve", pf[:], pi_[:], [pir], [pfr])
        cols, colr = self.sb([64, 2], F32)
        self.dma("sp", cols[:], CN["ropecols"], [], [colr])
        a, ar = self.sb([64, S_TOK], F32)
        k_, kr = self.sb([64, S_TOK], F32)
        r_, rr = self.sb([64, S_TOK], F32)
        o, orr = self.sb([64, 4, S_TOK], F32)
        MAG = 12582912.0
        self.ts(a[:], pf[:], cols[:, 0:1], ALU.mult, [pfr, colr], [ar])
        self.ts(k_[:], a[:], 1.0 / (2 * math.pi), ALU.mult, [ar], [kr], s2=MAG, op1=ALU.add)
        self.ts(k_[:], k_[:], -MAG, ALU.add, [kr], [kr])
        C1 = 6.28125
        C2 = 2 * math.pi - C1
        self.stt(r_[:], k_[:], -C1, a[:], ALU.mult, ALU.add, [kr, ar], [rr])
        self.stt(r_[:], k_[:], -C2, r_[:], ALU.mult, ALU.add, [kr, rr], [rr])
        PI_ = 3.1415925
        self.ts(r_[:], r_[:], PI_, ALU.min, [rr], [rr], s2=-PI_, op1=ALU.max)
        self.act(o[:, 1, :], r_[:], AF.Sin, [rr], [orr])
        self.ts(a[:], r_[:], -1.0, ALU.mult, [rr], [ar])
        self.tt(a[:], a[:], r_[:], ALU.min, [ar, rr], [ar])
        self.ts(a[:], a[:], math.pi / 2, ALU.add, [ar], [ar], s2=1.5707963, op1=ALU.min)
        self.act(o[:, 0, :], a[:], AF.Sin, [ar], [orr])
        self.ts(o[:, 1, :], o[:, 1, :], cols[:, 1:2], ALU.mult, [orr, colr], [orr])
        sc = 192 ** -0.5
        self.ts(o[:, 2, :], o[:, 0, :], sc, ALU.mult, [orr], [orr])
        self.ts(o[:, 3, :], o[:, 1, :], sc, ALU.mult, [orr], [orr])
        self.dma("sp", d["ropeT"].rearrange("f p s -> p f s"), o[:], [orr], [])
        hf, hfr = self.sb([1, S_TOK], F32)
        lf, lfr = self.sb([1, S_TOK], F32)
        self.ts(hf[:], pf[0:1, :], 1.0 / 128, ALU.mult, [pfr], [hfr], s2=-127.0 / 256, op1=ALU.add)
        self.ts(hf[:], hf[:], MAG, ALU.add, [hfr], [hfr])
        self.ts(hf[:], hf[:], -MAG, ALU.add, [hfr], [hfr])
        self.stt(lf[:], hf[:], -128.0, pf[0:1, :], ALU.mult, ALU.add, [hfr, pfr], [lfr])
        rows = [self.sb([1, S_TOK], BF16) for _ in range(4)]
        rb, rbr = rows[0]
        self.cp("dve", rb[:], hf[:], [hfr], [rbr])
        self.dma("sp", d["QBd"][0:1, :], rb[:], [rbr], [])
        rb, rbr = rows[1]
        self.cp("dve", rb[:], lf[:], [lfr], [rbr])
        self.dma("sp", d["QBd"][1:2, :], rb[:], [rbr], [])
        rb, rbr = rows[2]
        self.memset("dve", rb[:], 1.0, [rbr])
        self.dma("sp", d["QBd"][2:3, :], rb[:], [rbr], [])
        self.dma("sp", d["QBd"][3:4, :], rb[:], [rbr], [])
        k = 0
        for h in range(8):
            s = 2.0 ** -(h + 1)
            for ri, (src, sr_, val) in enumerate(((None, None, -128 * s), (None, None, -s), (hf, hfr, 128 * s), (lf, lfr, s))):
                rb, rbr = rows[k % 4]
                k += 1
                if src is None:
                    self.memset("dve", rb[:], val, [rbr])
                else:
                    self.ts(rb[:], src[:], val, ALU.mult, [sr_], [rbr])
                self.dma("sp", d["KBd"][h, ri:ri + 1, :], rb[:], [rbr], [])
        self.flush()
        self.release(m)

    def gdn_pre_alloc(self):
        A = {}
        A["cw"] = self.sb([128, 48, 4], F32)
        A["xps"] = [self.sb([128, S_TOK + 3], F32) for _ in range(2)]
        A["accs"] = [self.sb([128, S_TOK], F32) for _ in range(2)]
        A["sqs"] = [self.sb([128, 512], BF16) for _ in range(2)]
        A["lns"] = [self.sb([128, 512], F32) for _ in range(2)]
        A["rss"] = [self.sb([128, 512], F32) for _ in range(2)]
        A["a"] = self.sb([16, S_TOK], F32)
        A["b"] = self.sb([16, S_TOK], F32)
        A["hc"] = self.sb([16, 2], F32)
        A["cm"] = self.sb([16, S_TOK], F32)
        A["G"] = self.sb([16, 6, S_TOK], F32)
        A["x"] = self.sb([16, S_TOK], F32)
        A["y"] = self.sb([16, S_TOK], F32)
        A["nA"] = self.sb([16, 1], F32)
        return A

    def gdn_pre_emit(self, L, W, CN, A):
        d = self.dr
        cw, cwr = A["cw"]
        self.dma("sp", cw[:], W["gdn_conv_w"][L], [], [cwr])
        xps, accs, sqs, lns, rss = A["xps"], A["accs"], A["sqs"], A["lns"], A["rss"]
        for (xp, xpr) in xps:
            self.memset("dve", xp[:, 0:3], 0.0, [xpr])
        kk = 0
        for c in range(48):
            xp, xpr = xps[c % 2]
            acc, accr = accs[c % 2]
            self.dma("sp", xp[:, 3:], d["gqkvT"][c * 128:(c + 1) * 128, :], [], [xpr])
            self.ts(acc[:], xp[:, 0:S_TOK], cw[:, c, 0:1], ALU.mult, [xpr, cwr], [accr])
            for j in range(1, 4):
                self.stt(acc[:], xp[:, j:j + S_TOK], cw[:, c, j:j + 1], acc[:], ALU.mult, ALU.add, [xpr, cwr, accr], [accr])
            self.act(acc[:], acc[:], AF.Silu, [accr], [accr])
            if c < 32:
                for tb in range(4):
                    sl = slice(tb * 512, (tb + 1) * 512)
                    sq, sr = sqs[kk % 2]
                    ln, lnr = lns[kk % 2]
                    rs, rsr = rss[kk % 2]
                    kk += 1
                    ps, pr = self.bank()
                    self.act(sq[:], acc[:, sl], AF.Square, [accr], [sr])
                    self.mm(ps[:], self.ones_b[:], sq[:], True, True, [sr, self.r_ones_b], [pr])
                    self.act(ln[:], ps[:], AF.Ln, [pr, self.r_cb], [lnr], bias=self.cb[:, 0:1])
                    self.act(rs[:], ln[:], AF.Exp, [lnr], [rsr], scale=-0.5)
                    if c < 16:
                        self.stt(acc[:, sl], acc[:, sl], 128 ** -0.5, rs[:], ALU.mult, ALU.mult, [accr, rsr], [accr])
                    else:
                        self.tt(acc[:, sl], acc[:, sl], rs[:], ALU.mult, [accr, rsr], [accr])
            self.dma("act", d["gcT"][c * 128:(c + 1) * 128, :], acc[:], [accr], [])
        (a_, ar), (b_, br), (hc, hcr), (cm, cmr), (G, Gr), (x_, xr), (y_, yr), (nA, nAr) = \
            A["a"], A["b"], A["hc"], A["cm"], A["G"], A["x"], A["y"], A["nA"]
        self.dma("sp", a_[:], d["gabT"][0:16, :], [], [ar])
        self.dma("sp", b_[:], d["gabT"][16:32, :], [], [br])
        self.dma("sp", hc[:], W["gdn_hcols"][L], [], [hcr])
        self.dma("sp", cm[:], CN["cmask"], [], [cmr])
        self.act(nA[:], hc[:, 0:1], AF.Exp, [hcr], [nAr])
        self.ts(nA[:], nA[:], -1.0, ALU.mult, [nAr], [nAr])
        self.ts(x_[:], a_[:], hc[:, 1:2], ALU.add, [ar, hcr], [xr])
        self.ts(y_[:], x_[:], -1.0, ALU.mult, [xr], [yr])
        self.tt(y_[:], y_[:], x_[:], ALU.max, [yr, xr], [yr])
        self.act(y_[:], y_[:], AF.Exp, [yr], [yr], scale=-1.0)
        self.act(y_[:], y_[:], AF.Ln, [yr, self.r_cb], [yr], bias=self.cb[0:16, 1:2])
        self.ts(x_[:], x_[:], 0.0, ALU.max, [xr], [xr])
        self.tt(x_[:], x_[:], y_[:], ALU.add, [xr, yr], [xr])
        self.ts(x_[:], x_[:], nA[:, 0:1], ALU.mult, [xr, nAr], [xr])
        self.S.op("dve", lambda e: e.tensor_tensor_scan(out=G[:, 0, :], data0=cm[:], data1=x_[:], initial=0.0, op0=ALU.mult, op1=ALU.add), [cmr, xr], [Gr])
        self.act(G[:, 1, :], b_[:], AF.Sigmoid, [br], [Gr])
        self.act(G[:, 2, :], G[:, 0, :], AF.Exp, [Gr], [Gr])
        self.tt(G[:, 3, :], G[:, 1, :], G[:, 2, :], ALU.mult, [Gr], [Gr])
        gc3 = G[:, 0, :].rearrange("p (n l) -> p n l", l=64)
        self.tt(y_[:].rearrange("p (n l) -> p n l", l=64), gc3[:, :, 63:64].broadcast_to([16, 32, 64]), gc3, ALU.subtract, [Gr], [yr])
        self.act(G[:, 4, :], y_[:], AF.Exp, [yr], [Gr])
        self.ts(G[:, 5, :], G[:, 0, :], -1.0, ALU.mult, [Gr], [Gr])
        self.dma("sp", d["gG"].rearrange("f h s -> h f s"), G[:], [Gr], [])

    def ph_gdn_pre(self, L, W, CN):
        m = self.mark()
        A = self.gdn_pre_alloc()
        self.gdn_pre_emit(L, W, CN, A)
        self.flush()
        self.release(m)

    def ph_gdn(self, L, W, CN):
        d = self.dr
        m = self.mark()
        G5, Gr = self.sb([16, S_TOK], F32)
        self.dma("sp", G5[:], d["gG"][5], [], [Gr])
        negU, negUr = self.sb([128, 4, 128], F32)
        strU, strUr = self.sb([128, 4, 128], F32)
        id8, id8r = self.sb([128, 4, 128], F32)
        self.dma("sp", negU[:], CN["negU"], [], [negUr])
        self.dma("sp", strU[:], CN["strU"], [], [strUr])
        self.dma("sp", id8[:], CN["id8"], [], [id8r])
        nw, nwr = self.sb([128, 1], F32)
        self.dma("sp", nw[:], W["gdn_norm_w"][L], [], [nwr])
        ngT, ngTr = self.sb([128, 16, 16], F32)
        for g8 in range(2):
            tp, tpr = self.bank()
            for cc in range(8):
                c = g8 * 8 + cc
                self.tr(tp[:, cc * 16:(cc + 1) * 16], G5[:, c * 128:(c + 1) * 128], self.ident_f[0:16, 0:16], [Gr, self.r_ident_f], [tpr])
            self.cp("dve", ngT[:, g8 * 8:(g8 + 1) * 8, :].rearrange("p c h -> p (c h)"), tp[:, 0:128], [tpr], [ngTr])

        def alloc_head():
            def T3():
                return self.sb([128, 4, 128], F32)
            ins = [dict(q=self.sb([128, 512], F32), k=self.sb([128, 512], F32), v=self.sb([128, 512], F32),
                        z=self.sb([128, 512], BF16)) for _ in range(2)]
            kbgT, kbgTr = self.sb([128, 512], F32)
            ktlT, ktlTr = self.sb([128, 512], F32)
            vbT, vbTr = self.sb([128, 512], F32)
            qd, qdr = self.sb([128, 512], F32)
            glc, glcr = self.sb([128, 8], F32)
            tdt, tdtr = T3()
            DT, DTr = T3()
            DTs, DTsr = tdt, tdtr
            Bm, Bmr = T3()
            Am, Amr = T3()
            B2, B2r = T3()
            A2, A2r = T3()
            P, Pr = T3()
            Aqk, Aqkr = T3()
            kbg, kbgr = T3()
            ktl, ktlr = T3()
            vb, vbr = T3()
            u, ur = T3()
            wT, wTr = self.sb([128, 512], F32)
            vn, vnr = self.sb([128, 128], F32)
            St, Str = self.sb([128, 128], F32)
            oT, oTr = kbgT, kbgTr
            sq, sqr = self.sb([128, 512], BF16)
            tmp, tmr = ktlT, ktlTr
            rs, rsr = vbT, vbTr
            ybs = [self.sb([128, 512], BF16) for _ in range(1)]
            bcs = [[self.sb([128, 512], F32) for _ in range(5)] for _ in range(1)]
            return dict(locals())
        HBs = [alloc_head() for _ in range(3)]
        FX = self.fixed
        V4 = lambda ap: ap.rearrange("p (c l) -> p c l", l=128)

        def head(h, HB):
            ins = HB["ins"]
            bcs = HB["bcs"]
            ybs = HB["ybs"]
            kbgT = HB["kbgT"]
            kbgTr = HB["kbgTr"]
            ktlT = HB["ktlT"]
            ktlTr = HB["ktlTr"]
            vbT = HB["vbT"]
            vbTr = HB["vbTr"]
            qd = HB["qd"]
            qdr = HB["qdr"]
            glc = HB["glc"]
            glcr = HB["glcr"]
            tdt = HB["tdt"]
            tdtr = HB["tdtr"]
            DT = HB["DT"]
            DTr = HB["DTr"]
            DTs = HB["DTs"]
            DTsr = HB["DTsr"]
            Bm = HB["Bm"]
            Bmr = HB["Bmr"]
            Am = HB["Am"]
            Amr = HB["Amr"]
            B2 = HB["B2"]
            B2r = HB["B2r"]
            A2 = HB["A2"]
            A2r = HB["A2r"]
            P = HB["P"]
            Pr = HB["Pr"]
            Aqk = HB["Aqk"]
            Aqkr = HB["Aqkr"]
            kbg = HB["kbg"]
            kbgr = HB["kbgr"]
            ktl = HB["ktl"]
            ktlr = HB["ktlr"]
            vb = HB["vb"]
            vbr = HB["vbr"]
            u = HB["u"]
            ur = HB["ur"]
            wT = HB["wT"]
            wTr = HB["wTr"]
            vn = HB["vn"]
            vnr = HB["vnr"]
            St = HB["St"]
            Str = HB["Str"]
            oT = HB["oT"]
            oTr = HB["oTr"]
            sq = HB["sq"]
            sqr = HB["sqr"]
            tmp = HB["tmp"]
            tmr = HB["tmr"]
            rs = HB["rs"]
            rsr = HB["rsr"]
            self.memset("dve", St[:], 0.0, [Str])
            rows = slice(h * 128, (h + 1) * 128)
            for g in range(4):
                sl = slice(g * 512, (g + 1) * 512)
                I = ins[g % 2]
                yb, ybr = ybs[0]
                (qT, qTr), (kT, kTr), (vT, vTr), (zT, zTr) = I["q"], I["k"], I["v"], I["z"]
                self.dma("sp", qT[:], d["gcT"][h * 128:(h + 1) * 128, sl], [], [qTr])
                self.dma("sp", kT[:], d["gcT"][2048 + h * 128:2048 + (h + 1) * 128, sl], [], [kTr])
                self.dma("sp", vT[:], d["gcT"][4096 + h * 128:4096 + (h + 1) * 128, sl], [], [vTr])
                self.dma("sp", zT[:], d["gzT"][rows, sl], [], [zTr])
                bc = bcs[0]
                if g == 0:
                    for fi in range(5):
                        self.dma("sp", bc[fi][0][:], d["gG"][fi, h:h + 1, sl].broadcast_to([128, 512]), [], [bc[fi][1]])
                self.tt(tdt[:], V4(bc[0][0][:, :]), negU[:], ALU.add, [bc[0][1], negUr], [tdtr])
                for pp in range(4):
                    t_ = g * 4 + pp
                    self.act(DT[:, pp, :], tdt[:, pp, :], AF.Exp, [tdtr, ngTr], [DTr], bias=ngT[:, t_, h:h + 1])
                self.tt(DTs[:], DT[:], strU[:], ALU.mult, [DTr, strUr], [DTsr], eng="pool")
                self.tt(kbgT[:], kT[:], bc[3][0][:], ALU.mult, [kTr, bc[3][1]], [kbgTr])
                self.tt(ktlT[:], kT[:], bc[4][0][:], ALU.mult, [kTr, bc[4][1]], [ktlTr], eng="pool")
                psb, prb = bc[1]
                self.tt(vbT[:], vT[:], psb[:], ALU.mult, [vTr, prb], [vbTr])
                pk, pkr = FX(5)
                pq, pqr = FX(6)
                for pp in range(4):
                    cs = slice(pp * 128, (pp + 1) * 128)
                    self.mm(pk[:, cs], kT[:, cs], kT[:, cs], True, True, [kTr], [pkr])
                    self.mm(pq[:, cs], kT[:, cs], qT[:, cs], True, True, [kTr, qTr], [pqr])
                self.tt(Bm[:], V4(pk[:, :]), DTs[:], ALU.mult, [pkr, DTsr], [Bmr])
                self.tt(Bm[:], Bm[:], V4(psb[:, :]), ALU.mult, [Bmr, prb], [Bmr])
                self.tt(Aqk[:], V4(pq[:, :]), DT[:], ALU.mult, [pqr, DTr], [Aqkr])
                pse, pre = bc[2]
                self.tt(qd[:], qT[:], pse[:], ALU.mult, [qTr, pre], [qdr])
                self.cp("dve", glc[:], pse[:].rearrange("p (c l) -> p c l", l=64)[:, :, 63], [pre], [glcr])
                if g < 3:
                    sln = slice((g + 1) * 512, (g + 2) * 512)
                    for fi in range(5):
                        self.dma("sp", bc[fi][0][:], d["gG"][fi, h:h + 1, sln].broadcast_to([128, 512]), [], [bc[fi][1]])
                pa, par = FX(7)
                for pp in range(4):
                    self.tr(pa[:, pp * 128:(pp + 1) * 128], Bm[:, pp, :], self.ident_f[:], [Bmr, self.r_ident_f], [par])
                self.cp("act", Am[:], V4(pa[:, :]), [par], [Amr])
                self.tt(P[:], id8[:], Bm[:], ALU.subtract, [id8r, Bmr], [Pr], eng="pool")
                Ac, Acr, Bc, Bcr = Am, Amr, Bm, Bmr
                An, Anr, Bn, Bnr = A2, A2r, B2, B2r
                for lvl in range(5):
                    p1, p1r = FX(5)
                    p2, p2r = FX(6)
                    for pp in range(4):
                        cs = slice(pp * 128, (pp + 1) * 128)
                        self.mm(p1[:, cs], Bc[:, pp, :], Ac[:, pp, :], True, True, [Bcr, Acr], [p1r])
                    if lvl < 4:
                        for pp in range(4):
                            cs = slice(pp * 128, (pp + 1) * 128)
                            self.mm(p2[:, cs], Ac[:, pp, :], Bc[:, pp, :], True, True, [Bcr, Acr], [p2r])
                    self.cp("act", An[:], V4(p1[:, :]), [p1r], [Anr])
                    p3, p3r = FX(5)
                    for pp in range(4):
                        cs = slice(pp * 128, (pp + 1) * 128)
                        self.mm(p3[:, cs], An[:, pp, :], P[:, pp, :], True, True, [Anr, Pr], [p3r])
                    if lvl < 4:
                        self.cp("act", Bn[:], V4(p2[:, :]), [p2r], [Bnr])
                    self.tt(P[:], P[:], V4(p3[:, :]), ALU.add, [Pr, p3r], [Pr])
                    Ac, Acr, Bc, Bcr, An, Anr, Bn, Bnr = An, Anr, Bn, Bnr, Ac, Acr, Bc, Bcr
                for qi, (src, sr_, dst, dr_) in enumerate(((kbgT, kbgTr, kbg, kbgr), (ktlT, ktlTr, ktl, ktlr), (vbT, vbTr, vb, vbr))):
                    tp, tpr = FX(4 + qi)
                    for pp in range(4):
                        cs = slice(pp * 128, (pp + 1) * 128)
                        self.tr(tp[:, cs], src[:, cs], self.ident_f[:], [sr_, self.r_ident_f], [tpr])
                    self.cp("act" if qi == 1 else "dve", dst[:], V4(tp[:, :]), [tpr], [dr_])
                pu, pur = FX(7)
                for pp in range(4):
                    self.mm(pu[:, pp * 128:(pp + 1) * 128], P[:, pp, :], vb[:, pp, :], True, True, [Pr, vbr], [pur])
                self.cp("dve", u[:], V4(pu[:, :]), [pur], [ur])
                pw, pwr = FX(4)
                for pp in range(4):
                    self.mm(pw[:, pp * 128:(pp + 1) * 128], kbg[:, pp, :], P[:, pp, :], True, True, [kbgr, Pr], [pwr])
                self.cp("act", wT[:], pw[:, :], [pwr], [wTr])
                po, por = FX(5)
                for cc in range(8):
                    pp, hf = cc // 2, cc % 2
                    prt = slice(hf * 64, hf * 64 + 64)
                    cs = slice(cc * 64, (cc + 1) * 64)
                    p1, p1r = self.bank()
                    self.mm(p1[prt, 0:128], wT[:, cs], St[:], True, True, [wTr, Str], [p1r])
                    self.tt(vn[prt, :], u[prt, pp, :], p1[prt, 0:128], ALU.subtract, [ur, p1r], [vnr])
                    self.mm(po[:, cs], St[:], qd[:, cs], True, False, [Str, qdr], [por])
                    self.mm(po[:, cs], vn[prt, :], Aqk[prt, pp, hf * 64:hf * 64 + 64], False, True, [vnr, Aqkr], [por])
                    p2, p2r = self.bank()
                    self.mm(p2[:, 0:128], ktl[prt, pp, :], vn[prt, :], True, True, [ktlr, vnr], [p2r])
                    self.stt(St[:], St[:], glc[:, cc:cc + 1], p2[:, 0:128], ALU.mult, ALU.add, [Str, glcr, p2r], [Str])
                self.cp("dve", oT[:], po[:], [por], [oTr])
                self.act(sq[:], po[:], AF.Square, [por], [sqr])
                pn, pnr = FX(6)
                self.mm(pn[:], self.ones_b[:], sq[:], True, True, [sqr, self.r_ones_b], [pnr])
                self.rstd(rs, rsr, pn, pnr, 128, 1.0 / 128, tmp, tmr)
                self.stt(oT[:], oT[:], nw[:, 0:1], rs[:], ALU.mult, ALU.mult, [oTr, nwr, rsr], [oTr])
                self.tt(yb[:], oT[:], zT[:], ALU.mult, [oTr, zTr], [ybr], eng="pool")
                self.dma("act", d["yT"][1024 + h * 128:1024 + (h + 1) * 128, sl], yb[:], [ybr], [])

        NS = 3
        for h0 in range(0, 16, NS):
            hs = list(range(h0, min(16, h0 + NS)))
            self.S.begin_streams(len(hs))
            for si, h in enumerate(hs):
                if len(hs) == 1:
                    self.use_stream(si, [3, 2], {4: 0, 5: 1, 6: 2, 7: 3})
                else:
                    a_, b_ = 2 * si, 2 * si + 1
                    self.use_stream(si, [a_], {4: a_, 5: b_, 6: a_, 7: b_})
                head(h, HBs[si])
            self.end_streams()
        self.flush()
        self.release(m)

    def ph_merge(self, L, W):
        d = self.dr
        D = self.D
        m = self.mark()
        self.rot = [0, 1, 2, 3, 4, 5]
        yt, yr = self.load_x(d["yT"], 32)
        wb = self.make_wbufs([8, 16, 8], 2)
        gts = [[self.sb([128, 512], BF16) for _ in range(3)] for _ in range(2)]
        t1s = [self.sb([128, 512], F32) for _ in range(2)]
        t2s = [self.sb([128, 512], F32) for _ in range(2)]
        mbs = [self.sb([128, 512], BF16) for _ in range(2)]
        k = [0]
        srcs = [(yt[:, 0:8, :], yr, 8, W["w_branch_a"][L]), (yt[:, 8:24, :], yr, 16, W["w_branch_b"][L]),
                (yt[:, 24:32, :], yr, 8, W["w_branch_c"][L])]

        def epi(pss, n0, n_, tb):
            i = k[0] % 2
            k[0] += 1
            sl = slice(tb * 512, (tb + 1) * 512)
            gs = gts[i]
            for b in range(3):
                self.dma("sp", gs[b][0][:n_, :], d["gateT"][b * D + n0:b * D + n0 + n_, sl], [], [gs[b][1]])
            t1, t1r = t1s[i]
            t2, t2r = t2s[i]
            mb, mbr = mbs[i]
            self.tt(t1[:n_, :], pss[0][0][:n_, :], gs[0][0][:n_, :], ALU.mult, [pss[0][1], gs[0][1]], [t1r])
            self.tt(t2[:n_, :], pss[1][0][:n_, :], gs[1][0][:n_, :], ALU.mult, [pss[1][1], gs[1][1]], [t2r])
            self.tt(t1[:n_, :], t1[:n_, :], t2[:n_, :], ALU.add, [t1r, t2r], [t1r])
            self.tt(t2[:n_, :], pss[2][0][:n_, :], gs[2][0][:n_, :], ALU.mult, [pss[2][1], gs[2][1]], [t2r])
            self.tt(mb[:n_, :], t1[:n_, :], t2[:n_, :], ALU.add, [t1r, t2r], [mbr])
            self.dma("act", d["mT"][n0:n0 + n_, sl], mb[:n_, :], [mbr], [])
        self.multilinear(srcs, D, epi, wb)
        self.flush()
        self.rot = [0, 1, 2, 3]
        self.release(m)

    def ph_linear_simple(self, src, KC, w, ncols, dst, dt):
        m = self.mark()
        self.rot = [0, 1, 2, 3, 4, 5, 6, 7]
        xt, xr = self.load_x(src, KC)
        wb = self.make_wbufs([KC], 3)
        stg = [self.sb([128, 512], dt) for _ in range(3)]
        self.multilinear([(xt, xr, KC, w)], ncols, self.epi_store(dst, dt, stg=stg), wb)
        self.flush()
        self.rot = [0, 1, 2, 3]
        self.release(m)

    def ph_ffn(self, L, W):
        d = self.dr
        D, DFF, C, CF = self.D, self.DFF, self.C, self.CF
        m = self.mark()
        self.rot = [0, 1, 2, 3, 4, 5, 6, 7]
        xt, xr = self.load_x(d["h2T"], C)
        wb = self.make_wbufs([C, C], 3)
        sgs = [self.sb([128, 512], F32) for _ in range(2)]
        hbs = [self.sb([128, 512], BF16) for _ in range(3)]
        k = [0]

        def epi(pss, n0, n_, tb):
            sl = slice(tb * 512, (tb + 1) * 512)
            sg, sgr = sgs[k[0] % 2]
            hb, hbr = hbs[k[0] % 3]
            k[0] += 1
            self.act(sg[:n_, :], pss[0][0][:n_, :], AF.Silu, [pss[0][1]], [sgr])
            self.tt(hb[:n_, :], pss[1][0][:n_, :], sg[:n_, :], ALU.mult, [pss[1][1], sgr], [hbr])
            self.dma("sp", d["hidT"][n0:n0 + n_, sl], hb[:n_, :], [hbr], [])
        self.multilinear([(xt, xr, C, W["w_ffn_gate"][L]), (xt, xr, C, W["w_ffn_up"][L])], DFF, epi, wb)
        self.flush()
        self.release(m)
        m = self.mark()
        KP = 16
        npiece = (CF + KP - 1) // KP
        wps = [self.sb([128, KP, 512], BF16) for _ in range(4)]
        stg = [self.sb([128, 512], F32) for _ in range(3)]
        ht, hr = self.sb([128, CF, 512], BF16)
        hv = d["hidT"].rearrange("(c p) s -> p c s", p=128)
        wv = W["w_ffn_down"][L].rearrange("(c p) n -> p c n", p=128)
        wi = 0
        kk = 0
        for tb in range(4):
            sl = slice(tb * 512, (tb + 1) * 512)
            step = max(1, CF // 4)
            for c0 in range(0, CF, step):
                c1 = min(CF, c0 + step)
                self.dma("sp", ht[:, c0:c1, :], hv[:, c0:c1, sl], [], [hr])
            for n0 in range(0, D, 512):
                ncol = min(512, D - n0)
                nn_ = ncol // 128
                pss = [self.bank() for _ in range(nn_)]
                for pi in range(npiece):
                    k0 = pi * KP
                    k1 = min(CF, k0 + KP)
                    wt, wr = wps[wi % 4]
                    wi += 1
                    self.dma("pool", wt[:, 0:k1 - k0, 0:ncol], wv[:, k0:k1, n0:n0 + ncol], [], [wr])
                    for kc in range(k0, k1):
                        for nn in range(nn_):
                            self.mm(pss[nn][0][:, :], wt[:, kc - k0, nn * 128:(nn + 1) * 128], ht[:, kc, :],
                                    kc == 0, kc == CF - 1, [wr, hr], [pss[nn][1]])
                for nn in range(nn_):
                    st, sr = stg[kk % 3]
                    kk += 1
                    self.cp("act" if kk % 2 else "dve", st[:, :], pss[nn][0][:, :], [pss[nn][1]], [sr])
                    self.dma("act", d["fT"][n0 + nn * 128:n0 + (nn + 1) * 128, sl], st[:, :], [sr], [])
        self.flush()
        self.rot = [0, 1, 2, 3]
        self.release(m)

    def ph_ple(self, L, W, pT, xsrc, xdst):
        d = self.dr
        D, C = self.D, self.C
        m = self.mark()
        self.rot = [0, 1, 2, 3, 4, 5, 6, 7]
        xt, xr = self.load_x(d["xbT"], C)
        pt, pr = self.sb([128, 2, S_TOK], BF16)
        self.dma("pool", pt[:], pT[L].rearrange("(c p) s -> p c s", p=128), [], [pr])
        wb = self.make_wbufs([C, 2], 3)
        sgs = [self.sb([128, 512], F32) for _ in range(2)]
        xs = [self.sb([128, 512], F32) for _ in range(3)]
        k = [0]

        def epi(pss, n0, n_, tb):
            sl = slice(tb * 512, (tb + 1) * 512)
            sg, sgr = sgs[k[0] % 2]
            xx, xxr = xs[k[0] % 3]
            k[0] += 1
            self.dma("sp", xx[:n_, :], xsrc[n0:n0 + n_, sl], [], [xxr])
            self.act(sg[:n_, :], pss[0][0][:n_, :], AF.Sigmoid, [pss[0][1]], [sgr])
            self.tt(sg[:n_, :], pss[1][0][:n_, :], sg[:n_, :], ALU.mult, [pss[1][1], sgr], [sgr])
            self.tt(xx[:n_, :], xx[:n_, :], sg[:n_, :], ALU.add, [xxr, sgr], [xxr])
            self.dma("act", xdst[n0:n0 + n_, sl], xx[:n_, :], [xxr], [])
        self.multilinear([(xt, xr, C, W["w_ple_gate"][L]), (pt, pr, 2, W["w_ple_proj"][L])], D, epi, wb)
        self.flush()
        self.rot = [0, 1, 2, 3]
        self.release(m)


WNAMES = ["w_in", "mla_w_uq", "mla_w_ukv", "w_branch_gate", "w_branch_a", "w_branch_b", "w_branch_c",
          "w_out", "w_ffn_gate", "w_ffn_up", "w_ffn_down", "w_ple_gate", "w_ple_proj"]


def host_consts():
    S = S_TOK
    c = {}
    c["ident"] = np.eye(128, dtype=np.float32)
    causal = np.zeros((4, 128, 512), np.float32)
    for m_ in range(4):
        kk = m_ * 128 + np.arange(128)[:, None]
        qq = np.arange(512)[None, :]
        causal[m_] = np.where(kk <= qq, 0.0, NEG)
    c["causal"] = causal
    pastneg = np.zeros((128, 16, 8), np.float32)
    notown = np.ones((128, 16, 8), np.float32)
    for t in range(16):
        for n in range(8):
            if n >= t // 2:
                pastneg[:, t, n] = -1e30
            if n == t // 2:
                notown[:, t, n] = 0.0
    c["pastneg"] = pastneg
    c["notown"] = notown
    E = np.zeros((8, 8, 128), np.float32)
    for n in range(8):
        E[n, n, :] = 1.0
    c["E"] = E
    half = 32
    inv = (1.0 / (10000.0 ** (np.arange(half, dtype=np.float32) / half))).astype(np.float32)
    rc = np.zeros((64, 2), np.float32)
    rc[:, 0] = np.concatenate([inv, inv])
    rc[:, 1] = np.concatenate([-np.ones(32), np.ones(32)])
    c["ropecols"] = rc
    cm = np.ones((16, S), np.float32)
    cm[:, ::64] = 0.0
    c["cmask"] = cm
    oh = np.zeros((16, 16, 128), np.float32)
    for h in range(16):
        oh[h, h, :] = 1.0
    c["onehot16"] = oh
    a = np.arange(128)[:, None]
    b = np.arange(128)[None, :]
    same = (a // 64) == (b // 64)
    c["negU"] = np.ascontiguousarray(np.broadcast_to(np.where(same & (b >= a), 0.0, NEG)[:, None, :], (128, 4, 128))).astype(np.float32)
    c["strU"] = np.ascontiguousarray(np.broadcast_to((same & (b > a)).astype(np.float32)[:, None, :], (128, 4, 128)))
    c["id8"] = np.ascontiguousarray(np.broadcast_to((b == a).astype(np.float32)[:, None, :], (128, 4, 128)))
    return c


def build(D, DFF, DEPTH, debug=(), phases=None):
    kb = KB(D, DFF, DEPTH, debug)
    nc = kb.nc
    S = S_TOK
    C = D // 128

    def inp(name, shape, dt=F32):
        return nc.dram_tensor(name, list(shape), dt, kind="ExternalInput").ap()
    xT = inp("xT", [D, S])
    pT = inp("pT", [DEPTH, 256, S])
    pos = inp("pos", [1, S], I32)
    W = {}
    shapes = {"w_in": [D, IN_WIDTH], "mla_w_uq": [768, 1536], "mla_w_ukv": [512, 2048], "w_branch_gate": [D, 3 * D],
              "w_branch_a": [1024, D], "w_branch_b": [2048, D], "w_branch_c": [1024, D], "w_out": [D, D],
              "w_ffn_gate": [D, DFF], "w_ffn_up": [D, DFF], "w_ffn_down": [DFF, D], "w_ple_gate": [D, D],
              "w_ple_proj": [256, D]}
    for n in WNAMES:
        W[n] = inp(n, [DEPTH] + shapes[n])
    for n in ("norm_mix_in", "norm_mix_out", "norm_ffn_in", "norm_ffn_out"):
        W[n] = inp(n, [DEPTH, 128, C])
    W["mla_q_norm_w"] = inp("mla_q_norm_w", [DEPTH, 128, 6])
    W["mla_kv_norm_w"] = inp("mla_kv_norm_w", [DEPTH, 128, 4])
    W["gdn_norm_w"] = inp("gdn_norm_w", [DEPTH, 128, 1])
    W["gdn_conv_w"] = inp("gdn_conv_w", [DEPTH, 128, 48, 4])
    W["gdn_hcols"] = inp("gdn_hcols", [DEPTH, 16, 2])
    hc = host_consts()
    CN = {k: inp("c_" + k, v.shape) for k, v in hc.items()}
    outT = nc.dram_tensor("outT", [D, S], F32, kind="ExternalOutput").ap()
    dr = kb.dram
    dr("xA", [D, S], F32)
    dr("xB", [D, S], F32)
    dr("hT", [D, S], BF16)
    dr("mqT", [1024, S], BF16)
    dr("mkT", [1024, S], BF16)
    dr("mv", [S, 1024], BF16)
    dr("gqkvT", [6144, S], F32)
    dr("gcT", [6144, S], F32)
    dr("gabT", [32, S], F32)
    dr("gzT", [2048, S], BF16)
    dr("gG", [6, 16, S], F32)
    dr("cqT", [768, S], F32)
    dr("ckvT", [512, S], F32)
    dr("krT", [64, S], F32)
    dr("krsT", [64, S], F32)
    dr("gateT", [3 * D, S], BF16)
    dr("yT", [4096, S], BF16)
    dr("mT", [D, S], BF16)
    dr("oT", [D, S], F32)
    dr("h2T", [D, S], BF16)
    dr("hidT", [DFF, S], BF16)
    dr("fT", [D, S], F32)
    dr("xbT", [D, S], BF16)
    dr("ropeT", [4, 64, S], F32)
    dr("QBd", [4, S], BF16)
    dr("KBd", [8, 4, S], BF16)
    d = kb.dr
    kb.setup_consts(CN["ident"])
    kb.ph_posconst(pos, CN)
    xcur = xT
    cnt = [0]

    def go(fn, *a, **k):
        cnt[0] += 1
        if phases is None or cnt[0] <= phases:
            fn(*a, **k)
    for L in range(DEPTH):
        go(kb.ph_norm, xcur, C, W["norm_mix_in"][L], d["hT"])
        go(kb.ph_inproj, L, d["hT"], W)
        go(kb.ph_moba, CN, pre=(kb.gdn_pre_alloc, lambda A, L=L: kb.gdn_pre_emit(L, W, CN, A)))
        go(lambda: None)
        go(kb.ph_gdn, L, W, CN)
        go(kb.ph_mla, L, W, CN)
        go(kb.ph_merge, L, W)
        go(kb.ph_linear_simple, d["mT"], C, W["w_out"][L], D, d["oT"], F32)
        go(kb.ph_resnorm, xcur, d["oT"], W["norm_mix_out"][L], d["xA"], gain_n=W["norm_ffn_in"][L], ndst=d["h2T"])
        go(kb.ph_ffn, L, W)
        go(kb.ph_resnorm, d["xA"], d["fT"], W["norm_ffn_out"][L], d["xB"], bdst=d["xbT"])
        last = (L == DEPTH - 1)
        xnext = outT if last else d["xA"]
        go(kb.ph_ple, L, W, pT, d["xB"], xnext)
        xcur = xnext
    return kb


def make_inputs_for_core(b, inputs, DEPTH, consts):
    f = np.float32
    m = {}
    m["xT"] = np.ascontiguousarray(inputs["x"][b].T)
    m["pT"] = np.ascontiguousarray(np.transpose(inputs["p"][:, b], (0, 2, 1)))
    m["pos"] = np.ascontiguousarray(inputs["positions"][b][None, :]).astype(np.int32)
    for k, v in consts.items():
        m["c_" + k] = v
    return m


def shared_inputs(inputs, D):
    C = D // 128
    m = {}
    for n in WNAMES:
        m[n] = np.ascontiguousarray(inputs[n], dtype=np.float32)
    for n in ("norm_mix_in", "norm_mix_out", "norm_ffn_in", "norm_ffn_out"):
        v = np.asarray(inputs[n], np.float32)
        m[n] = np.ascontiguousarray(v.reshape(v.shape[0], C, 128).transpose(0, 2, 1))
    v = np.asarray(inputs["mla_q_norm_w"], np.float32)
    m["mla_q_norm_w"] = np.ascontiguousarray(v.reshape(-1, 6, 128).transpose(0, 2, 1))
    v = np.asarray(inputs["mla_kv_norm_w"], np.float32)
    m["mla_kv_norm_w"] = np.ascontiguousarray(v.reshape(-1, 4, 128).transpose(0, 2, 1))
    v = np.asarray(inputs["gdn_norm_w"], np.float32)
    m["gdn_norm_w"] = np.ascontiguousarray(v.reshape(-1, 128, 1))
    v = np.asarray(inputs["gdn_conv_w"], np.float32)
    m["gdn_conv_w"] = np.ascontiguousarray(v.reshape(v.shape[0], 4, 48, 128).transpose(0, 3, 2, 1))
    m["gdn_hcols"] = np.ascontiguousarray(np.stack([np.asarray(inputs["gdn_a_log"], np.float32),
                                                   np.asarray(inputs["gdn_dt_bias"], np.float32)], axis=-1))
    return m


_CACHE = {}


def run(inputs, D, DFF, DEPTH, debug=(), trace=False, phases=None):
    B = inputs["x"].shape[0]
    key = (D, DFF, DEPTH, tuple(debug))
    kb = build(D, DFF, DEPTH, debug, phases)
    consts = host_consts()
    sh = shared_inputs(inputs, D)
    in_maps = []
    for b in range(B):
        m = make_inputs_for_core(b, inputs, DEPTH, consts)
        m.update(sh)
        in_maps.append(m)
    res = run_bass_kernel_spmd(kb.nc, in_maps, core_ids=list(range(B)), trace=trace)
    out = np.stack([np.ascontiguousarray(r["outT"].T) for r in res.results], axis=0)
    return out, res


def kernel(**inputs):
    inputs = {k: np.asarray(v) for k, v in inputs.items()}
    out, _ = run(inputs, 4096, 11008, 2)
    return out.astype(np.float32)
```

```python
import math
import numpy as np
import concourse.bass as bass
import concourse.mybir as mybir
from concourse.bass_utils import run_bass_kernel_spmd

F32 = mybir.dt.float32
BF16 = mybir.dt.bfloat16
I32 = mybir.dt.int32
AF = mybir.ActivationFunctionType
ALU = mybir.AluOpType
AX = mybir.AxisListType

ENGS = ("pe", "act", "dve", "pool", "sp")
S_TOK = 2048
NEG = -30000.0


class Res:
    __slots__ = ("w", "r")

    def __init__(self):
        self.w = None
        self.r = []


class Op:
    __slots__ = ("eng", "fn", "deps", "dma", "needed", "cnt", "dsem", "dval", "done", "key")

    def __init__(self, eng, fn, dma):
        self.eng = eng
        self.fn = fn
        self.deps = []
        self.dma = dma
        self.needed = False
        self.cnt = 0
        self.dsem = None
        self.dval = 0
        self.done = False


class Sched:
    NDMA = 12

    def __init__(self, nc):
        self.nc = nc
        self.ops = {e: [] for e in ENGS}
        self.esem = {e: nc.alloc_semaphore(name=f"es_{e}") for e in ENGS}
        self.ecnt = {e: 0 for e in ENGS}
        qs = ("sp", "act", "pool")
        self.dsems = {e: [nc.alloc_semaphore(name=f"ds_{e}{i}") for i in range(self.NDMA)] for e in qs}
        self.dcnt = {e: [0] * self.NDMA for e in qs}
        self.dnext = {e: 0 for e in qs}
        self.dlast = {e: [None] * self.NDMA for e in qs}
        self.waited = {e: {} for e in ENGS}
        self.pending_dma = []
        self.nops = 0
        self.cur = None
        self.nstreams = 0
        self.sops = []
        self.sidx = []
        self.sdn = []

    def begin_streams(self, n, stagger=0.0):
        self.nstreams = n
        self.stagger = stagger
        self.sops = [{e: [] for e in ENGS} for _ in range(n)]
        self.sidx = [0] * n
        per = self.NDMA // n
        self.sslots = [list(range(i * per, (i + 1) * per)) for i in range(n)]
        self.sdn = [{q: 0 for q in ("sp", "act", "pool")} for _ in range(n)]

    def merge_streams(self):
        n = self.nstreams
        lens = [max(1, self.sidx[i]) for i in range(n)]
        for e in ENGS:
            allops = []
            for i in range(n):
                for o in self.sops[i][e]:
                    o.key = (o.key[0] / lens[i] + i * self.stagger, i)
                    allops.append(o)
            allops.sort(key=lambda o: o.key)
            self.ops[e].extend(allops)
        self.cur = None
        self.nstreams = 0
        self.sops = []

    def op(self, eng, fn, reads=(), writes=(), dma=False):
        o = Op(eng, fn, dma)
        deps = o.deps
        for r in reads:
            if r.w is not None and not r.w.done:
                deps.append(r.w)
        for w in writes:
            if w.w is not None and not w.w.done:
                deps.append(w.w)
            for x in w.r:
                if not x.done:
                    deps.append(x)
        for r in reads:
            r.r.append(o)
        for w in writes:
            w.w = o
            w.r = []
        if dma:
            q = eng
            if self.cur is None:
                i = self.dnext[q]
                self.dnext[q] = (i + 1) % self.NDMA
            else:
                sl_ = self.sslots[self.cur]
                i = sl_[self.sdn[self.cur][q] % len(sl_)]
                self.sdn[self.cur][q] += 1
            prev = self.dlast[q][i]
            if prev is not None and not prev.done:
                deps.append(prev)
            self.dlast[q][i] = o
            self.dcnt[q][i] += 16
            o.dsem = self.dsems[q][i]
            o.dval = self.dcnt[q][i]
            self.pending_dma.append(o)
        if self.cur is None:
            self.ops[eng].append(o)
        else:
            c = self.cur
            o.key = (self.sidx[c], c)
            self.sidx[c] += 1
            self.sops[c][eng].append(o)
        self.nops += 1
        return o

    def flush(self):
        nc = self.nc
        for e in ENGS:
            for o in self.ops[e]:
                for d in o.deps:
                    if d.dma:
                        continue
                    if d.eng == "pe" and o.eng == "pe" and not o.dma:
                        continue
                    d.needed = True
        for e in ENGS:
            c = self.ecnt[e]
            for o in self.ops[e]:
                if o.dma:
                    continue
                if o.needed:
                    c += 1
                    o.cnt = c
            self.ecnt[e] = c
        pend = self.pending_dma
        esem = self.esem
        with nc.Block() as block:
            for e in ENGS:
                ops = self.ops[e]
                is_last = (e == "sp")
                if not ops and not (is_last and pend):
                    continue
                waited = self.waited[e]

                def body(eng, ops=ops, e=e, waited=waited, is_last=is_last):
                    for o in ops:
                        for d in o.deps:
                            if d.dma:
                                s, v = d.dsem, d.dval
                            else:
                                if d.eng == "pe" and e == "pe" and not o.dma:
                                    continue
                                s, v = esem[d.eng], d.cnt
                            k = id(s)
                            if waited.get(k, 0) >= v:
                                continue
                            waited[k] = v
                            eng.wait_ge(s, v)
                        inst = o.fn(eng)
                        if o.dma:
                            inst.then_inc(o.dsem, 16)
                        elif o.needed:
                            inst.then_inc(esem[e], 1)
                    if is_last:
                        for o in pend:
                            k = id(o.dsem)
                            if waited.get(k, 0) >= o.dval:
                                continue
                            waited[k] = o.dval
                            eng.wait_ge(o.dsem, o.dval)

                {"pe": block.tensor, "act": block.scalar, "dve": block.vector,
                 "pool": block.gpsimd, "sp": block.sync}[e](body)
        for e in ENGS:
            for o in self.ops[e]:
                o.fn = None
                o.done = True
        self.ops = {e: [] for e in ENGS}
        self.pending_dma = []


MOBA_W = 1024
GDN_KW = 2048
GDN_VW = 2048
IN_SIZES = (1024, 1024, 1024, 2048, 2048, 2048, 16, 16, 2048, 768, 512, 64)
IN_OFF = np.concatenate([[0], np.cumsum(IN_SIZES)]).astype(int).tolist()
IN_WIDTH = IN_OFF[-1]


class KB:
    def __init__(self, D, DFF, DEPTH, debug=()):
        self.D, self.DFF, self.DEPTH = D, DFF, DEPTH
        self.C = D // 128
        self.CF = DFF // 128
        self.debug = set(debug)
        self.nc = nc = bass.Bass("TRN2", target_bir_lowering=False)
        self.S = Sched(nc)
        self.BASE = 16512
        self.TOP = 229344
        self.off = self.BASE
        self.uid = 0
        self.banks = [(nc.alloc_psum_tensor(f"psb{i}", [128, 512], F32), Res()) for i in range(8)]
        self.rot = [0, 1, 2, 3]
        self.ri = 0
        self.fxmap = {}
        self.dr = {}

    def sb(self, shape, dt):
        esz = 2 if dt == BF16 else 4
        nb = int(np.prod(shape[1:])) * esz
        nb = (nb + 63) // 64 * 64
        assert self.off + nb <= self.TOP, f"SBUF overflow {self.off + nb}"
        self.uid += 1
        t = self.nc.alloc_sbuf_tensor_at(f"sb{self.uid}", list(shape), dt, offset=self.off)
        self.off += nb
        return t, Res()

    def mark(self):
        return self.off

    def release(self, m):
        self.off = m

    def bank(self):
        i = self.rot[self.ri % len(self.rot)]
        self.ri += 1
        return self.banks[i]

    def fixed(self, i):
        return self.banks[self.fxmap.get(i, i)]

    def use_stream(self, sid, rot, fxmap=None):
        self.S.cur = sid
        self.rot = list(rot)
        self.ri = 0
        self.fxmap = dict(fxmap or {})

    def end_streams(self):
        self.S.merge_streams()
        self.rot = [0, 1, 2, 3]
        self.ri = 0
        self.fxmap = {}

    def dram(self, name, shape, dt, kind=None):
        if kind is None:
            kind = "ExternalOutput" if name in self.debug else "Internal"
        t = self.nc.dram_tensor(name, list(shape), dt, kind=kind).ap()
        self.dr[name] = t
        return t

    def mm(self, out, lhsT, rhs, start, stop, R, W):
        self.S.op("pe", lambda e: e.matmul(out, lhsT, rhs, start=start, stop=stop), R, W)

    def tr(self, out, in_, ident, R, W):
        self.S.op("pe", lambda e: e.transpose(out, in_, ident), R, W)

    def act(self, out, in_, func, R, W, scale=None, bias=None):
        kw = {}
        if scale is not None:
            kw["scale"] = scale
        if bias is not None:
            kw["bias"] = bias
        self.S.op("act", lambda e: e.activation(out=out, in_=in_, func=func, **kw), R, W)

    def tt(self, out, in0, in1, op, R, W, eng="dve"):
        self.S.op(eng, lambda e: e.tensor_tensor(out=out, in0=in0, in1=in1, op=op), R, W)

    def ts(self, out, in0, s1, op0, R, W, s2=None, op1=None, eng="dve"):
        if op1 is None:
            self.S.op(eng, lambda e: e.tensor_scalar(out=out, in0=in0, scalar1=s1, scalar2=None, op0=op0), R, W)
        else:
            self.S.op(eng, lambda e: e.tensor_scalar(out=out, in0=in0, scalar1=s1, scalar2=s2, op0=op0, op1=op1), R, W)

    def stt(self, out, in0, scalar, in1, op0, op1, R, W):
        self.S.op("dve", lambda e: e.scalar_tensor_tensor(out=out, in0=in0, scalar=scalar, in1=in1, op0=op0, op1=op1), R, W)

    def cp(self, eng, out, in_, R, W):
        if eng == "act":
            self.S.op("act", lambda e: e.activation(out=out, in_=in_, func=AF.Copy), R, W)
        else:
            self.S.op(eng, lambda e: e.tensor_copy(out=out, in_=in_), R, W)

    def recip(self, out, in_, R, W):
        self.S.op("dve", lambda e: e.reciprocal(out=out, in_=in_), R, W)

    def memset(self, eng, ap, val, W):
        self.S.op(eng, lambda e: e.memset(ap, val), (), W)

    def dma(self, q, out, in_, R, W):
        self.S.op(q, lambda e: e.dma_start(out=out, in_=in_), R, W, dma=True)

    def flush(self):
        self.S.flush()

    def setup_consts(self, c_ident):
        self.ident_f, self.r_ident_f = self.sb([128, 128], F32)
        self.ident_b, self.r_ident_b = self.sb([128, 128], BF16)
        self.ones_b, self.r_ones_b = self.sb([128, 128], BF16)
        self.cb, self.r_cb = self.sb([128, 4], F32)
        self.dma("sp", self.ident_f[:], c_ident, [], [self.r_ident_f])
        self.dma("pool", self.ident_b[:], c_ident, [], [self.r_ident_b])
        self.memset("dve", self.ones_b[:], 1.0, [self.r_ones_b])
        self.memset("dve", self.cb[:, 0:1], 1e-6, [self.r_cb])
        self.memset("dve", self.cb[:, 1:2], 1.0, [self.r_cb])
        self.memset("dve", self.cb[:, 2:3], math.pi / 2, [self.r_cb])
        self.memset("dve", self.cb[:, 3:4], 0.0, [self.r_cb])
        self.eps = self.cb[:, 0:1]
        self.persist = self.off
        self.flush()

    def rstd(self, out, ores, ps, pres, n, scale, tmp, tres):
        self.act(tmp[:n, :], ps[:n, :], AF.Sqrt, [pres, self.r_cb], [tres], scale=scale, bias=self.cb[:n, 0:1])
        self.recip(out[:n, :], tmp[:n, :], [tres], [ores])

    def ph_norm(self, src, C, gain_dram, dst, dst_dt=BF16):
        m = self.mark()
        g, gr = self.sb([128, C], F32)
        self.dma("sp", g[:], gain_dram, [], [gr])
        xts = [self.sb([128, C, 512], F32) for _ in range(2)]
        hts = [(self.sb([128, C, 512], dst_dt)[0], [Res() for _ in range(C)]) for _ in range(2)]
        sqs = [self.sb([128, 512], BF16) for _ in range(4)]
        tmp, tmr = self.sb([128, 512], F32)
        rs, rsr = self.sb([128, 512], F32)
        sv = src.rearrange("(c p) s -> p c s", p=128)
        dv = dst.rearrange("(c p) s -> p c s", p=128)
        for tb in range(4):
            xt, xr = xts[tb % 2]
            ht, hr = hts[tb % 2]
            sl = slice(tb * 512, (tb + 1) * 512)
            self.dma("sp", xt[:], sv[:, :, sl], [], [xr])
            ps, pr = self.bank()
            for c in range(C):
                sq, sr = sqs[c % 4]
                self.act(sq[:], xt[:, c, :], AF.Square, [xr], [sr])
                self.mm(ps[:], self.ones_b[:], sq[:], c == 0, c == C - 1, [sr, self.r_ones_b], [pr])
            self.rstd(rs, rsr, ps, pr, 128, 1.0 / (C * 128), tmp, tmr)
            for c in range(C):
                self.stt(ht[:, c, :], xt[:, c, :], g[:, c:c + 1], rs[:], ALU.mult, ALU.mult, [xr, gr, rsr], [hr[c]])
            self.dma("sp", dv[:, :, sl], ht[:], hr, [])
        self.flush()
        self.release(m)

    def ph_resnorm(self, xsrc, usrc, gain_u, xdst, gain_n=None, ndst=None, bdst=None):
        C = self.C
        TW = 256
        m = self.mark()
        gu, gur = self.sb([128, C], F32)
        self.dma("sp", gu[:], gain_u, [], [gur])
        if gain_n is not None:
            gn, gnr = self.sb([128, C], F32)
            self.dma("sp", gn[:], gain_n, [], [gnr])
        xts = [(self.sb([128, C, TW], F32)[0], [Res() for _ in range(C)]) for _ in range(2)]
        uts = [(self.sb([128, C, TW], F32)[0], [Res() for _ in range(C)]) for _ in range(2)]
        hts = [(self.sb([128, C, TW], BF16)[0], [Res() for _ in range(C)]) for _ in range(2)]
        sqs = [self.sb([128, TW], BF16) for _ in range(4)]
        tmps = [self.sb([128, 512], F32) for _ in range(2)]
        rss = [self.sb([128, 512], F32) for _ in range(2)]
        xv = xsrc.rearrange("(c p) s -> p c s", p=128)
        uv = usrc.rearrange("(c p) s -> p c s", p=128)
        xdv = xdst.rearrange("(c p) s -> p c s", p=128)
        for tb in range(S_TOK // TW):
            sl = slice(tb * TW, (tb + 1) * TW)
            xt, xr = xts[tb % 2]
            ut, ur = uts[tb % 2]
            ht, hr = hts[tb % 2]
            tmp, tmr = tmps[tb % 2]
            rs, rsr = rss[tb % 2]
            self.dma("sp", ut[:], uv[:, :, sl], [], ur)
            self.dma("sp", xt[:], xv[:, :, sl], [], xr)
            ps, pr = self.bank()
            for c in range(C):
                sq, sr = sqs[c % 4]
                self.act(sq[:], ut[:, c, :], AF.Square, [ur[c]], [sr])
                self.mm(ps[:, :TW], self.ones_b[:], sq[:], c == 0, c == C - 1, [sr, self.r_ones_b], [pr])
            self.act(tmp[:, :TW], ps[:, :TW], AF.Sqrt, [pr, self.r_cb], [tmr], scale=1.0 / (C * 128), bias=self.cb[:, 0:1])
            self.recip(rs[:, :TW], tmp[:, :TW], [tmr], [rsr])
            if gain_n is not None:
                ps2, pr2 = self.bank()
            for c in range(C):
                self.stt(ut[:, c, :], ut[:, c, :], gu[:, c:c + 1], rs[:, :TW], ALU.mult, ALU.mult, [ur[c], gur, rsr], [ur[c]])
                self.tt(xt[:, c, :], xt[:, c, :], ut[:, c, :], ALU.add, [xr[c], ur[c]], [xr[c]], eng="pool")
                if gain_n is not None:
                    sq, sr = sqs[c % 4]
                    self.act(sq[:], xt[:, c, :], AF.Square, [xr[c]], [sr])
                    self.mm(ps2[:, :TW], self.ones_b[:], sq[:], c == 0, c == C - 1, [sr, self.r_ones_b], [pr2])
                if bdst is not None:
                    self.cp("act", ht[:, c, :], xt[:, c, :], [xr[c]], [hr[c]])
            self.dma("sp", xdv[:, :, sl], xt[:], xr, [])
            if gain_n is not None:
                self.act(tmp[:, :TW], ps2[:, :TW], AF.Sqrt, [pr2, self.r_cb], [tmr], scale=1.0 / (C * 128), bias=self.cb[:, 0:1])
                self.recip(rs[:, :TW], tmp[:, :TW], [tmr], [rsr])
                for c in range(C):
                    self.stt(ht[:, c, :], xt[:, c, :], gn[:, c:c + 1], rs[:, :TW], ALU.mult, ALU.mult, [xr[c], gnr, rsr], [hr[c]])
                self.dma("sp", ndst.rearrange("(c p) s -> p c s", p=128)[:, :, sl], ht[:], hr, [])
            if bdst is not None:
                self.dma("sp", bdst.rearrange("(c p) s -> p c s", p=128)[:, :, sl], ht[:], hr, [])
        self.flush()
        self.release(m)

    def load_x(self, src, KC, dt=BF16):
        xt, _ = self.sb([128, KC, S_TOK], dt)
        sv = src.rearrange("(c p) s -> p c s", p=128)
        rs = [Res() for _ in range(4)]
        for q in range(4):
            sl = slice(q * 512, (q + 1) * 512)
            self.dma("sp", xt[:, :, sl], sv[:, :, sl], [], [rs[q]])
        return xt, rs

    def make_wbufs(self, KCs, n=3):
        return [[self.sb([128, kc, 128], BF16) for _ in range(n)] for kc in KCs]

    def multilinear(self, srcs, ncols_total, epi, wbufs, tbs=(0, 1, 2, 3), tw=512):
        wi = getattr(self, "_wi", 0)
        chunks = [(n0, min(128, ncols_total - n0)) for n0 in range(0, ncols_total, 128)]

        def issue(ci, wi_):
            n0, n_ = chunks[ci]
            wts = []
            for si, (xt, xres, KC, w) in enumerate(srcs):
                wt, wres = wbufs[si][wi_ % len(wbufs[si])]
                wv = w.rearrange("(c p) n -> p c n", p=128)
                self.dma("pool", wt[:, :KC, :n_], wv[:, :, n0:n0 + n_], [], [wres])
                wts.append((wt, wres))
            return wts
        nxt = issue(0, wi)
        for ci, (n0, n_) in enumerate(chunks):
            wts = nxt
            wi += 1
            if ci + 1 < len(chunks):
                nxt = issue(ci + 1, wi)
            for tb in tbs:
                pss = []
                for si, (xt, xres, KC, w) in enumerate(srcs):
                    ps, pres = self.bank()
                    wt, wres = wts[si]
                    for kc in range(KC):
                        self.mm(ps[:n_, :tw], wt[:, kc, :n_], xt[:, kc, tb * tw:(tb + 1) * tw],
                                kc == 0, kc == KC - 1, [wres, xres[tb] if isinstance(xres, list) else xres], [pres])
                    pss.append((ps, pres))
                epi(pss, n0, n_, tb)
        self._wi = wi

    def epi_store(self, dst, dt, func=None, scale=None, stg=None, row0=0):
        k = [0]

        def epi(pss, n0, n_, tb):
            ps, pres = pss[0]
            st, sr = stg[k[0] % len(stg)]
            k[0] += 1
            sl = slice(tb * 512, (tb + 1) * 512)
            if func is None and (k[0] % 2 == 0):
                if scale is None:
                    self.cp("dve", st[:n_, :], ps[:n_, :], [pres], [sr])
                else:
                    self.ts(st[:n_, :], ps[:n_, :], scale, ALU.mult, [pres], [sr])
            else:
                self.act(st[:n_, :], ps[:n_, :], func or AF.Copy, [pres], [sr], scale=scale)
            self.dma("sp", dst[row0 + n0:row0 + n0 + n_, sl], st[:n_, :], [sr], [])
        return epi

    def ph_inproj(self, L, hT, W):
        D, C = self.D, self.C
        m = self.mark()
        self.rot = [0, 1, 2, 3, 4, 5, 6, 7]
        xt, xr = self.load_x(hT, C)
        wb = self.make_wbufs([C], 3)
        stf = [self.sb([128, 512], F32) for _ in range(3)]
        stb = [self.sb([128, 512], BF16) for _ in range(3)]
        w_in = W["w_in"][L]
        d = self.dr

        def grp(gi):
            return w_in[:, IN_OFF[gi]:IN_OFF[gi + 1]]
        srcs = lambda gi: [(xt, xr, C, grp(gi))]
        self.multilinear(srcs(0), 1024, self.epi_store(d["mqT"], BF16, scale=128 ** -0.5, stg=stb), wb)
        self.multilinear(srcs(1), 1024, self.epi_store(d["mkT"], BF16, stg=stb), wb)
        self.multilinear([(xt, xr, C, w_in[:, IN_OFF[3]:IN_OFF[6]])], 6144, self.epi_store(d["gqkvT"], F32, stg=stf), wb)
        self.multilinear([(xt, xr, C, w_in[:, IN_OFF[6]:IN_OFF[8]])], 32, self.epi_store(d["gabT"], F32, stg=stf), wb)
        self.multilinear(srcs(8), 2048, self.epi_store(d["gzT"], BF16, func=AF.Silu, stg=stb), wb)
        self.multilinear(srcs(9), 768, self.epi_store(d["cqT"], F32, stg=stf), wb)
        self.multilinear(srcs(10), 512, self.epi_store(d["ckvT"], F32, stg=stf), wb)
        self.multilinear(srcs(11), 64, self.epi_store(d["krT"], F32, stg=stf), wb)
        o = IN_OFF[11]
        self.multilinear([(xt, xr, C, w_in[:, o + 32:o + 64])], 32, self.epi_store(d["krsT"], F32, stg=stf, row0=0), wb)
        self.multilinear([(xt, xr, C, w_in[:, o:o + 32])], 32, self.epi_store(d["krsT"], F32, stg=stf, row0=32), wb)
        self.multilinear([(xt, xr, C, W["w_branch_gate"][L])], 3 * D, self.epi_store(d["gateT"], BF16, func=AF.Sigmoid, stg=stb), wb)
        wt, wtr = self.sb([128, C, 512], BF16)
        wv = w_in[:, IN_OFF[2]:IN_OFF[3]].rearrange("(c p) n -> p c n", p=128)
        k = 0
        for n0 in range(0, 1024, 512):
            self.dma("pool", wt[:], wv[:, :, n0:n0 + 512], [], [wtr])
            for t in range(16):
                ps, pres = self.bank()
                for kc in range(C):
                    self.mm(ps[:], xt[:, kc, t * 128:(t + 1) * 128], wt[:, kc, :], kc == 0, kc == C - 1, [xr[t // 4], wtr], [pres])
                st, sr = stb[k % 3]
                k += 1
                self.cp("act" if k % 2 else "dve", st[:], ps[:], [pres], [sr])
                self.dma("sp", d["mv"][t * 128:(t + 1) * 128, n0:n0 + 512], st[:], [sr], [])
        self.flush()
        self.rot = [0, 1, 2, 3]
        self.release(m)

    def attn_core(self, qk_parts, extras, V, Vr, masks, mr, dst_rows, pts, ystg):
        pk = 0
        for j in range(4):
            num, numr = self.fixed(4 + 2 * (j % 2))
            den, denr = self.fixed(5 + 2 * (j % 2))
            nk = 4 * j + 4
            qs = slice(j * 512, (j + 1) * 512)
            for i in range(nk):
                ks = slice(i * 128, (i + 1) * 128)
                ps, pres = self.bank()
                lst = [(kT[:, ks], qT[:, qs], [kr, qr]) for (kT, kr, qT, qr) in qk_parts]
                lst += extras(i, j)
                if i >= 4 * j:
                    lst.append((self.ident_b[:], masks[:, i - 4 * j, :], [self.r_ident_b, mr]))
                for idx, (l, r, R) in enumerate(lst):
                    self.mm(ps[:], l, r, idx == 0, idx == len(lst) - 1, R, [pres])
                pt, ptr = pts[pk % len(pts)]
                pk += 1
                self.act(pt[:], ps[:], AF.Exp, [pres], [ptr])
                self.mm(num[:], V[:, i, :], pt[:], i == 0, i == nk - 1, [Vr, ptr], [numr])
                self.mm(den[:], self.ones_b[:], pt[:], i == 0, i == nk - 1, [self.r_ones_b, ptr], [denr])
            rd, rdr = ystg[0]
            yb, ybr = ystg[1 + (j % 2)]
            self.act(rd[:], den[:], AF.Ln, [denr], [rdr])
            self.act(rd[:], rd[:], AF.Exp, [rdr], [rdr], scale=-1.0)
            self.tt(yb[:], num[:], rd[:], ALU.mult, [numr, rdr], [ybr])
            self.dma("sp", dst_rows[:, qs], yb[:], [ybr], [])

    def ph_moba(self, CN, pre=None):
        d = self.dr
        m = self.mark()
        masks, mr = self.sb([128, 4, 512], BF16)
        self.dma("pool", masks[:], CN["causal"].rearrange("m p q -> p m q"), [], [mr])
        pastneg, pnr = self.sb([128, 16, 8], F32)
        notown, nor = self.sb([128, 16, 8], F32)
        self.dma("sp", pastneg[:], CN["pastneg"], [], [pnr])
        self.dma("sp", notown[:], CN["notown"], [], [nor])
        E, Er = self.sb([8, 8, 128], BF16)
        self.dma("pool", E[:], CN["E"], [], [Er])
        QB, QBr = self.sb([4, S_TOK], BF16)
        self.dma("sp", QB[:], d["QBd"], [], [QBr])
        def alloc_stream():
            pts = [self.sb([128, 512], BF16) for _ in range(2)]
            ystg = [self.sb([128, 512], F32)] + [self.sb([128, 512], BF16) for _ in range(2)]
            hb = []
            for _ in range(1 if pre is not None else 2):
                hb.append(dict(q=self.sb([128, S_TOK], BF16), k=self.sb([128, S_TOK], BF16),
                               v=self.sb([128, 16, 128], BF16), kb=self.sb([4, S_TOK], BF16),
                               km=self.sb([128, 8], F32), kmb=self.sb([128, 8], BF16),
                               gm=self.sb([128, 16, 8], F32), m8=self.sb([128, 16, 8], F32),
                               thr=self.sb([128, 16], F32), ns=self.sb([128, 16, 8], F32),
                               nsT=self.sb([8, S_TOK], BF16)))
            return pts, ystg, hb
        SB_ = [alloc_stream() for _ in range(2)]
        if pre is not None:
            pre_alloc = pre[0]()
        self.S.begin_streams(3 if pre is not None else 2)
        for si in range(2):
          pts, ystg, hb = SB_[si]
          b0 = 3 * si
          self.use_stream(si, [b0], {4: b0 + 1, 5: b0 + 2, 6: b0 + 1, 7: b0 + 2})
          for h in range(4 * si, 4 * si + 4):
            B = hb[h % len(hb)]
            (q, qr), (k, kr), (v, vr), (kb, kbr) = B["q"], B["k"], B["v"], B["kb"]
            rows = slice(h * 128, (h + 1) * 128)
            self.dma("sp", q[:], d["mqT"][rows, :], [], [qr])
            self.dma("sp", k[:], d["mkT"][rows, :], [], [kr])
            self.dma("sp", v[:], d["mv"].rearrange("(t p) c -> p t c", p=128)[:, :, rows], [], [vr])
            self.dma("sp", kb[:], d["KBd"][h], [], [kbr])
            km, kmr = B["km"]
            kmb, kmbr = B["kmb"]
            self.S.op("dve", lambda e, km=km, k=k: e.tensor_reduce(out=km[:], in_=k[:].rearrange("p (n b) -> p n b", b=256), axis=AX.X, op=ALU.add), [kr], [kmr])
            self.ts(kmb[:], km[:], 1.0 / 256, ALU.mult, [kmr], [kmbr])
            gps, gpr = self.bank()
            for t in range(16):
                self.mm(gps[:, t * 8:(t + 1) * 8], q[:, t * 128:(t + 1) * 128], kmb[:], True, True, [qr, kmbr], [gpr])
            gm, gmr = B["gm"]
            m8, m8r = B["m8"]
            thr, thrr = B["thr"]
            ns, nsr = B["ns"]
            self.tt(gm[:], gps[:, 0:128].rearrange("p (t n) -> p t n", n=8), pastneg[:], ALU.add, [gpr, pnr], [gmr])
            for t in range(16):
                self.S.op("dve", lambda e, m8=m8, gm=gm, t=t: e.max(m8[:, t, :], gm[:, t, :]), [gmr], [m8r])
            self.ts(thr[:], m8[:, :, 2], -1e29, ALU.max, [m8r], [thrr])
            self.tt(ns[:], gm[:], thr[:].unsqueeze(2).broadcast_to([128, 16, 8]), ALU.is_lt, [gmr, thrr], [nsr])
            self.stt(ns[:], ns[:], NEG, notown[:], ALU.mult, ALU.mult, [nsr, nor], [nsr])
            nsT, nsTr = B["nsT"]
            for g4 in range(4):
                tp, tpr = self.bank()
                for tt_ in range(4):
                    t = g4 * 4 + tt_
                    self.tr(tp[0:8, tt_ * 128:(tt_ + 1) * 128], ns[:, t, :], self.ident_f[:], [nsr, self.r_ident_f], [tpr])
                self.cp("act", nsT[:, g4 * 512:(g4 + 1) * 512], tp[0:8, :], [tpr], [nsTr])

            def extras(i, j, kb=kb, kbr=kbr, nsT=nsT, nsTr=nsTr):
                ks = slice(i * 128, (i + 1) * 128)
                qs = slice(j * 512, (j + 1) * 512)
                return [(kb[:, ks], QB[:, qs], [kbr, QBr]),
                        (E[:, i // 2, :], nsT[:, qs], [Er, nsTr])]
            self.attn_core([(k, kr, q, qr)], extras, v, vr, masks, mr, d["yT"][rows, :], pts, ystg)
        if pre is not None:
            self.use_stream(2, [6, 7], {})
            pre[1](pre_alloc)
        self.end_streams()
        self.flush()
        self.release(m)

    def ph_mla(self, L, W, CN):
        d = self.dr
        m = self.mark()
        sc = 192 ** -0.5
        masks, mr = self.sb([128, 4, 512], BF16)
        self.dma("pool", masks[:], CN["causal"].rearrange("m p q -> p m q"), [], [mr])
        cqn, cqnr = self.sb([128, 6, S_TOK], BF16)
        ckn, cknr = self.sb([128, 4, S_TOK], BF16)
        wuq, wuqr = self.sb([128, 6, 1536], BF16)
        wukv, wukvr = self.sb([128, 4, 2048], BF16)
        wsw, wswr = self.sb([128, 6, 512], BF16)
        self.dma("pool", wuq[:], W["mla_w_uq"][L].rearrange("(c p) n -> p c n", p=128), [], [wuqr])
        self.dma("pool", wukv[:], W["mla_w_ukv"][L].rearrange("(c p) n -> p c n", p=128), [], [wukvr])
        wq4 = W["mla_w_uq"][L].rearrange("(c p) (h e) -> p c h e", p=128, e=192)
        wsw4 = wsw[:].rearrange("p c (h e) -> p c h e", e=64)
        for c in range(6):
            self.dma("pool", wsw4[:, c, :, 0:32], wq4[:, c, :, 160:192], [], [wswr])
            self.dma("pool", wsw4[:, c, :, 32:64], wq4[:, c, :, 128:160], [], [wswr])
        rope, roper = self.sb([64, 4, S_TOK], F32)
        self.dma("sp", rope[:], d["ropeT"].rearrange("f p s -> p f s"), [], [roper])
        kpe, kper = self.sb([64, S_TOK], BF16)
        tmp, tmr = self.sb([128, 512], F32)
        rs, rsr = self.sb([128, 512], F32)
        m2 = self.mark()
        sqs = [self.sb([128, 512], BF16) for _ in range(3)]
        for (src, Cn, gname, dstt, dr_) in ((d["cqT"], 6, "mla_q_norm_w", cqn, cqnr), (d["ckvT"], 4, "mla_kv_norm_w", ckn, cknr)):
            g, gr = self.sb([128, Cn], F32)
            self.dma("sp", g[:], W[gname][L], [], [gr])
            xt, xr = self.sb([128, Cn, 512], F32)
            sv = src.rearrange("(c p) s -> p c s", p=128)
            for tb in range(4):
                sl = slice(tb * 512, (tb + 1) * 512)
                self.dma("sp", xt[:], sv[:, :, sl], [], [xr])
                ps, pr = self.bank()
                for c in range(Cn):
                    sq, sr = sqs[c % 3]
                    self.act(sq[:], xt[:, c, :], AF.Square, [xr], [sr])
                    self.mm(ps[:], self.ones_b[:], sq[:], c == 0, c == Cn - 1, [sr, self.r_ones_b], [pr])
                self.rstd(rs, rsr, ps, pr, 128, 1.0 / (Cn * 128), tmp, tmr)
                for c in range(Cn):
                    self.stt(dstt[:, c, sl], xt[:, c, :], g[:, c:c + 1], rs[:], ALU.mult, ALU.mult, [xr, gr, rsr], [dr_])
        kr_, krr = self.sb([64, S_TOK], F32)
        krs, krsr = self.sb([64, S_TOK], F32)
        self.dma("sp", kr_[:], d["krT"], [], [krr])
        self.dma("sp", krs[:], d["krsT"], [], [krsr])
        self.tt(kr_[:], kr_[:], rope[:, 0, :], ALU.mult, [krr, roper], [krr])
        self.tt(krs[:], krs[:], rope[:, 1, :], ALU.mult, [krsr, roper], [krsr])
        self.tt(kpe[:], kr_[:], krs[:], ALU.add, [krr, krsr], [kper])
        self.flush()
        self.release(m2)
        def alloc_stream():
            pts = [self.sb([128, 512], BF16) for _ in range(2)]
            ystg = [self.sb([128, 512], F32)] + [self.sb([128, 512], BF16) for _ in range(2)]
            t1 = self.sb([64, 512], F32)
            t2 = self.sb([64, 512], F32)
            hb = [dict(qn=self.sb([128, S_TOK], BF16), qpe=self.sb([64, S_TOK], BF16),
                       kn=self.sb([128, S_TOK], BF16), v=self.sb([128, 16, 128], BF16)) for _ in range(1)]
            return pts, ystg, t1, t2, hb
        SB_ = [alloc_stream() for _ in range(2)]
        self.S.begin_streams(2)
        for si in range(2):
          pts, ystg, (t1, t1r), (t2, t2r), hb = SB_[si]
          b0 = 4 * si
          self.use_stream(si, [b0, b0 + 1], {4: b0 + 2, 5: b0 + 3, 6: b0 + 2, 7: b0 + 3})
          for h in range(4 * si, 4 * si + 4):
            B = hb[0]
            (qn, qnr), (qpe, qper), (kn, knr), (v, vr) = B["qn"], B["qpe"], B["kn"], B["v"]
            for tb in range(4):
                sl = slice(tb * 512, (tb + 1) * 512)
                ps, pr = self.bank()
                for c in range(6):
                    self.mm(ps[:], wuq[:, c, h * 192:h * 192 + 128], cqn[:, c, sl], c == 0, c == 5, [wuqr, cqnr], [pr])
                self.act(qn[:, sl], ps[:], AF.Copy, [pr], [qnr], scale=sc)
                ps, pr = self.bank()
                for c in range(4):
                    self.mm(ps[:], wukv[:, c, h * 256:h * 256 + 128], ckn[:, c, sl], c == 0, c == 3, [wukvr, cknr], [pr])
                self.cp("dve", kn[:, sl], ps[:], [pr], [knr])
                ps, pr = self.bank()
                for c in range(6):
                    self.mm(ps[0:64, :], wuq[:, c, h * 192 + 128:h * 192 + 192], cqn[:, c, sl], c == 0, c == 5, [wuqr, cqnr], [pr])
                ps2, pr2 = self.bank()
                for c in range(6):
                    self.mm(ps2[0:64, :], wsw[:, c, h * 64:(h + 1) * 64], cqn[:, c, sl], c == 0, c == 5, [wswr, cqnr], [pr2])
                self.tt(t1[:], ps[0:64, :], rope[:, 2, sl], ALU.mult, [pr, roper], [t1r])
                self.tt(t2[:], ps2[0:64, :], rope[:, 3, sl], ALU.mult, [pr2, roper], [t2r])
                self.tt(qpe[:, sl], t1[:], t2[:], ALU.add, [t1r, t2r], [qper])
            for t in range(16):
                ps, pr = self.bank()
                for c in range(4):
                    self.mm(ps[:, 0:128], ckn[:, c, t * 128:(t + 1) * 128], wukv[:, c, h * 256 + 128:h * 256 + 256], c == 0, c == 3, [cknr, wukvr], [pr])
                self.cp("act" if t % 2 else "dve", v[:, t, :], ps[:, 0:128], [pr], [vr])
            self.attn_core([(kn, knr, qn, qnr), (kpe, kper, qpe, qper)], lambda i, j: [], v, vr, masks, mr,
                           d["yT"][3072 + h * 128:3072 + (h + 1) * 128, :], pts, ystg)
        self.end_streams()
        self.flush()
        self.release(m)

    def ph_posconst(self, pos, CN):
        d = self.dr
        m = self.mark()
        pi_, pir = self.sb([64, S_TOK], I32)
        self.dma("sp", pi_[:], pos.broadcast_to([64, S_TOK]), [], [pir])
        pf, pfr = self.sb([64, S_TOK], F32)
        self.cp("dve", pf[:], pi_[:], [pir], [pfr])
        cols, colr = self.sb([64, 2], F32)
        self.dma("sp", cols[:], CN["ropecols"], [], [colr])
        a, ar = self.sb([64, S_TOK], F32)
        k_, kr = self.sb([64, S_TOK], F32)
        r_, rr = self.sb([64, S_TOK], F32)
        o, orr = self.sb([64, 4, S_TOK], F32)
        MAG = 12582912.0
        self.ts(a[:], pf[:], cols[:, 0:1], ALU.mult, [pfr, colr], [ar])
        self.ts(k_[:], a[:], 1.0 / (2 * math.pi), ALU.mult, [ar], [kr], s2=MAG, op1=ALU.add)
        self.ts(k_[:], k_[:], -MAG, ALU.add, [kr], [kr])
        C1 = 6.28125
        C2 = 2 * math.pi - C1
        self.stt(r_[:], k_[:], -C1, a[:], ALU.mult, ALU.add, [kr, ar], [rr])
        self.stt(r_[:], k_[:], -C2, r_[:], ALU.mult, ALU.add, [kr, rr], [rr])
        PI_ = 3.1415925
        self.ts(r_[:], r_[:], PI_, ALU.min, [rr], [rr], s2=-PI_, op1=ALU.max)
        self.act(o[:, 1, :], r_[:], AF.Sin, [rr], [orr])
        self.ts(a[:], r_[:], -1.0, ALU.mult, [rr], [ar])
        self.tt(a[:], a[:], r_[:], ALU.min, [ar, rr], [ar])
        self.ts(a[:], a[:], math.pi / 2, ALU.add, [ar], [ar], s2=1.5707963, op1=ALU.min)
        self.act(o[:, 0, :], a[:], AF.Sin, [ar], [orr])
        self.ts(o[:, 1, :], o[:, 1, :], cols[:, 1:2], ALU.mult, [orr, colr], [orr])
        sc = 192 ** -0.5
        self.ts(o[:, 2, :], o[:, 0, :], sc, ALU.mult, [orr], [orr])
        self.ts(o[:, 3, :], o[:, 1, :], sc, ALU.mult, [orr], [orr])
        self.dma("sp", d["ropeT"].rearrange("f p s -> p f s"), o[:], [orr], [])
        hf, hfr = self.sb([1, S_TOK], F32)
        lf, lfr = self.sb([1, S_TOK], F32)
        self.ts(hf[:], pf[0:1, :], 1.0 / 128, ALU.mult, [pfr], [hfr], s2=-127.0 / 256, op1=ALU.add)
        self.ts(hf[:], hf[:], MAG, ALU.add, [hfr], [hfr])
        self.ts(hf[:], hf[:], -MAG, ALU.add, [hfr], [hfr])
        self.stt(lf[:], hf[:], -128.0, pf[0:1, :], ALU.mult, ALU.add, [hfr, pfr], [lfr])
        rows = [self.sb([1, S_TOK], BF16) for _ in range(4)]
        rb, rbr = rows[0]
        self.cp("dve", rb[:], hf[:], [hfr], [rbr])
        self.dma("sp", d["QBd"][0:1, :], rb[:], [rbr], [])
        rb, rbr = rows[1]
        self.cp("dve", rb[:], lf[:], [lfr], [rbr])
        self.dma("sp", d["QBd"][1:2, :], rb[:], [rbr], [])
        rb, rbr = rows[2]
        self.memset("dve", rb[:], 1.0, [rbr])
        self.dma("sp", d["QBd"][2:3, :], rb[:], [rbr], [])
        self.dma("sp", d["QBd"][3:4, :], rb[:], [rbr], [])
        k = 0
        for h in range(8):
            s = 2.0 ** -(h + 1)
            for ri, (src, sr_, val) in enumerate(((None, None, -128 * s), (None, None, -s), (hf, hfr, 128 * s), (lf, lfr, s))):
                rb, rbr = rows[k % 4]
                k += 1
                if src is None:
                    self.memset("dve", rb[:], val, [rbr])
                else:
                    self.ts(rb[:], src[:], val, ALU.mult, [sr_], [rbr])
                self.dma("sp", d["KBd"][h, ri:ri + 1, :], rb[:], [rbr], [])
        self.flush()
        self.release(m)

    def gdn_pre_alloc(self):
        A = {}
        A["cw"] = self.sb([128, 48, 4], F32)
        A["xps"] = [self.sb([128, S_TOK + 3], F32) for _ in range(2)]
        A["accs"] = [self.sb([128, S_TOK], F32) for _ in range(2)]
        A["sqs"] = [self.sb([128, 512], BF16) for _ in range(2)]
        A["lns"] = [self.sb([128, 512], F32) for _ in range(2)]
        A["rss"] = [self.sb([128, 512], F32) for _ in range(2)]
        A["a"] = self.sb([16, S_TOK], F32)
        A["b"] = self.sb([16, S_TOK], F32)
        A["hc"] = self.sb([16, 2], F32)
        A["cm"] = self.sb([16, S_TOK], F32)
        A["G"] = self.sb([16, 6, S_TOK], F32)
        A["x"] = self.sb([16, S_TOK], F32)
        A["y"] = self.sb([16, S_TOK], F32)
        A["nA"] = self.sb([16, 1], F32)
        return A

    def gdn_pre_emit(self, L, W, CN, A):
        d = self.dr
        cw, cwr = A["cw"]
        self.dma("sp", cw[:], W["gdn_conv_w"][L], [], [cwr])
        xps, accs, sqs, lns, rss = A["xps"], A["accs"], A["sqs"], A["lns"], A["rss"]
        for (xp, xpr) in xps:
            self.memset("dve", xp[:, 0:3], 0.0, [xpr])
        kk = 0
        for c in range(48):
            xp, xpr = xps[c % 2]
            acc, accr = accs[c % 2]
            self.dma("sp", xp[:, 3:], d["gqkvT"][c * 128:(c + 1) * 128, :], [], [xpr])
            self.ts(acc[:], xp[:, 0:S_TOK], cw[:, c, 0:1], ALU.mult, [xpr, cwr], [accr])
            for j in range(1, 4):
                self.stt(acc[:], xp[:, j:j + S_TOK], cw[:, c, j:j + 1], acc[:], ALU.mult, ALU.add, [xpr, cwr, accr], [accr])
            self.act(acc[:], acc[:], AF.Silu, [accr], [accr])
            if c < 32:
                for tb in range(4):
                    sl = slice(tb * 512, (tb + 1) * 512)
                    sq, sr = sqs[kk % 2]
                    ln, lnr = lns[kk % 2]
                    rs, rsr = rss[kk % 2]
                    kk += 1
                    ps, pr = self.bank()
                    self.act(sq[:], acc[:, sl], AF.Square, [accr], [sr])
                    self.mm(ps[:], self.ones_b[:], sq[:], True, True, [sr, self.r_ones_b], [pr])
                    self.act(ln[:], ps[:], AF.Ln, [pr, self.r_cb], [lnr], bias=self.cb[:, 0:1])
                    self.act(rs[:], ln[:], AF.Exp, [lnr], [rsr], scale=-0.5)
                    if c < 16:
                        self.stt(acc[:, sl], acc[:, sl], 128 ** -0.5, rs[:], ALU.mult, ALU.mult, [accr, rsr], [accr])
                    else:
                        self.tt(acc[:, sl], acc[:, sl], rs[:], ALU.mult, [accr, rsr], [accr])
            self.dma("act", d["gcT"][c * 128:(c + 1) * 128, :], acc[:], [accr], [])
        (a_, ar), (b_, br), (hc, hcr), (cm, cmr), (G, Gr), (x_, xr), (y_, yr), (nA, nAr) = \
            A["a"], A["b"], A["hc"], A["cm"], A["G"], A["x"], A["y"], A["nA"]
        self.dma("sp", a_[:], d["gabT"][0:16, :], [], [ar])
        self.dma("sp", b_[:], d["gabT"][16:32, :], [], [br])
        self.dma("sp", hc[:], W["gdn_hcols"][L], [], [hcr])
        self.dma("sp", cm[:], CN["cmask"], [], [cmr])
        self.act(nA[:], hc[:, 0:1], AF.Exp, [hcr], [nAr])
        self.ts(nA[:], nA[:], -1.0, ALU.mult, [nAr], [nAr])
        self.ts(x_[:], a_[:], hc[:, 1:2], ALU.add, [ar, hcr], [xr])
        self.ts(y_[:], x_[:], -1.0, ALU.mult, [xr], [yr])
        self.tt(y_[:], y_[:], x_[:], ALU.max, [yr, xr], [yr])
        self.act(y_[:], y_[:], AF.Exp, [yr], [yr], scale=-1.0)
        self.act(y_[:], y_[:], AF.Ln, [yr, self.r_cb], [yr], bias=self.cb[0:16, 1:2])
        self.ts(x_[:], x_[:], 0.0, ALU.max, [xr], [xr])
        self.tt(x_[:], x_[:], y_[:], ALU.add, [xr, yr], [xr])
        self.ts(x_[:], x_[:], nA[:, 0:1], ALU.mult, [xr, nAr], [xr])
        self.S.op("dve", lambda e: e.tensor_tensor_scan(out=G[:, 0, :], data0=cm[:], data1=x_[:], initial=0.0, op0=ALU.mult, op1=ALU.add), [cmr, xr], [Gr])
        self.act(G[:, 1, :], b_[:], AF.Sigmoid, [br], [Gr])
        self.act(G[:, 2, :], G[:, 0, :], AF.Exp, [Gr], [Gr])
        self.tt(G[:, 3, :], G[:, 1, :], G[:, 2, :], ALU.mult, [Gr], [Gr])
        gc3 = G[:, 0, :].rearrange("p (n l) -> p n l", l=64)
        self.tt(y_[:].rearrange("p (n l) -> p n l", l=64), gc3[:, :, 63:64].broadcast_to([16, 32, 64]), gc3, ALU.subtract, [Gr], [yr])
        self.act(G[:, 4, :], y_[:], AF.Exp, [yr], [Gr])
        self.ts(G[:, 5, :], G[:, 0, :], -1.0, ALU.mult, [Gr], [Gr])
        self.dma("sp", d["gG"].rearrange("f h s -> h f s"), G[:], [Gr], [])

    def ph_gdn_pre(self, L, W, CN):
        m = self.mark()
        A = self.gdn_pre_alloc()
        self.gdn_pre_emit(L, W, CN, A)
        self.flush()
        self.release(m)

    def ph_gdn(self, L, W, CN):
        d = self.dr
        m = self.mark()
        G5, Gr = self.sb([16, S_TOK], F32)
        self.dma("sp", G5[:], d["gG"][5], [], [Gr])
        negU, negUr = self.sb([128, 4, 128], F32)
        strU, strUr = self.sb([128, 4, 128], F32)
        id8, id8r = self.sb([128, 4, 128], F32)
        self.dma("sp", negU[:], CN["negU"], [], [negUr])
        self.dma("sp", strU[:], CN["strU"], [], [strUr])
        self.dma("sp", id8[:], CN["id8"], [], [id8r])
        nw, nwr = self.sb([128, 1], F32)
        self.dma("sp", nw[:], W["gdn_norm_w"][L], [], [nwr])
        ngT, ngTr = self.sb([128, 16, 16], F32)
        for g8 in range(2):
            tp, tpr = self.bank()
            for cc in range(8):
                c = g8 * 8 + cc
                self.tr(tp[:, cc * 16:(cc + 1) * 16], G5[:, c * 128:(c + 1) * 128], self.ident_f[0:16, 0:16], [Gr, self.r_ident_f], [tpr])
            self.cp("dve", ngT[:, g8 * 8:(g8 + 1) * 8, :].rearrange("p c h -> p (c h)"), tp[:, 0:128], [tpr], [ngTr])

        def alloc_head():
            def T3():
                return self.sb([128, 4, 128], F32)
            ins = [dict(q=self.sb([128, 512], F32), k=self.sb([128, 512], F32), v=self.sb([128, 512], F32),
                        z=self.sb([128, 512], BF16)) for _ in range(2)]
            kbgT, kbgTr = self.sb([128, 512], F32)
            ktlT, ktlTr = self.sb([128, 512], F32)
            vbT, vbTr = self.sb([128, 512], F32)
            qd, qdr = self.sb([128, 512], F32)
            glc, glcr = self.sb([128, 8], F32)
            tdt, tdtr = T3()
            DT, DTr = T3()
            DTs, DTsr = tdt, tdtr
            Bm, Bmr = T3()
            Am, Amr = T3()
            B2, B2r = T3()
            A2, A2r = T3()
            P, Pr = T3()
            Aqk, Aqkr = T3()
            kbg, kbgr = T3()
            ktl, ktlr = T3()
            vb, vbr = T3()
            u, ur = T3()
            wT, wTr = self.sb([128, 512], F32)
            vn, vnr = self.sb([128, 128], F32)
            St, Str = self.sb([128, 128], F32)
            oT, oTr = kbgT, kbgTr
            sq, sqr = self.sb([128, 512], BF16)
            tmp, tmr = ktlT, ktlTr
            rs, rsr = vbT, vbTr
            ybs = [self.sb([128, 512], BF16) for _ in range(1)]
            bcs = [[self.sb([128, 512], F32) for _ in range(5)] for _ in range(1)]
            return dict(locals())
        HBs = [alloc_head() for _ in range(3)]
        FX = self.fixed
        V4 = lambda ap: ap.rearrange("p (c l) -> p c l", l=128)

        def head(h, HB):
            ins = HB["ins"]
            bcs = HB["bcs"]
            ybs = HB["ybs"]
            kbgT = HB["kbgT"]
            kbgTr = HB["kbgTr"]
            ktlT = HB["ktlT"]
            ktlTr = HB["ktlTr"]
            vbT = HB["vbT"]
            vbTr = HB["vbTr"]
            qd = HB["qd"]
            qdr = HB["qdr"]
            glc = HB["glc"]
            glcr = HB["glcr"]
            tdt = HB["tdt"]
            tdtr = HB["tdtr"]
            DT = HB["DT"]
            DTr = HB["DTr"]
            DTs = HB["DTs"]
            DTsr = HB["DTsr"]
            Bm = HB["Bm"]
            Bmr = HB["Bmr"]
            Am = HB["Am"]
            Amr = HB["Amr"]
            B2 = HB["B2"]
            B2r = HB["B2r"]
            A2 = HB["A2"]
            A2r = HB["A2r"]
            P = HB["P"]
            Pr = HB["Pr"]
            Aqk = HB["Aqk"]
            Aqkr = HB["Aqkr"]
            kbg = HB["kbg"]
            kbgr = HB["kbgr"]
            ktl = HB["ktl"]
            ktlr = HB["ktlr"]
            vb = HB["vb"]
            vbr = HB["vbr"]
            u = HB["u"]
            ur = HB["ur"]
            wT = HB["wT"]
            wTr = HB["wTr"]
            vn = HB["vn"]
            vnr = HB["vnr"]
            St = HB["St"]
            Str = HB["Str"]
            oT = HB["oT"]
            oTr = HB["oTr"]
            sq = HB["sq"]
            sqr = HB["sqr"]
            tmp = HB["tmp"]
            tmr = HB["tmr"]
            rs = HB["rs"]
            rsr = HB["rsr"]
            self.memset("dve", St[:], 0.0, [Str])
            rows = slice(h * 128, (h + 1) * 128)
            for g in range(4):
                sl = slice(g * 512, (g + 1) * 512)
                I = ins[g % 2]
                yb, ybr = ybs[0]
                (qT, qTr), (kT, kTr), (vT, vTr), (zT, zTr) = I["q"], I["k"], I["v"], I["z"]
                self.dma("sp", qT[:], d["gcT"][h * 128:(h + 1) * 128, sl], [], [qTr])
                self.dma("sp", kT[:], d["gcT"][2048 + h * 128:2048 + (h + 1) * 128, sl], [], [kTr])
                self.dma("sp", vT[:], d["gcT"][4096 + h * 128:4096 + (h + 1) * 128, sl], [], [vTr])
                self.dma("sp", zT[:], d["gzT"][rows, sl], [], [zTr])
                bc = bcs[0]
                if g == 0:
                    for fi in range(5):
                        self.dma("sp", bc[fi][0][:], d["gG"][fi, h:h + 1, sl].broadcast_to([128, 512]), [], [bc[fi][1]])
                self.tt(tdt[:], V4(bc[0][0][:, :]), negU[:], ALU.add, [bc[0][1], negUr], [tdtr])
                for pp in range(4):
                    t_ = g * 4 + pp
                    self.act(DT[:, pp, :], tdt[:, pp, :], AF.Exp, [tdtr, ngTr], [DTr], bias=ngT[:, t_, h:h + 1])
                self.tt(DTs[:], DT[:], strU[:], ALU.mult, [DTr, strUr], [DTsr], eng="pool")
                self.tt(kbgT[:], kT[:], bc[3][0][:], ALU.mult, [kTr, bc[3][1]], [kbgTr])
                self.tt(ktlT[:], kT[:], bc[4][0][:], ALU.mult, [kTr, bc[4][1]], [ktlTr], eng="pool")
                psb, prb = bc[1]
                self.tt(vbT[:], vT[:], psb[:], ALU.mult, [vTr, prb], [vbTr])
                pk, pkr = FX(5)
                pq, pqr = FX(6)
                for pp in range(4):
                    cs = slice(pp * 128, (pp + 1) * 128)
                    self.mm(pk[:, cs], kT[:, cs], kT[:, cs], True, True, [kTr], [pkr])
                    self.mm(pq[:, cs], kT[:, cs], qT[:, cs], True, True, [kTr, qTr], [pqr])
                self.tt(Bm[:], V4(pk[:, :]), DTs[:], ALU.mult, [pkr, DTsr], [Bmr])
                self.tt(Bm[:], Bm[:], V4(psb[:, :]), ALU.mult, [Bmr, prb], [Bmr])
                self.tt(Aqk[:], V4(pq[:, :]), DT[:], ALU.mult, [pqr, DTr], [Aqkr])
                pse, pre = bc[2]
                self.tt(qd[:], qT[:], pse[:], ALU.mult, [qTr, pre], [qdr])
                self.cp("dve", glc[:], pse[:].rearrange("p (c l) -> p c l", l=64)[:, :, 63], [pre], [glcr])
                if g < 3:
                    sln = slice((g + 1) * 512, (g + 2) * 512)
                    for fi in range(5):
                        self.dma("sp", bc[fi][0][:], d["gG"][fi, h:h + 1, sln].broadcast_to([128, 512]), [], [bc[fi][1]])
                pa, par = FX(7)
                for pp in range(4):
                    self.tr(pa[:, pp * 128:(pp + 1) * 128], Bm[:, pp, :], self.ident_f[:], [Bmr, self.r_ident_f], [par])
                self.cp("act", Am[:], V4(pa[:, :]), [par], [Amr])
                self.tt(P[:], id8[:], Bm[:], ALU.subtract, [id8r, Bmr], [Pr], eng="pool")
                Ac, Acr, Bc, Bcr = Am, Amr, Bm, Bmr
                An, Anr, Bn, Bnr = A2, A2r, B2, B2r
                for lvl in range(5):
                    p1, p1r = FX(5)
                    p2, p2r = FX(6)
                    for pp in range(4):
                        cs = slice(pp * 128, (pp + 1) * 128)
                        self.mm(p1[:, cs], Bc[:, pp, :], Ac[:, pp, :], True, True, [Bcr, Acr], [p1r])
                    if lvl < 4:
                        for pp in range(4):
                            cs = slice(pp * 128, (pp + 1) * 128)
                            self.mm(p2[:, cs], Ac[:, pp, :], Bc[:, pp, :], True, True, [Bcr, Acr], [p2r])
                    self.cp("act", An[:], V4(p1[:, :]), [p1r], [Anr])
                    p3, p3r = FX(5)
                    for pp in range(4):
                        cs = slice(pp * 128, (pp + 1) * 128)
                        self.mm(p3[:, cs], An[:, pp, :], P[:, pp, :], True, True, [Anr, Pr], [p3r])
                    if lvl < 4:
                        self.cp("act", Bn[:], V4(p2[:, :]), [p2r], [Bnr])
                    self.tt(P[:], P[:], V4(p3[:, :]), ALU.add, [Pr, p3r], [Pr])
                    Ac, Acr, Bc, Bcr, An, Anr, Bn, Bnr = An, Anr, Bn, Bnr, Ac, Acr, Bc, Bcr
                for qi, (src, sr_, dst, dr_) in enumerate(((kbgT, kbgTr, kbg, kbgr), (ktlT, ktlTr, ktl, ktlr), (vbT, vbTr, vb, vbr))):
                    tp, tpr = FX(4 + qi)
                    for pp in range(4):
                        cs = slice(pp * 128, (pp + 1) * 128)
                        self.tr(tp[:, cs], src[:, cs], self.ident_f[:], [sr_, self.r_ident_f], [tpr])
                    self.cp("act" if qi == 1 else "dve", dst[:], V4(tp[:, :]), [tpr], [dr_])
                pu, pur = FX(7)
                for pp in range(4):
                    self.mm(pu[:, pp * 128:(pp + 1) * 128], P[:, pp, :], vb[:, pp, :], True, True, [Pr, vbr], [pur])
                self.cp("dve", u[:], V4(pu[:, :]), [pur], [ur])
                pw, pwr = FX(4)
                for pp in range(4):
                    self.mm(pw[:, pp * 128:(pp + 1) * 128], kbg[:, pp, :], P[:, pp, :], True, True, [kbgr, Pr], [pwr])
                self.cp("act", wT[:], pw[:, :], [pwr], [wTr])
                po, por = FX(5)
                for cc in range(8):
                    pp, hf = cc // 2, cc % 2
                    prt = slice(hf * 64, hf * 64 + 64)
                    cs = slice(cc * 64, (cc + 1) * 64)
                    p1, p1r = self.bank()
                    self.mm(p1[prt, 0:128], wT[:, cs], St[:], True, True, [wTr, Str], [p1r])
                    self.tt(vn[prt, :], u[prt, pp, :], p1[prt, 0:128], ALU.subtract, [ur, p1r], [vnr])
                    self.mm(po[:, cs], St[:], qd[:, cs], True, False, [Str, qdr], [por])
                    self.mm(po[:, cs], vn[prt, :], Aqk[prt, pp, hf * 64:hf * 64 + 64], False, True, [vnr, Aqkr], [por])
                    p2, p2r = self.bank()
                    self.mm(p2[:, 0:128], ktl[prt, pp, :], vn[prt, :], True, True, [ktlr, vnr], [p2r])
                    self.stt(St[:], St[:], glc[:, cc:cc + 1], p2[:, 0:128], ALU.mult, ALU.add, [Str, glcr, p2r], [Str])
                self.cp("dve", oT[:], po[:], [por], [oTr])
                self.act(sq[:], po[:], AF.Square, [por], [sqr])
                pn, pnr = FX(6)
                self.mm(pn[:], self.ones_b[:], sq[:], True, True, [sqr, self.r_ones_b], [pnr])
                self.act(tmp[:], pn[:], AF.Ln, [pnr, self.r_cb], [tmr], scale=1.0 / 128, bias=self.cb[:, 0:1])
                self.act(rs[:], tmp[:], AF.Exp, [tmr], [rsr], scale=-0.5)
                self.stt(oT[:], oT[:], nw[:, 0:1], rs[:], ALU.mult, ALU.mult, [oTr, nwr, rsr], [oTr])
                self.tt(yb[:], oT[:], zT[:], ALU.mult, [oTr, zTr], [ybr], eng="pool")
                self.dma("act", d["yT"][1024 + h * 128:1024 + (h + 1) * 128, sl], yb[:], [ybr], [])

        NS = 3
        for h0 in range(0, 16, NS):
            hs = list(range(h0, min(16, h0 + NS)))
            self.S.begin_streams(len(hs))
            for si, h in enumerate(hs):
                if len(hs) == 1:
                    self.use_stream(si, [3, 2], {4: 0, 5: 1, 6: 2, 7: 3})
                else:
                    a_, b_ = 2 * si, 2 * si + 1
                    self.use_stream(si, [a_], {4: a_, 5: b_, 6: a_, 7: b_})
                head(h, HBs[si])
            self.end_streams()
        self.flush()
        self.release(m)

    def ph_merge(self, L, W):
        d = self.dr
        D = self.D
        m = self.mark()
        self.rot = [0, 1, 2, 3, 4, 5]
        yt, yr = self.load_x(d["yT"], 32)
        wb = self.make_wbufs([8, 16, 8], 2)
        gts = [[self.sb([128, 512], BF16) for _ in range(3)] for _ in range(2)]
        t1s = [self.sb([128, 512], F32) for _ in range(2)]
        t2s = [self.sb([128, 512], F32) for _ in range(2)]
        mbs = [self.sb([128, 512], BF16) for _ in range(2)]
        k = [0]
        srcs = [(yt[:, 0:8, :], yr, 8, W["w_branch_a"][L]), (yt[:, 8:24, :], yr, 16, W["w_branch_b"][L]),
                (yt[:, 24:32, :], yr, 8, W["w_branch_c"][L])]

        def epi(pss, n0, n_, tb):
            i = k[0] % 2
            k[0] += 1
            sl = slice(tb * 512, (tb + 1) * 512)
            gs = gts[i]
            for b in range(3):
                self.dma("sp", gs[b][0][:n_, :], d["gateT"][b * D + n0:b * D + n0 + n_, sl], [], [gs[b][1]])
            t1, t1r = t1s[i]
            t2, t2r = t2s[i]
            mb, mbr = mbs[i]
            self.tt(t1[:n_, :], pss[0][0][:n_, :], gs[0][0][:n_, :], ALU.mult, [pss[0][1], gs[0][1]], [t1r])
            self.tt(t2[:n_, :], pss[1][0][:n_, :], gs[1][0][:n_, :], ALU.mult, [pss[1][1], gs[1][1]], [t2r])
            self.tt(t1[:n_, :], t1[:n_, :], t2[:n_, :], ALU.add, [t1r, t2r], [t1r])
            self.tt(t2[:n_, :], pss[2][0][:n_, :], gs[2][0][:n_, :], ALU.mult, [pss[2][1], gs[2][1]], [t2r])
            self.tt(mb[:n_, :], t1[:n_, :], t2[:n_, :], ALU.add, [t1r, t2r], [mbr])
            self.dma("act", d["mT"][n0:n0 + n_, sl], mb[:n_, :], [mbr], [])
        self.multilinear(srcs, D, epi, wb)
        self.flush()
        self.rot = [0, 1, 2, 3]
        self.release(m)

    def ph_linear_simple(self, src, KC, w, ncols, dst, dt):
        m = self.mark()
        self.rot = [0, 1, 2, 3, 4, 5, 6, 7]
        xt, xr = self.load_x(src, KC)
        wb = self.make_wbufs([KC], 3)
        stg = [self.sb([128, 512], dt) for _ in range(3)]
        self.multilinear([(xt, xr, KC, w)], ncols, self.epi_store(dst, dt, stg=stg), wb)
        self.flush()
        self.rot = [0, 1, 2, 3]
        self.release(m)

    def ph_ffn(self, L, W):
        d = self.dr
        D, DFF, C, CF = self.D, self.DFF, self.C, self.CF
        m = self.mark()
        self.rot = [0, 1, 2, 3, 4, 5, 6, 7]
        xt, xr = self.load_x(d["h2T"], C)
        wb = self.make_wbufs([C, C], 3)
        sgs = [self.sb([128, 512], F32) for _ in range(2)]
        hbs = [self.sb([128, 512], BF16) for _ in range(3)]
        k = [0]

        def epi(pss, n0, n_, tb):
            sl = slice(tb * 512, (tb + 1) * 512)
            sg, sgr = sgs[k[0] % 2]
            hb, hbr = hbs[k[0] % 3]
            k[0] += 1
            self.act(sg[:n_, :], pss[0][0][:n_, :], AF.Silu, [pss[0][1]], [sgr])
            self.tt(hb[:n_, :], pss[1][0][:n_, :], sg[:n_, :], ALU.mult, [pss[1][1], sgr], [hbr])
            self.dma("sp", d["hidT"][n0:n0 + n_, sl], hb[:n_, :], [hbr], [])
        self.multilinear([(xt, xr, C, W["w_ffn_gate"][L]), (xt, xr, C, W["w_ffn_up"][L])], DFF, epi, wb)
        self.flush()
        self.release(m)
        m = self.mark()
        KP = 16
        npiece = (CF + KP - 1) // KP
        wps = [self.sb([128, KP, 512], BF16) for _ in range(4)]
        stg = [self.sb([128, 512], F32) for _ in range(3)]
        ht, hr = self.sb([128, CF, 512], BF16)
        hv = d["hidT"].rearrange("(c p) s -> p c s", p=128)
        wv = W["w_ffn_down"][L].rearrange("(c p) n -> p c n", p=128)
        wi = 0
        kk = 0
        for tb in range(4):
            sl = slice(tb * 512, (tb + 1) * 512)
            step = max(1, CF // 4)
            for c0 in range(0, CF, step):
                c1 = min(CF, c0 + step)
                self.dma("sp", ht[:, c0:c1, :], hv[:, c0:c1, sl], [], [hr])
            for n0 in range(0, D, 512):
                ncol = min(512, D - n0)
                nn_ = ncol // 128
                pss = [self.bank() for _ in range(nn_)]
                for pi in range(npiece):
                    k0 = pi * KP
                    k1 = min(CF, k0 + KP)
                    wt, wr = wps[wi % 4]
                    wi += 1
                    self.dma("pool", wt[:, 0:k1 - k0, 0:ncol], wv[:, k0:k1, n0:n0 + ncol], [], [wr])
                    for kc in range(k0, k1):
                        for nn in range(nn_):
                            self.mm(pss[nn][0][:, :], wt[:, kc - k0, nn * 128:(nn + 1) * 128], ht[:, kc, :],
                                    kc == 0, kc == CF - 1, [wr, hr], [pss[nn][1]])
                for nn in range(nn_):
                    st, sr = stg[kk % 3]
                    kk += 1
                    self.cp("act" if kk % 2 else "dve", st[:, :], pss[nn][0][:, :], [pss[nn][1]], [sr])
                    self.dma("act", d["fT"][n0 + nn * 128:n0 + (nn + 1) * 128, sl], st[:, :], [sr], [])
        self.flush()
        self.rot = [0, 1, 2, 3]
        self.release(m)

    def ph_ple(self, L, W, pT, xsrc, xdst):
        d = self.dr
        D, C = self.D, self.C
        m = self.mark()
        self.rot = [0, 1, 2, 3, 4, 5, 6, 7]
        xt, xr = self.load_x(d["xbT"], C)
        pt, pr = self.sb([128, 2, S_TOK], BF16)
        self.dma("pool", pt[:], pT[L].rearrange("(c p) s -> p c s", p=128), [], [pr])
        wb = self.make_wbufs([C, 2], 3)
        sgs = [self.sb([128, 512], F32) for _ in range(2)]
        xs = [self.sb([128, 512], F32) for _ in range(3)]
        k = [0]

        def epi(pss, n0, n_, tb):
            sl = slice(tb * 512, (tb + 1) * 512)
            sg, sgr = sgs[k[0] % 2]
            xx, xxr = xs[k[0] % 3]
            k[0] += 1
            self.dma("sp", xx[:n_, :], xsrc[n0:n0 + n_, sl], [], [xxr])
            self.act(sg[:n_, :], pss[0][0][:n_, :], AF.Sigmoid, [pss[0][1]], [sgr])
            self.tt(sg[:n_, :], pss[1][0][:n_, :], sg[:n_, :], ALU.mult, [pss[1][1], sgr], [sgr])
            self.tt(xx[:n_, :], xx[:n_, :], sg[:n_, :], ALU.add, [xxr, sgr], [xxr])
            self.dma("act", xdst[n0:n0 + n_, sl], xx[:n_, :], [xxr], [])
        self.multilinear([(xt, xr, C, W["w_ple_gate"][L]), (pt, pr, 2, W["w_ple_proj"][L])], D, epi, wb)
        self.flush()
        self.rot = [0, 1, 2, 3]
        self.release(m)


WNAMES = ["w_in", "mla_w_uq", "mla_w_ukv", "w_branch_gate", "w_branch_a", "w_branch_b", "w_branch_c",
          "w_out", "w_ffn_gate", "w_ffn_up", "w_ffn_down", "w_ple_gate", "w_ple_proj"]


def host_consts():
    S = S_TOK
    c = {}
    c["ident"] = np.eye(128, dtype=np.float32)
    causal = np.zeros((4, 128, 512), np.float32)
    for m_ in range(4):
        kk = m_ * 128 + np.arange(128)[:, None]
        qq = np.arange(512)[None, :]
        causal[m_] = np.where(kk <= qq, 0.0, NEG)
    c["causal"] = causal
    pastneg = np.zeros((128, 16, 8), np.float32)
    notown = np.ones((128, 16, 8), np.float32)
    for t in range(16):
        for n in range(8):
            if n >= t // 2:
                pastneg[:, t, n] = -1e30
            if n == t // 2:
                notown[:, t, n] = 0.0
    c["pastneg"] = pastneg
    c["notown"] = notown
    E = np.zeros((8, 8, 128), np.float32)
    for n in range(8):
        E[n, n, :] = 1.0
    c["E"] = E
    half = 32
    inv = (1.0 / (10000.0 ** (np.arange(half, dtype=np.float32) / half))).astype(np.float32)
    rc = np.zeros((64, 2), np.float32)
    rc[:, 0] = np.concatenate([inv, inv])
    rc[:, 1] = np.concatenate([-np.ones(32), np.ones(32)])
    c["ropecols"] = rc
    cm = np.ones((16, S), np.float32)
    cm[:, ::64] = 0.0
    c["cmask"] = cm
    oh = np.zeros((16, 16, 128), np.float32)
    for h in range(16):
        oh[h, h, :] = 1.0
    c["onehot16"] = oh
    a = np.arange(128)[:, None]
    b = np.arange(128)[None, :]
    same = (a // 64) == (b // 64)
    c["negU"] = np.ascontiguousarray(np.broadcast_to(np.where(same & (b >= a), 0.0, NEG)[:, None, :], (128, 4, 128))).astype(np.float32)
    c["strU"] = np.ascontiguousarray(np.broadcast_to((same & (b > a)).astype(np.float32)[:, None, :], (128, 4, 128)))
    c["id8"] = np.ascontiguousarray(np.broadcast_to((b == a).astype(np.float32)[:, None, :], (128, 4, 128)))
    return c


def build(D, DFF, DEPTH, debug=(), phases=None):
    kb = KB(D, DFF, DEPTH, debug)
    nc = kb.nc
    S = S_TOK
    C = D // 128

    def inp(name, shape, dt=F32):
        return nc.dram_tensor(name, list(shape), dt, kind="ExternalInput").ap()
    xT = inp("xT", [D, S])
    pT = inp("pT", [DEPTH, 256, S])
    pos = inp("pos", [1, S], I32)
    W = {}
    shapes = {"w_in": [D, IN_WIDTH], "mla_w_uq": [768, 1536], "mla_w_ukv": [512, 2048], "w_branch_gate": [D, 3 * D],
              "w_branch_a": [1024, D], "w_branch_b": [2048, D], "w_branch_c": [1024, D], "w_out": [D, D],
              "w_ffn_gate": [D, DFF], "w_ffn_up": [D, DFF], "w_ffn_down": [DFF, D], "w_ple_gate": [D, D],
              "w_ple_proj": [256, D]}
    for n in WNAMES:
        W[n] = inp(n, [DEPTH] + shapes[n])
    for n in ("norm_mix_in", "norm_mix_out", "norm_ffn_in", "norm_ffn_out"):
        W[n] = inp(n, [DEPTH, 128, C])
    W["mla_q_norm_w"] = inp("mla_q_norm_w", [DEPTH, 128, 6])
    W["mla_kv_norm_w"] = inp("mla_kv_norm_w", [DEPTH, 128, 4])
    W["gdn_norm_w"] = inp("gdn_norm_w", [DEPTH, 128, 1])
    W["gdn_conv_w"] = inp("gdn_conv_w", [DEPTH, 128, 48, 4])
    W["gdn_hcols"] = inp("gdn_hcols", [DEPTH, 16, 2])
    hc = host_consts()
    CN = {k: inp("c_" + k, v.shape) for k, v in hc.items()}
    outT = nc.dram_tensor("outT", [D, S], F32, kind="ExternalOutput").ap()
    dr = kb.dram
    dr("xA", [D, S], F32)
    dr("xB", [D, S], F32)
    dr("hT", [D, S], BF16)
    dr("mqT", [1024, S], BF16)
    dr("mkT", [1024, S], BF16)
    dr("mv", [S, 1024], BF16)
    dr("gqkvT", [6144, S], F32)
    dr("gcT", [6144, S], F32)
    dr("gabT", [32, S], F32)
    dr("gzT", [2048, S], BF16)
    dr("gG", [6, 16, S], F32)
    dr("cqT", [768, S], F32)
    dr("ckvT", [512, S], F32)
    dr("krT", [64, S], F32)
    dr("krsT", [64, S], F32)
    dr("gateT", [3 * D, S], BF16)
    dr("yT", [4096, S], BF16)
    dr("mT", [D, S], BF16)
    dr("oT", [D, S], F32)
    dr("h2T", [D, S], BF16)
    dr("hidT", [DFF, S], BF16)
    dr("fT", [D, S], F32)
    dr("xbT", [D, S], BF16)
    dr("ropeT", [4, 64, S], F32)
    dr("QBd", [4, S], BF16)
    dr("KBd", [8, 4, S], BF16)
    d = kb.dr
    kb.setup_consts(CN["ident"])
    kb.ph_posconst(pos, CN)
    xcur = xT
    cnt = [0]

    def go(fn, *a, **k):
        cnt[0] += 1
        if phases is None or cnt[0] <= phases:
            fn(*a, **k)
    for L in range(DEPTH):
        go(kb.ph_norm, xcur, C, W["norm_mix_in"][L], d["hT"])
        go(kb.ph_inproj, L, d["hT"], W)
        go(kb.ph_moba, CN, pre=(kb.gdn_pre_alloc, lambda A, L=L: kb.gdn_pre_emit(L, W, CN, A)))
        go(lambda: None)
        go(kb.ph_gdn, L, W, CN)
        go(kb.ph_mla, L, W, CN)
        go(kb.ph_merge, L, W)
        go(kb.ph_linear_simple, d["mT"], C, W["w_out"][L], D, d["oT"], F32)
        go(kb.ph_resnorm, xcur, d["oT"], W["norm_mix_out"][L], d["xA"], gain_n=W["norm_ffn_in"][L], ndst=d["h2T"])
        go(kb.ph_ffn, L, W)
        go(kb.ph_resnorm, d["xA"], d["fT"], W["norm_ffn_out"][L], d["xB"], bdst=d["xbT"])
        last = (L == DEPTH - 1)
        xnext = outT if last else d["xA"]
        go(kb.ph_ple, L, W, pT, d["xB"], xnext)
        xcur = xnext
    return kb


def make_inputs_for_core(b, inputs, DEPTH, consts):
    f = np.float32
    m = {}
    m["xT"] = np.ascontiguousarray(inputs["x"][b].T)
    m["pT"] = np.ascontiguousarray(np.transpose(inputs["p"][:, b], (0, 2, 1)))
    m["pos"] = np.ascontiguousarray(inputs["positions"][b][None, :]).astype(np.int32)
    for k, v in consts.items():
        m["c_" + k] = v
    return m


def shared_inputs(inputs, D):
    C = D // 128
    m = {}
    for n in WNAMES:
        m[n] = np.ascontiguousarray(inputs[n], dtype=np.float32)
    for n in ("norm_mix_in", "norm_mix_out", "norm_ffn_in", "norm_ffn_out"):
        v = np.asarray(inputs[n], np.float32)
        m[n] = np.ascontiguousarray(v.reshape(v.shape[0], C, 128).transpose(0, 2, 1))
    v = np.asarray(inputs["mla_q_norm_w"], np.float32)
    m["mla_q_norm_w"] = np.ascontiguousarray(v.reshape(-1, 6, 128).transpose(0, 2, 1))
    v = np.asarray(inputs["mla_kv_norm_w"], np.float32)
    m["mla_kv_norm_w"] = np.ascontiguousarray(v.reshape(-1, 4, 128).transpose(0, 2, 1))
    v = np.asarray(inputs["gdn_norm_w"], np.float32)
    m["gdn_norm_w"] = np.ascontiguousarray(v.reshape(-1, 128, 1))
    v = np.asarray(inputs["gdn_conv_w"], np.float32)
    m["gdn_conv_w"] = np.ascontiguousarray(v.reshape(v.shape[0], 4, 48, 128).transpose(0, 3, 2, 1))
    m["gdn_hcols"] = np.ascontiguousarray(np.stack([np.asarray(inputs["gdn_a_log"], np.float32),
                                                   np.asarray(inputs["gdn_dt_bias"], np.float32)], axis=-1))
    return m


_CACHE = {}


def run(inputs, D, DFF, DEPTH, debug=(), trace=False, phases=None):
    B = inputs["x"].shape[0]
    key = (D, DFF, DEPTH, tuple(debug))
    kb = build(D, DFF, DEPTH, debug, phases)
    consts = host_consts()
    sh = shared_inputs(inputs, D)
    in_maps = []
    for b in range(B):
        m = make_inputs_for_core(b, inputs, DEPTH, consts)
        m.update(sh)
        in_maps.append(m)
    res = run_bass_kernel_spmd(kb.nc, in_maps, core_ids=list(range(B)), trace=trace)
    out = np.stack([np.ascontiguousarray(r["outT"].T) for r in res.results], axis=0)
    return out, res


def kernel(**inputs):
    inputs = {k: np.asarray(v) for k, v in inputs.items()}
    out, _ = run(inputs, 4096, 11008, 2)
    return out.astype(np.float32)
```
